# Optimizing a Trainium2 kernel written in Bass

```python
import jax, jax.numpy as jnp
from jax import lax
import numpy as np

D_MODEL = 1024
BATCH = 8
SEQ = 4096
DEPTH = 4

N_META = 16
H_A = 4
QK_NOPE = 128
QK_ROPE = 64
V_DIM = 128
Q_LORA = 256
KV_LORA = 128
ROPE_THETA = 10000.0
Q_BLOCK = 128
H_B = 8
N_B = 64
RW = H_B * N_B
DECAY_LORA = 64
AAA_LORA = 64
GATE_LORA = 128
VRES_LORA = 32
RWKV_GN_EPS = 64e-5
MIX = H_A * V_DIM + RW
MLA_COLS = Q_LORA + KV_LORA + QK_ROPE
RWKV_COLS = 3 * RW + 2 * DECAY_LORA + 2 * AAA_LORA + GATE_LORA
IN_COLS = MLA_COLS + RWKV_COLS
N_GROUPS = 4
EXPERTS_PER_GROUP = 8
N_EXPERTS = N_GROUPS * EXPERTS_PER_GROUP
TOP_K = 2
E_HID = 256
MOE_BLOCK = 128
DN_ALPHA = (2 * DEPTH) ** 0.25
DN_BETA = (8 * DEPTH) ** -0.25
LN_EPS = 1e-5
RMS_EPS = 1e-6

kernel_name = 'hymba_mla_rwkv7_hmoe_deepnorm_encoder'


def _split(t, sizes):
    return jnp.split(t, np.cumsum(sizes)[:-1].tolist(), axis=-1)


def _layer_norm(x, g, b):
    xf = x.astype(jnp.float32)
    mu = jnp.mean(xf, -1, keepdims=True)
    var = jnp.mean(jnp.square(xf - mu), -1, keepdims=True)
    return ((xf - mu) * lax.rsqrt(var + LN_EPS) * g + b).astype(x.dtype)


def _rms_norm(x, g):
    xf = x.astype(jnp.float32)
    return (xf * lax.rsqrt(jnp.mean(xf * xf, -1, keepdims=True) + RMS_EPS) * g).astype(x.dtype)


def _rope(t, cos, sin):
    half = t.shape[-1] // 2
    t1, t2 = t[..., :half], t[..., half:]
    return jnp.concatenate([t1 * cos - t2 * sin, t2 * cos + t1 * sin], -1).astype(t.dtype)


def _centred_shift(p, mu_prev, mu_next):
    prev = jnp.pad(p[:, :-1], ((0, 0), (1, 0), (0, 0)))
    nxt = jnp.pad(p[:, 1:], ((0, 0), (0, 1), (0, 0)))
    return p + mu_prev * (prev - p) + mu_next * (nxt - p)


def _mla(c_q, c_kv, k_pe, cos, sin, q_norm, kv_norm, w_uq, w_ukv, out_norm):
    B, L, _ = c_q.shape
    q = (_rms_norm(c_q, q_norm) @ w_uq).reshape(B, L, H_A, QK_NOPE + QK_ROPE)
    q_nope = q[..., :QK_NOPE]
    q_pe = _rope(q[..., QK_NOPE:], cos[:, :, None, :], sin[:, :, None, :])
    kv = (_rms_norm(c_kv, kv_norm) @ w_ukv).reshape(B, L, H_A, QK_NOPE + V_DIM)
    k_nope, v = kv[..., :QK_NOPE], kv[..., QK_NOPE:]
    k_pe = _rope(k_pe, cos, sin)
    scale = (QK_NOPE + QK_ROPE) ** -0.5

    def attend(blk):
        qn, qp = blk
        s = jnp.einsum('bqhd,bkhd->bhqk', qn, k_nope) + jnp.einsum('bqhd,bkd->bhqk', qp, k_pe)
        p = jax.nn.softmax(s.astype(jnp.float32) * scale, axis=-1).astype(v.dtype)
        return jnp.einsum('bhqk,bkhd->bqhd', p, v)

    out_meta = attend((q_nope[:, :N_META], q_pe[:, :N_META]))
    n_blk = (L - N_META) // Q_BLOCK

    def to_blocks(t):
        return jnp.moveaxis(t[:, N_META:].reshape(B, n_blk, Q_BLOCK, H_A, t.shape[-1]), 1, 0)

    out_real = lax.map(attend, (to_blocks(q_nope), to_blocks(q_pe)))
    out_real = jnp.moveaxis(out_real, 0, 1).reshape(B, L - N_META, H_A, V_DIM)
    out = jnp.concatenate([out_meta, out_real], axis=1)
    out = _rms_norm(out, out_norm.reshape(H_A, V_DIM))
    return out.reshape(B, L, H_A * V_DIM)


def _wkv_scan(r, decay, k, v, kk, a, reverse):
    B = r.shape[0]

    def step(S, inp):
        r_t, w_t, k_t, v_t, kk_t, b_t = inp
        sa = jnp.einsum('bhvk,bhk->bhv', S, kk_t)
        S = S * w_t[:, :, None, :] - sa[..., None] * b_t[:, :, None, :] + v_t[..., None] * k_t[:, :, None, :]
        return S, jnp.einsum('bhvk,bhk->bhv', S, r_t)

    xs = tuple(jnp.moveaxis(t, 1, 0) for t in (r, decay, k, v, kk, kk * a))
    S0 = jnp.zeros((B, H_B, N_B, N_B), jnp.float32)
    _, ys = lax.scan(step, S0, xs, reverse=reverse)
    return jnp.moveaxis(ys, 0, 1)


def _rwkv_bidir(u, v_first, vres, w0, w2, a0, a2, g2, k_k, k_a, r_k, gn_g, gn_b):
    dt = u.dtype
    u = u.astype(jnp.float32)
    B, L, _ = u.shape
    r, k, v, wd_f, wd_b, ad_f, ad_b, gd = _split(
        u, [RW, RW, RW, DECAY_LORA, DECAY_LORA, AAA_LORA, AAA_LORA, GATE_LORA])
    if vres is None:
        v_first = v
    else:
        v0, v1, v2 = vres
        v = v + (v_first - v) * jax.nn.sigmoid(v0 + (v @ v1) @ v2)
    g = jax.nn.sigmoid(gd) @ g2

    def heads(t):
        return t.reshape(B, L, H_B, N_B)

    kk = heads(k * k_k)
    kk = kk * lax.rsqrt(jnp.sum(kk * kk, -1, keepdims=True) + 1e-12)
    rh, vh = heads(r), heads(v)

    def direction(d, wd, ad, reverse):
        wl = w0[d] + jnp.tanh(wd) @ w2[d]
        decay = jnp.exp(-jnp.exp(-jax.nn.softplus(-wl) - 0.5))
        a = jax.nn.sigmoid(a0[d] + ad @ a2[d])
        kd = heads(k * (1.0 + (a - 1.0) * k_a))
        y = _wkv_scan(rh, heads(decay), kd, vh, kk, heads(a), reverse)
        bonus = jnp.sum(rh * kd * r_k, -1, keepdims=True) * vh
        return y, bonus

    y_f, bonus_f = direction(0, wd_f, ad_f, False)
    y_b, bonus_b = direction(1, wd_b, ad_b, True)
    y = y_f + y_b
    mu = jnp.mean(y, -1, keepdims=True)
    var = jnp.mean(jnp.square(y - mu), -1, keepdims=True)
    yn = ((y - mu) * lax.rsqrt(var + RWKV_GN_EPS)).reshape(B, L, RW) * gn_g + gn_b
    out = (yn + (bonus_f + bonus_b).reshape(B, L, RW)) * g
    return out.astype(dt), v_first


def _hier_moe(h, w_group, b_group, w_expert, b_expert, w1, w3, w2):
    T, D = h.shape
    g_prob = jax.nn.softmax((h @ w_group).astype(jnp.float32) + b_group, axis=-1)
    g_top_p, g_top = lax.top_k(g_prob, 1)
    e_logits = ((h @ w_expert).astype(jnp.float32) + b_expert).reshape(T, N_GROUPS, EXPERTS_PER_GROUP)
    e_logits = jnp.take_along_axis(e_logits, g_top[:, :, None], axis=1)[:, 0]
    e_top_p, e_top = lax.top_k(jax.nn.softmax(e_logits, axis=-1), TOP_K)
    gate = (g_top_p * e_top_p).reshape(-1)
    flat_e = (g_top * EXPERTS_PER_GROUP + e_top).reshape(-1)
    flat_tok = jnp.repeat(jnp.arange(T, dtype=jnp.int32), TOP_K)
    order = jnp.argsort(flat_e)
    se = flat_e[order]
    counts = jnp.bincount(flat_e, length=N_EXPERTS)
    starts = jnp.cumsum(counts) - counts
    padded = (counts + MOE_BLOCK - 1) // MOE_BLOCK * MOE_BLOCK
    pends = jnp.cumsum(padded)
    dest = (pends - padded)[se] + jnp.arange(T * TOP_K) - starts[se]
    n_rows = (T * TOP_K + N_EXPERTS * (MOE_BLOCK - 1) + MOE_BLOCK - 1) // MOE_BLOCK * MOE_BLOCK
    n_blocks = n_rows // MOE_BLOCK
    row_tok = jnp.zeros((n_rows,), jnp.int32).at[dest].set(flat_tok[order])
    row_gate = jnp.zeros((n_rows,), jnp.float32).at[dest].set(gate[order])
    block_e = jnp.clip(jnp.searchsorted(pends, jnp.arange(n_blocks) * MOE_BLOCK, side='right'),
                       0, N_EXPERTS - 1)
    xb = h[row_tok].reshape(n_blocks, MOE_BLOCK, D)

    def expert_block(args):
        xe, e = args
        return (jax.nn.silu(xe @ w1[e]) * (xe @ w3[e])) @ w2[e]

    yb = lax.map(expert_block, (xb, block_e)).reshape(n_rows, D)
    y = jnp.zeros((T, D), jnp.float32).at[row_tok].add(yb.astype(jnp.float32) * row_gate[:, None])
    return y.astype(h.dtype)


def setup_inputs(seed: int = 0) -> dict:
    key = jax.random.key(seed)
    ks = iter(jax.random.split(key, 48))

    def nrm(shape, s):
        return jax.random.normal(next(ks), shape, jnp.float32) * s

    def uni(shape, lo, hi):
        return jax.random.uniform(next(ks), shape, jnp.float32, lo, hi)

    D = D_MODEL
    return {
        'x': nrm((BATCH, SEQ, D), 1.0),
        'positions': jnp.tile(jnp.arange(SEQ, dtype=jnp.int32)[None], (BATCH, 1)),
        'meta_tokens': nrm((N_META, D), 1.0),
        'emb_ln_g': 1.0 + nrm((D,), 0.02),
        'emb_ln_b': nrm((D,), 0.02),
        'w_in': nrm((DEPTH, D, IN_COLS), D ** -0.5),
        'mla_q_norm': 1.0 + nrm((DEPTH, Q_LORA), 0.02),
        'mla_kv_norm': 1.0 + nrm((DEPTH, KV_LORA), 0.02),
        'mla_w_uq': nrm((DEPTH, Q_LORA, H_A * (QK_NOPE + QK_ROPE)), Q_LORA ** -0.5),
        'mla_w_ukv': nrm((DEPTH, KV_LORA, H_A * (QK_NOPE + V_DIM)), KV_LORA ** -0.5),
        'mla_out_norm': 1.0 + nrm((DEPTH, H_A * V_DIM), 0.02),
        'rwkv_mu_prev': uni((DEPTH, RWKV_COLS), 0.0, 0.5),
        'rwkv_mu_next': uni((DEPTH, RWKV_COLS), 0.0, 0.5),
        'rwkv_w0': uni((DEPTH, 2, RW), -4.0, 0.0),
        'rwkv_w2': nrm((DEPTH, 2, DECAY_LORA, RW), 0.1),
        'rwkv_a0': nrm((DEPTH, 2, RW), 0.3),
        'rwkv_a2': nrm((DEPTH, 2, AAA_LORA, RW), 0.1),
        'rwkv_g2': nrm((DEPTH, GATE_LORA, RW), GATE_LORA ** -0.5),
        'rwkv_k_k': 0.85 + nrm((DEPTH, RW), 0.05),
        'rwkv_k_a': 1.0 + nrm((DEPTH, RW), 0.05),
        'rwkv_r_k': nrm((DEPTH, H_B, N_B), 0.1),
        'rwkv_gn_g': 1.0 + nrm((DEPTH, RW), 0.02),
        'rwkv_gn_b': nrm((DEPTH, RW), 0.02),
        'rwkv_v0': nrm((DEPTH - 1, RW), 0.3),
        'rwkv_v1': nrm((DEPTH - 1, RW, VRES_LORA), RW ** -0.5),
        'rwkv_v2': nrm((DEPTH - 1, VRES_LORA, RW), 0.5 * VRES_LORA ** -0.5),
        'w_out': nrm((DEPTH, MIX, D), DN_BETA * MIX ** -0.5),
        'ln1_g': 1.0 + nrm((DEPTH, D), 0.02),
        'ln1_b': nrm((DEPTH, D), 0.02),
        'moe_w_group': nrm((DEPTH, D, N_GROUPS), D ** -0.5),
        'moe_b_group': nrm((DEPTH, N_GROUPS), 0.01),
        'moe_w_expert': nrm((DEPTH, D, N_EXPERTS), D ** -0.5),
        'moe_b_expert': nrm((DEPTH, N_EXPERTS), 0.01),
        'moe_w1': nrm((DEPTH, N_EXPERTS, D, E_HID), D ** -0.5),
        'moe_w3': nrm((DEPTH, N_EXPERTS, D, E_HID), D ** -0.5),
        'moe_w2': nrm((DEPTH, N_EXPERTS, E_HID, D), DN_BETA * E_HID ** -0.5),
        'ln2_g': 1.0 + nrm((DEPTH, D), 0.02),
        'ln2_b': nrm((DEPTH, D), 0.02),
    }


def reference(x, positions, meta_tokens, emb_ln_g, emb_ln_b, w_in, mla_q_norm, mla_kv_norm,
              mla_w_uq, mla_w_ukv, mla_out_norm, rwkv_mu_prev, rwkv_mu_next, rwkv_w0, rwkv_w2,
              rwkv_a0, rwkv_a2, rwkv_g2, rwkv_k_k, rwkv_k_a, rwkv_r_k, rwkv_gn_g, rwkv_gn_b,
              rwkv_v0, rwkv_v1, rwkv_v2, w_out, ln1_g, ln1_b, moe_w_group, moe_b_group,
              moe_w_expert, moe_b_expert, moe_w1, moe_w3, moe_w2, ln2_g, ln2_b):
    B = x.shape[0]
    meta = jnp.broadcast_to(meta_tokens.astype(x.dtype)[None], (B, N_META, D_MODEL))
    h = _layer_norm(jnp.concatenate([meta, x], axis=1), emb_ln_g, emb_ln_b)
    L = h.shape[1]
    pos = jnp.concatenate([jnp.broadcast_to(jnp.arange(N_META, dtype=jnp.int32), (B, N_META)),
                           positions + N_META], axis=1)
    inv_freq = ROPE_THETA ** (-jnp.arange(0, QK_ROPE, 2, dtype=jnp.float32) / QK_ROPE)
    ang = pos.astype(jnp.float32)[..., None] * inv_freq
    cos, sin = jnp.cos(ang), jnp.sin(ang)
    v_first = None
    for li in range(DEPTH):
        proj = h @ w_in[li]
        c_q, c_kv, k_pe, rw = _split(proj, [Q_LORA, KV_LORA, QK_ROPE, RWKV_COLS])
        y_att = _mla(c_q, c_kv, k_pe, cos, sin, mla_q_norm[li], mla_kv_norm[li],
                     mla_w_uq[li], mla_w_ukv[li], mla_out_norm[li])
        u = _centred_shift(rw, rwkv_mu_prev[li], rwkv_mu_next[li])
        vres = None if li == 0 else (rwkv_v0[li - 1], rwkv_v1[li - 1], rwkv_v2[li - 1])
        y_rwkv, v_first = _rwkv_bidir(u, v_first, vres, rwkv_w0[li], rwkv_w2[li], rwkv_a0[li],
                                      rwkv_a2[li], rwkv_g2[li], rwkv_k_k[li], rwkv_k_a[li],
                                      rwkv_r_k[li], rwkv_gn_g[li], rwkv_gn_b[li])
        mixed = jnp.concatenate([y_att, y_rwkv], axis=-1) @ w_out[li]
        h = _layer_norm(DN_ALPHA * h + mixed, ln1_g[li], ln1_b[li])
        ff = _hier_moe(h.reshape(B * L, D_MODEL), moe_w_group[li], moe_b_group[li],
                       moe_w_expert[li], moe_b_expert[li], moe_w1[li], moe_w3[li],
                       moe_w2[li]).reshape(B, L, D_MODEL)
        h = _layer_norm(DN_ALPHA * h + ff, ln2_g[li], ln2_b[li])
    return h[:, N_META:]
```

```python
from contextlib import ExitStack
import numpy as np
import concourse.bass as bass
import concourse.mybir as mybir
from concourse.bass_utils import run_bass_kernel_spmd

F32 = mybir.dt.float32
BF16 = mybir.dt.bfloat16
I32 = mybir.dt.int32
AF = mybir.ActivationFunctionType
ALU = mybir.AluOpType
AX = mybir.AxisListType

D = 1024
SEQ = 4096
DEPTH = 4
NMETA = 16
L = SEQ + NMETA
NT = 33
LP = NT * 128
HA = 4
QKN = 128
QKR = 64
VD = 128
QL = 256
KVL = 128
HB = 8
NB = 64
RW = 512
INC = 2368
RWC = 1920
NG = 4
EPG = 8
NE = 32
EH = 256
DN_ALPHA = (2 * DEPTH) ** 0.25
LN_EPS = 1e-5
RMS_EPS = 1e-6
GN_EPS = 64e-5
ATT_SCALE = (QKN + QKR) ** -0.5
CH = 64
NCH = 65


def ts(i):
    return 128 if i < NT - 1 else L - 128 * (NT - 1)


COMPUTE = ('pe', 'act', 'dve', 'pool')
N_DMA_SEMS = 32
N_SW_SEMS = 3
SEM_WRAP = 20000


class _Buf:
    __slots__ = ('w', 'r')

    def __init__(self):
        self.w = None
        self.r = []


class _Op:
    __slots__ = ('eng', 'fn', 'deps', 'is_dma', 'needed', 'tok', 'idx')


class Sched:
    def __init__(self, nc):
        self.nc = nc
        self.engs = {'pe': nc.tensor, 'act': nc.scalar, 'dve': nc.vector,
                     'pool': nc.gpsimd, 'sp': nc.sync}
        self.ops = []
        self.bufs = {}
        self.last = {}
        self.dmas_since_bar = []
        self.sfx = ''
        self.nosfx = ()

    def _b(self, name):
        b = self.bufs.get(name)
        if b is None:
            b = self.bufs[name] = _Buf()
        return b

    def op(self, eng, fn, reads=(), writes=(), dma=False):
        o = _Op()
        o.eng = eng
        o.fn = fn
        o.is_dma = dma
        o.needed = False
        o.tok = None
        o.idx = len(self.ops)
        deps = set()
        if self.sfx:
            reads = [x if x in self.nosfx else x + self.sfx for x in reads]
            writes = [x if x in self.nosfx else x + self.sfx for x in writes]
        rb = [self._b(x) for x in reads]
        wb = [self._b(x) for x in writes]
        for b in rb:
            if b.w is not None:
                deps.add(b.w)
        for b in wb:
            if b.w is not None:
                deps.add(b.w)
            deps.update(b.r)
        for b in rb:
            b.r.append(o.idx)
        for b in wb:
            b.w = o.idx
            b.r = []
        deps.discard(o.idx)
        o.deps = deps
        self.ops.append(o)
        if dma:
            self.dmas_since_bar.append(o.idx)
        else:
            self.last[eng] = o.idx
        return o

    def barrier(self):
        pend = set(self.dmas_since_bar) | set(self.last.values())
        self.dmas_since_bar = []
        for e in ('pe', 'act', 'dve', 'pool', 'sp'):
            o = self.op(e, lambda en: en.nop())
            o.deps |= {p for p in pend if p != o.idx}
        self.bufs = {}

    def emit(self):
        nc = self.nc
        ops = self.ops
        for o in ops:
            nd = set()
            best = {}
            for d in o.deps:
                p = ops[d]
                if p.is_dma:
                    nd.add(d)
                    continue
                if (not o.is_dma) and p.eng == 'pe' and o.eng == 'pe':
                    continue
                if best.get(p.eng, -1) < d:
                    best[p.eng] = d
            nd.update(best.values())
            for d in nd:
                ops[d].needed = True
            o.deps = nd
        csem = {e: nc.alloc_semaphore(name=f"c_{e}_0") for e in COMPUTE}
        csem['sp'] = nc.alloc_semaphore(name="c_sp_0")
        ccnt = {e: 0 for e in csem}
        cgen = {e: 0 for e in csem}
        dsems = [nc.alloc_semaphore(name=f"d_{i}") for i in range(N_DMA_SEMS + N_SW_SEMS)]
        dcnt = [0] * (N_DMA_SEMS + N_SW_SEMS)
        dnext = 0
        swnext = 0
        waited = {}
        n_wait = 0
        for o in ops:
            e = self.engs[o.eng]
            for d in sorted(o.deps):
                p = ops[d]
                sem, val = p.tok
                key = (o.eng, sem.num)
                if waited.get(key, 0) >= val:
                    continue
                e.wait_ge(sem, val)
                n_wait += 1
                waited[key] = val
            if o.is_dma:
                if o.eng == 'pool':
                    si = N_DMA_SEMS + swnext
                    swnext = (swnext + 1) % N_SW_SEMS
                else:
                    si = dnext
                    dnext = (dnext + 1) % N_DMA_SEMS
                sem = dsems[si]
                if dcnt[si] > 0:
                    key = (o.eng, sem.num)
                    if waited.get(key, 0) < dcnt[si]:
                        e.wait_ge(sem, dcnt[si])
                        waited[key] = dcnt[si]
                        n_wait += 1
                ins = o.fn(e)
                dcnt[si] += 16
                ins.then_inc(sem, 16)
                o.tok = (sem, dcnt[si])
            else:
                ins = o.fn(e)
                if o.needed:
                    if ccnt[o.eng] >= SEM_WRAP:
                        cgen[o.eng] += 1
                        csem[o.eng] = nc.alloc_semaphore(name=f"c_{o.eng}_{cgen[o.eng]}")
                        ccnt[o.eng] = 0
                    ccnt[o.eng] += 1
                    sem = csem[o.eng]
                    ins.then_inc(sem, 1)
                    o.tok = (sem, ccnt[o.eng])
        return dict(n_ops=len(ops), n_wait=n_wait, ccnt=dict(ccnt), cgen=dict(cgen), dcnt_max=max(dcnt))


class Builder:
    def __init__(self, nc, dbg=()):
        self.nc = nc
        self.S = Sched(nc)
        self.dbg = set(dbg)
        self.out_dmas = []
        self._uid = 0

    def sb(self, stack, name, shape, dt):
        self._uid += 1
        return stack.enter_context(self.nc.sbuf_tensor(f"{name}_u{self._uid}", list(shape), dt))

    def ps(self, stack, name, shape, dt=F32):
        self._uid += 1
        return stack.enter_context(self.nc.psum_tensor(f"{name}_u{self._uid}", list(shape), dt))

    def dram(self, name, shape, dt, kind=None):
        if kind is None:
            kind = "ExternalOutput" if name in self.dbg else "Internal"
        return self.nc.dram_tensor(name, list(shape), dt, kind=kind).ap()

    def mm(self, out, lhsT, rhs, start=True, stop=True, rd=(), wr=()):
        return self.S.op('pe', lambda e: e.matmul(out, lhsT=lhsT, rhs=rhs, start=start, stop=stop),
                         reads=rd, writes=wr)

    def tr(self, out, in_, ident, rd=(), wr=()):
        return self.S.op('pe', lambda e: e.transpose(out=out, in_=in_, identity=ident),
                         reads=rd, writes=wr)

    def act(self, out, in_, func, rd=(), wr=(), bias=None, scale=None, accum=None, eng='act'):
        kw = {}
        if bias is not None:
            kw['bias'] = bias
        if scale is not None:
            kw['scale'] = scale
        if accum is not None:
            kw['accum_out'] = accum
        return self.S.op(eng, lambda e: e.activation(out=out, in_=in_, func=func, **kw),
                         reads=rd, writes=wr)

    def tt(self, eng, out, in0, in1, op, rd=(), wr=()):
        return self.S.op(eng, lambda e: e.tensor_tensor(out=out, in0=in0, in1=in1, op=op),
                         reads=rd, writes=wr)

    def tsc(self, eng, out, in0, s1, s2, op0, op1=None, rd=(), wr=()):
        if op1 is None:
            return self.S.op(eng, lambda e: e.tensor_scalar(out=out, in0=in0, scalar1=s1, scalar2=None,
                                                            op0=op0), reads=rd, writes=wr)
        return self.S.op(eng, lambda e: e.tensor_scalar(out=out, in0=in0, scalar1=s1, scalar2=s2,
                                                        op0=op0, op1=op1), reads=rd, writes=wr)

    def stt(self, out, in0, scalar, in1, op0, op1, rd=(), wr=()):
        return self.S.op('dve', lambda e: e.scalar_tensor_tensor(out=out, in0=in0, scalar=scalar, in1=in1,
                                                                 op0=op0, op1=op1), reads=rd, writes=wr)

    def cp(self, eng, out, in_, rd=(), wr=()):
        if eng == 'act':
            return self.S.op('act', lambda e: e.copy(out=out, in_=in_), reads=rd, writes=wr)
        return self.S.op(eng, lambda e: e.tensor_copy(out=out, in_=in_), reads=rd, writes=wr)

    def recip(self, out, in_, rd=(), wr=()):
        return self.S.op('dve', lambda e: e.reciprocal(out=out, in_=in_), reads=rd, writes=wr)

    def red(self, out, in_, op, rd=(), wr=(), axis=AX.X):
        return self.S.op('dve', lambda e: e.tensor_reduce(out=out, in_=in_, axis=axis, op=op),
                         reads=rd, writes=wr)

    def memset(self, eng, ap, val, wr=()):
        return self.S.op(eng, lambda e: e.memset(ap, val), writes=wr)

    def dma(self, q, out, in_, rd=(), wr=()):
        return self.S.op(q, lambda e: e.dma_start(out=out, in_=in_), reads=rd, writes=wr, dma=True)

    def barrier(self):
        self.S.barrier()

    def rsqrt_(self, ap, mul, eps, key):
        self.act(ap, ap, AF.Sqrt, rd=[key], wr=[key], bias=self.eps_ap(eps, ap.shape[0]), scale=mul)
        self.recip(ap, ap, rd=[key], wr=[key])

    def eps_ap(self, eps, n):
        return self.epst[eps][:n, :]


def host_consts():
    c = {}
    c['c_ident'] = np.eye(128, dtype=np.float32)
    c['c_invf'] = (10000.0 ** (-np.arange(0, 64, 2, dtype=np.float32) / 64) / (2 * np.pi)).astype(np.float32)
    p = np.arange(128)
    same = (p[:, None] // 64) == (p[None, :] // 64)
    dc = -float(np.exp(-0.5))
    tri = np.zeros((4, 128, 128), np.float32)
    tri[0] = dc * (same & (p[:, None] <= p[None, :]))
    tri[1] = dc * (same & (p[:, None] < p[None, :]))
    tri[2] = dc * (same & (p[:, None] >= p[None, :]))
    tri[3] = dc * (same & (p[:, None] > p[None, :]))
    c['c_tri'] = tri
    r = (p % 64)[:, None]
    cc = np.arange(64)[None, :]
    c['c_masks'] = np.stack([r < cc, r <= cc, r > cc, r >= cc, r == cc]).astype(np.float32)
    c['c_moe'] = np.concatenate([128.0 * np.arange(33), 128.0 * np.arange(96)]).astype(np.float32)
    c['c_trix'] = (p[:, None] < p[None, :]).astype(np.float32)
    return c


def host_layout(inputs):
    m = {}
    m['moe_wr'] = np.ascontiguousarray(np.concatenate([inputs['moe_w_group'], inputs['moe_w_expert']], axis=-1))
    m['moe_br'] = np.ascontiguousarray(np.concatenate([inputs['moe_b_group'], inputs['moe_b_expert']], axis=-1))
    return m


def build_program(nlayers=DEPTH, dbg=(), stop_after=None):
    nc = bass.Bass("TRN2", target_bir_lowering=False)
    B = Builder(nc, dbg)
    S = B.S

    def inp(name, shape, dt=F32):
        return nc.dram_tensor(name, list(shape), dt, kind="ExternalInput").ap()

    x = inp("x", [SEQ, D])
    pos_in = inp("positions", [SEQ, 1], I32)
    meta = inp("meta_tokens", [NMETA, D])
    emb_g = inp("emb_ln_g", [D])
    emb_b = inp("emb_ln_b", [D])
    w_in = inp("w_in", [DEPTH, D, INC])
    q_norm = inp("mla_q_norm", [DEPTH, QL])
    kv_norm = inp("mla_kv_norm", [DEPTH, KVL])
    w_uq = inp("mla_w_uq", [DEPTH, QL, HA * (QKN + QKR)])
    w_ukv = inp("mla_w_ukv", [DEPTH, KVL, HA * (QKN + VD)])
    out_norm = inp("mla_out_norm", [DEPTH, HA * VD])
    ident_in = inp("c_ident", [128, 128])
    tri_in = inp("c_tri", [4, 128, 128])
    cmoe_in = inp("c_moe", [33 + 96])
    trix_in = inp("c_trix", [128, 128])
    masks_in = inp("c_masks", [5, 128, 64])
    ff_params = dict(
        w_out=inp("w_out", [DEPTH, D, D]), ln1_g=inp("ln1_g", [DEPTH, D]), ln1_b=inp("ln1_b", [DEPTH, D]),
        wr=inp("moe_wr", [DEPTH, D, NG + NE]), br=inp("moe_br", [DEPTH, NG + NE]),
        w1=inp("moe_w1", [DEPTH, NE, D, EH]), w3=inp("moe_w3", [DEPTH, NE, D, EH]), w2=inp("moe_w2", [DEPTH, NE, EH, D]),
        ln2_g=inp("ln2_g", [DEPTH, D]), ln2_b=inp("ln2_b", [DEPTH, D]))
    rw_params = dict(
        mu_prev=inp("rwkv_mu_prev", [DEPTH, RWC]), mu_next=inp("rwkv_mu_next", [DEPTH, RWC]),
        w0=inp("rwkv_w0", [DEPTH, 2, RW]), w2=inp("rwkv_w2", [DEPTH, 2, 64, RW]),
        a0=inp("rwkv_a0", [DEPTH, 2, RW]), a2=inp("rwkv_a2", [DEPTH, 2, 64, RW]),
        g2=inp("rwkv_g2", [DEPTH, 128, RW]), k_k=inp("rwkv_k_k", [DEPTH, RW]), k_a=inp("rwkv_k_a", [DEPTH, RW]),
        r_k=inp("rwkv_r_k", [DEPTH, HB, NB]), gn_g=inp("rwkv_gn_g", [DEPTH, RW]), gn_b=inp("rwkv_gn_b", [DEPTH, RW]),
        v0=inp("rwkv_v0", [DEPTH - 1, RW]), v1=inp("rwkv_v1", [DEPTH - 1, RW, 32]), v2=inp("rwkv_v2", [DEPTH - 1, 32, RW]))
    invf_in = inp("c_invf", [32])
    out = nc.dram_tensor("out", [SEQ, D], F32, kind="ExternalOutput").ap()

    hD = B.dram("hD", [LP, D], F32)
    projD = B.dram("projD", [LP, INC], F32)
    ymixD = B.dram("ymixD", [LP, D], BF16)
    moe_scr = dict(W13D=B.dram("W13D", [NE * 128, 8 * 2 * EH], BF16), W2D=B.dram("W2D", [NE * 128, 2 * D], BF16),
                   XgD=B.dram("XgD", [96 * 128, D], BF16), YgD=B.dram("YgD", [96 * 128, D], F32))
    hbD = B.dram("hbD", [LP, D], BF16)
    rw_scr = dict(opsD=[B.dram(f"opsD{d}", [LP, 4, RW], BF16) for d in range(2)],
                  vbD=B.dram("vbD", [LP, RW], BF16), gCD=B.dram("gCD", [NT, 64, 32], F32),
                  yD=[B.dram(f"yD{d}", [LP, RW], F32) for d in range(2)],
                  bonusD=B.dram("bonusD", [LP, RW], F32), gD=B.dram("gD", [LP, RW], F32),
                  vfD=B.dram("vfD", [LP, RW], F32))

    with ExitStack() as gs:
        ident = B.sb(gs, "ident", [128, 128], F32)
        identb = B.sb(gs, "identb", [128, 128], BF16)
        cosT = B.sb(gs, "cosT", [128, NT, 32], F32)
        sinT = B.sb(gs, "sinT", [128, NT, 32], F32)
        B.epst = {}
        for ev in (LN_EPS, RMS_EPS, GN_EPS, 1e-12, 1e-30):
            t = B.sb(gs, f"eps{len(B.epst)}", [128, 1], F32)
            B.memset('pool', t[:], ev, wr=['eps'])
            B.epst[ev] = t
        B.dma('sp', ident[:], ident_in, wr=['ident'])
        tri_t = [B.sb(gs, f"tri{q}", [128, 128], F32) for q in range(4)]
        for q in range(4):
            B.dma('sp', tri_t[q][:], tri_in[q], wr=['tri'])
        mk_t = [B.sb(gs, f"mk{q}", [128, 64], F32) for q in range(5)]
        for q in range(5):
            B.dma('sp', mk_t[q][:], masks_in[q], wr=['masks'])
        negc = B.sb(gs, "negc", [128, 1], F32)
        B.memset('pool', negc[:], DEC_C, wr=['negc'])
        rw_consts = dict(tri=[(tri_t[0], tri_t[1]), (tri_t[2], tri_t[3])], negc=negc,
                         masks=dict(lt=mk_t[0], le=mk_t[1], gt=mk_t[2], ge=mk_t[3]), i64=mk_t[4])
        B.cp('dve', identb[:], ident[:], rd=['ident'], wr=['identb'])

        with ExitStack() as st:
            posi = B.sb(st, "posi", [128, NT], I32)
            posf = B.sb(st, "posf", [128, NT], F32)
            invf = B.sb(st, "invf", [128, 32], F32)
            fr = B.sb(st, "fr", [128, NT, 32], F32)
            fri = B.sb(st, "fri", [128, NT, 32], I32)
            frf = B.sb(st, "frf", [128, NT, 32], F32)
            msk = B.sb(st, "msk", [128, NT, 32], F32)
            B.memset('pool', posi[:], 0, wr=['posi'])
            B.dma('sp', posi[16:128, 0:1], pos_in[0:112, :], rd=[], wr=['posi'])
            for i in range(1, NT):
                n = ts(i)
                B.dma('sp', posi[:n, i:i + 1], pos_in[128 * i - 16:128 * i - 16 + n, :], wr=['posi'])
            B.dma('sp', invf[:], invf_in.partition_broadcast(128), wr=['invf'])
            B.cp('dve', posf[:], posi[:], rd=['posi'], wr=['posf'])
            B.tsc('dve', posf[:], posf[:], float(NMETA), None, ALU.add, rd=['posf'], wr=['posf'])
            S.op('pool', lambda e: e.iota(posi[0:16, 0:1], pattern=[[0, 1]], base=0, channel_multiplier=1),
                 reads=[], writes=['posi'])
            B.cp('dve', posf[0:16, 0:1], posi[0:16, 0:1], rd=['posi', 'posf'], wr=['posf'])
            for (tab, off) in ((sinT, 0.0), (cosT, 0.25)):
                B.tt('dve', fr[:], posf[:].unsqueeze(2).broadcast_to([128, NT, 32]),
                     invf[:].unsqueeze(1).broadcast_to([128, NT, 32]), ALU.mult,
                     rd=['posf', 'invf'], wr=['fr'])
                if off:
                    B.tsc('dve', fr[:], fr[:], off, None, ALU.add, rd=['fr'], wr=['fr'])
                B.cp('dve', fri[:], fr[:], rd=['fr'], wr=['fri'])
                B.cp('dve', frf[:], fri[:], rd=['fri'], wr=['frf'])
                B.tt('dve', fr[:], fr[:], frf[:], ALU.subtract, rd=['fr', 'frf'], wr=['fr'])
                B.tsc('dve', msk[:], fr[:], 0.5, None, ALU.is_gt, rd=['fr'], wr=['msk'])
                B.tt('dve', fr[:], fr[:], msk[:], ALU.subtract, rd=['fr', 'msk'], wr=['fr'])
                B.tsc('dve', msk[:], fr[:], -0.5, None, ALU.is_lt, rd=['fr'], wr=['msk'])
                B.tt('dve', fr[:], fr[:], msk[:], ALU.add, rd=['fr', 'msk'], wr=['fr'])
                B.act(tab[:], fr[:], AF.Sin, rd=['fr'], wr=['tab'], scale=2.0 * np.pi)
            B.barrier()

        with ExitStack() as st:
            gbc = B.sb(st, "gbc", [128, D], F32)
            bbc = B.sb(st, "bbc", [128, D], F32)
            B.dma('sp', gbc[:], emb_g.partition_broadcast(128), wr=['gbc'])
            B.dma('sp', bbc[:], emb_b.partition_broadcast(128), wr=['bbc'])
            xts = [B.sb(st, f"xt{j}", [128, D], F32) for j in range(2)]
            lnw = _ln_work(B, st)
            for i in range(NT):
                n = ts(i)
                xt = xts[i % 2]
                k = f"xt{i % 2}"
                if i == 0:
                    B.dma('sp', xt[0:NMETA, :], meta, wr=[k])
                    B.dma('sp', xt[NMETA:128, :], x[0:128 - NMETA, :], wr=[k])
                else:
                    B.dma('sp', xt[:n, :], x[128 * i - NMETA:128 * i - NMETA + n, :], wr=[k])
                _layer_norm(B, lnw, xt[:n, :], n, gbc, bbc, LN_EPS, k)
                B.dma('act', hD[128 * i:128 * i + n, :], xt[:n, :], rd=[k], wr=[f"hD{i}"])
            B.barrier()

        with ExitStack() as st:
            z = B.sb(st, "zpad", [128, INC], F32)
            B.memset('pool', z[:], 0.0, wr=['z'])
            B.dma('sp', projD[L:LP, :], z[0:LP - L, :], rd=['z'], wr=['projpad'])
            B.barrier()

        for li in range(nlayers):
            _mixer_attention(B, li, locals())
            if stop_after == f"att{li}":
                break
            _rwkv(B, li, locals())
            if stop_after in (f"rwkv{li}", f"rwkvC{li}"):
                break
            _outproj_ln1(B, li, locals())
            if stop_after == f"ln1_{li}":
                break
            if MOE_SPARSE:
                _moe_sparse_ln2(B, li, locals(), last=(li == nlayers - 1))
            else:
                _moe_ln2(B, li, locals(), last=(li == nlayers - 1))
            if stop_after == f"moe{li}":
                break

        if 'ymixD' in B.dbg or True:
            pass

    fin = S.op('sp', lambda e: e.nop())
    fin.deps |= set(S.dmas_since_bar)
    stats = S.emit()
    return nc, stats


def _ln_work(B, st):
    return dict(stats=B.sb(st, "ln_st", [128, 2, 6], F32), mv=B.sb(st, "ln_mv", [128, 2], F32),
                rs=B.sb(st, "ln_rs", [128, 1], F32), nb=B.sb(st, "ln_nb", [128, 1], F32))


def _layer_norm(B, w, xap, n, gbc, bbc, eps, key):
    S = B.S
    stt_, mv, rs, nb = w['stats'], w['mv'], w['rs'], w['nb']
    for c in range(2):
        S.op('dve', (lambda c: lambda e: e.bn_stats(out=stt_[:n, c, :], in_=xap[:, c * 512:(c + 1) * 512]))(c),
             reads=[key], writes=['ln_st'])
    S.op('dve', lambda e: e.bn_aggr(out=mv[:n, :], in_=stt_[:n, :, :]), reads=['ln_st'], writes=['ln_mv'])
    B.act(rs[:n, :], mv[:n, 1:2], AF.Sqrt, rd=['ln_mv'], wr=['ln_rs'], bias=B.eps_ap(eps, n), scale=1.0)
    B.recip(rs[:n, :], rs[:n, :], rd=['ln_rs'], wr=['ln_rs'])
    B.tsc('dve', xap, xap, mv[:n, 0:1], rs[:n, 0:1], ALU.subtract, ALU.mult, rd=[key, 'ln_mv', 'ln_rs'], wr=[key])
    B.tt('pool', xap, xap, gbc[:n, :], ALU.mult, rd=[key, 'gbc'], wr=[key])
    B.tt('pool', xap, xap, bbc[:n, :], ALU.add, rd=[key, 'bbc'], wr=[key])


def _rope(B, eng, dst, src, cos_ap, sin_ap, tmp, n, shape, rd, wr, tkey):
    s1, s2 = src
    d1, d2 = dst
    ta, tb = tmp
    B.tt(eng, ta, s1, cos_ap, ALU.mult, rd=rd + ['tab'], wr=[tkey + 'a'])
    B.tt(eng, tb, s2, sin_ap, ALU.mult, rd=rd + ['tab'], wr=[tkey + 'b'])
    B.tt(eng, d1, ta, tb, ALU.subtract, rd=[tkey + 'a', tkey + 'b'], wr=wr)
    B.tt(eng, ta, s2, cos_ap, ALU.mult, rd=rd + ['tab', tkey + 'a'], wr=[tkey + 'a'])
    B.tt(eng, tb, s1, sin_ap, ALU.mult, rd=rd + ['tab', tkey + 'b'], wr=[tkey + 'b'])
    B.tt(eng, d2, ta, tb, ALU.add, rd=[tkey + 'a', tkey + 'b'], wr=wr)


def _mixer_attention(B, li, g):
    nc, S = B.nc, B.S
    hD, projD, ymixD = g['hD'], g['projD'], g['ymixD']
    ident, identb, cosT, sinT = g['ident'], g['identb'], g['cosT'], g['sinT']
    w_in, w_uq, w_ukv = g['w_in'], g['w_uq'], g['w_ukv']
    q_norm, kv_norm, out_norm = g['q_norm'], g['kv_norm'], g['out_norm']

    with ExitStack() as rs_:
        KnT = B.sb(rs_, "KnT", [128, HA, LP], BF16)
        KpT = B.sb(rs_, "KpT", [65, LP], BF16)
        Vaug = B.sb(rs_, "Vaug", [128, NT, HA, VD + 1], BF16)
        kmaxb = B.sb(rs_, "kmaxb", [128, 1], F32)
        with ExitStack() as st:
            win = B.sb(st, "win", [128, 8, INC], BF16)
            wukv = B.sb(st, "wukv", [128, HA, 2, 128], BF16)
            gkv = B.sb(st, "gkv", [128, KVL], F32)
            kmax = B.sb(st, "kmax", [128, HA], F32)
            hts = [B.sb(st, f"ht{j}", [128, D], F32) for j in range(2)]
            hTs = [B.sb(st, f"hT{j}", [128, 8, 128], BF16) for j in range(2)]
            pjs = [B.sb(st, f"pj{j}", [128, INC], F32) for j in range(2)]
            ckvn = B.sb(st, "ckvn", [128, KVL], BF16)
            ckvT = B.sb(st, "ckvT", [128, 128], BF16)
            ssq = B.sb(st, "ssq", [128, 1], F32)
            junk = B.sb(st, "junk", [128, 512], F32)
            kss = B.sb(st, "kss", [128, HA], F32)
            kpss = B.sb(st, "kpss", [128, 1], F32)
            kpe = B.sb(st, "kpe", [128, QKR], BF16)
            rta = B.sb(st, "rta", [128, 32], F32)
            rtb = B.sb(st, "rtb", [128, 32], F32)
            pT = [B.ps(st, f"pT{j}", [128, 4, 128], F32) for j in range(2)]
            pP = [B.ps(st, f"pP{j}", [128, 512], F32) for j in range(2)]
            pKV = B.ps(st, "pKV", [128, 2, 512], F32)
            pK = B.ps(st, "pK", [128, HA, 128], F32)
            pTb = B.ps(st, "pTb", [128, 2, 128], BF16)

            for c in range(8):
                B.dma('pool', win[:, c, :], w_in[li, c * 128:(c + 1) * 128, :], wr=['win'])
            B.dma('pool', wukv[:].rearrange("p h t d -> p (h t d)"), w_ukv[li], wr=['wukv'])
            B.dma('sp', gkv[:], kv_norm[li].partition_broadcast(128), wr=['gkv'])
            B.memset('pool', kmax[:], 0.0, wr=['kmax'])
            B.memset('pool', Vaug[:], 0.0, wr=['Vaug'])
            for i in range(NT):
                B.memset('pool', Vaug[:ts(i), i, :, VD:VD + 1], 1.0, wr=['Vaug'])
            B.memset('pool', KpT[64:65, :], 1.0, wr=['KpT'])

            a1_pend = []
            for i in range(NT):
                n = ts(i)
                j = i % 2
                ht, hT, pj = hts[j], hTs[j], pjs[j]
                kh, khT, kpj = f"ht{j}", f"hT{j}", f"pj{j}"
                r0 = 128 * i
                B.dma('sp', ht[:n, :], hD[r0:r0 + n, :], rd=[f"hD{i}"], wr=[kh])
                for f_ in a1_pend:
                    f_()
                del a1_pend[:]
                for half in range(2):
                    pt = pT[half]
                    for c in range(4):
                        B.tr(pt[:, c, :n], ht[:n, (half * 4 + c) * 128:(half * 4 + c + 1) * 128], ident[:n, :n],
                             rd=[kh, 'ident'], wr=[f"pT{half}"])
                    B.cp('act' if half == 0 else 'dve', hT[:, half * 4:half * 4 + 4, :n], pt[:, :, :n],
                         rd=[f"pT{half}"], wr=[khT])
                c0 = 0
                ci = 0
                while c0 < INC:
                    w = min(512, INC - c0)
                    pp = pP[ci % 2]
                    for k in range(8):
                        B.mm(pp[:n, :w], hT[:, k, :n], win[:, k, c0:c0 + w], start=(k == 0), stop=(k == 7),
                             rd=[khT, 'win'], wr=[f"pP{ci % 2}"])
                    B.cp('act' if ci % 2 == 0 else 'dve', pj[:n, c0:c0 + w], pp[:n, :w], rd=[f"pP{ci % 2}"], wr=[kpj])
                    c0 += w
                    ci += 1
                a1_pend.append(lambda i=i, n=n, r0=r0, pj=pj, kpj=kpj: B.dma(
                    'sp', projD[r0:r0 + n, :], pj[:n, :], rd=[kpj], wr=[f"projD{i}"]))
                ckv = pj[:n, QL:QL + KVL]
                B.act(junk[:n, :KVL], ckv, AF.Square, rd=[kpj], wr=['junk', 'ssq'], accum=ssq[:n, :])
                B.rsqrt_(ssq[:n, :], 1.0 / KVL, RMS_EPS, 'ssq')
                B.stt(ckvn[:n, :], ckv, ssq[:n, 0:1], gkv[:n, :], ALU.mult, ALU.mult, rd=[kpj, 'ssq', 'gkv'], wr=['ckvn'])
                B.tr(pTb[:, 0, :n], ckvn[:n, :], identb[:n, :n], rd=['ckvn', 'identb'], wr=['pTb0'])
                B.cp('act', ckvT[:, :n], pTb[:, 0, :n], rd=['pTb0'], wr=['ckvT'])
                for t_ in range(2):
                    B.mm(pKV[:n, t_, :].rearrange("p (h d) -> p h d", h=HA), ckvT[:, :n], wukv[:, :, t_, :],
                         rd=['ckvT', 'wukv'], wr=[f"pKV{t_}"])
                B.cp('dve', Vaug[:n, i, :, 0:VD], pKV[:n, 1, :].rearrange("p (h d) -> p h d", h=HA),
                     rd=['pKV1'], wr=['Vaug'])
                B.act(junk[:n, :], pKV[:n, 0, :], AF.Square, rd=['pKV0'], wr=['junk'])
                B.red(kss[:n, :], junk[:n, :].rearrange("p (h d) -> p h d", h=HA), ALU.add, rd=['junk'], wr=['kss'])
                kp = pj[:n, QL + KVL:QL + KVL + QKR]
                B.act(junk[:n, :QKR], kp, AF.Square, rd=[kpj, 'kss'], wr=['junk', 'kpss'], accum=kpss[:n, :])
                B.tsc('dve', kss[:n, :], kss[:n, :], kpss[:n, 0:1], None, ALU.add, rd=['kss', 'kpss'], wr=['kss'])
                B.tt('dve', kmax[:n, :], kmax[:n, :], kss[:n, :], ALU.max, rd=['kmax', 'kss'], wr=['kmax'])
                _rope(B, 'pool', (kpe[:n, 0:32], kpe[:n, 32:64]), (kp[:, 0:32], kp[:, 32:64]),
                      cosT[:n, i, :], sinT[:n, i, :], (rta[:n, :], rtb[:n, :]), n, None, [kpj], ['kpe'], 'rt')
                B.tr(pTb[0:QKR, 1, :n], kpe[:n, :], identb[:n, :n], rd=['kpe', 'identb'], wr=['pTb1'])
                B.cp('act', KpT[0:QKR, r0:r0 + n], pTb[0:QKR, 1, :n], rd=['pTb1'], wr=['KpT'])
                for h in range(HA):
                    B.mm(pK[:, h, :n], wukv[:, h, 0, :], ckvT[:, :n], rd=['wukv', 'ckvT'], wr=['pK'])
                B.cp('dve', KnT[:, :, r0:r0 + n], pK[:, :, :n], rd=['pK'], wr=['KnT'])
            for f_ in a1_pend:
                f_()
            del a1_pend[:]
            kmr = B.sb(st, "kmr", [128, 1], F32)
            kmT = B.sb(st, "kmT", [1, 128], F32)
            km1 = B.sb(st, "km1", [1, 1], F32)
            ones1 = B.sb(st, "ones1", [1, 128], F32)
            B.memset('pool', ones1[:], 1.0, wr=['ones1'])
            B.red(kmr[:], kmax[:], ALU.max, rd=['kmax'], wr=['kmr'])
            B.tr(pT[0][0:1, 0, :], kmr[:, 0:1], ident[:, :], rd=['kmr', 'ident'], wr=['pT0'])
            B.cp('act', kmT[:], pT[0][0:1, 0, :], rd=['pT0'], wr=['kmT'])
            B.red(km1[:], kmT[:], ALU.max, rd=['kmT'], wr=['km1'])
            B.mm(pT[1][:, 0, 0:1], ones1[:], km1[:], rd=['ones1', 'km1'], wr=['pT1'])
            B.cp('act', kmaxb[:], pT[1][:, 0, 0:1], rd=['pT1'], wr=['kmaxb'])
            B.barrier()

        with ExitStack() as rq_:
            QnT = B.sb(rq_, "QnT", [128, HA, LP], BF16)
            QpT = B.sb(rq_, "QpT", [65, HA, LP], BF16)
            with ExitStack() as st:
                wuq = B.sb(st, "wuq", [128, 2, HA * (QKN + QKR)], BF16)
                gq = B.sb(st, "gq", [128, QL], F32)
                cqs = [B.sb(st, f"cq{j}", [128, QL], F32) for j in range(2)]
                cqn = B.sb(st, "cqn", [128, QL], BF16)
                cqT = B.sb(st, "cqT", [128, 2, 128], BF16)
                qf = B.sb(st, "qf", [128, HA, QKN + QKR], F32)
                qb = B.sb(st, "qb", [128, HA, QKN], BF16)
                qpa = B.sb(st, "qpa", [128, HA, QKR + 1], BF16)
                ssq = B.sb(st, "ssq2", [128, 1], F32)
                qss = B.sb(st, "qss", [128, HA], F32)
                junk = B.sb(st, "junk2", [128, HA * (QKN + QKR)], F32)
                rta = B.sb(st, "rta2", [128, HA, 32], F32)
                rtb = B.sb(st, "rtb2", [128, HA, 32], F32)
                pTb = B.ps(st, "pTb2", [128, 2, 128], BF16)
                pQ = B.ps(st, "pQ", [128, 2, 512], F32)
                pQT = B.ps(st, "pQT", [128, HA, 128], BF16)
                pQP = B.ps(st, "pQP", [65, HA, 128], BF16)
                for c in range(2):
                    B.dma('pool', wuq[:, c, :], w_uq[li, c * 128:(c + 1) * 128, :], wr=['wuq'])
                B.dma('sp', gq[:], q_norm[li].partition_broadcast(128), wr=['gq'])
                for i in range(NT):
                    n = ts(i)
                    j = i % 2
                    cq, kcq = cqs[j], f"cq{j}"
                    r0 = 128 * i
                    B.dma('sp', cq[:n, :], projD[r0:r0 + n, 0:QL], rd=[f"projD{i}"], wr=[kcq])
                    B.act(junk[:n, :QL], cq[:n, :], AF.Square, rd=[kcq], wr=['junk', 'ssq'], accum=ssq[:n, :])
                    B.rsqrt_(ssq[:n, :], 1.0 / QL, RMS_EPS, 'ssq')
                    B.stt(cqn[:n, :], cq[:n, :], ssq[:n, 0:1], gq[:n, :], ALU.mult, ALU.mult, rd=[kcq, 'ssq', 'gq'], wr=['cqn'])
                    for c in range(2):
                        B.tr(pTb[:, c, :n], cqn[:n, c * 128:(c + 1) * 128], identb[:n, :n], rd=['cqn', 'identb'], wr=['pTb'])
                    B.cp('act', cqT[:, :, :n], pTb[:, :, :n], rd=['pTb'], wr=['cqT'])
                    for cc in range(2):
                        for k in range(2):
                            B.mm(pQ[:n, cc, 0:384], cqT[:, k, :n], wuq[:, k, cc * 384:(cc + 1) * 384],
                                 start=(k == 0), stop=(k == 1), rd=['cqT', 'wuq'], wr=[f"pQ{cc}"])
                    qff = qf[:].rearrange("p h d -> p (h d)")
                    B.cp('act', qff[:n, 0:384], pQ[:n, 0, 0:384], rd=['pQ0'], wr=['qf'])
                    B.cp('dve', qff[:n, 384:768], pQ[:n, 1, 0:384], rd=['pQ1'], wr=['qf'])
                    B.act(junk[:n, :], qff[:n, :], AF.Square, rd=['qf'], wr=['junk'])
                    B.red(qss[:n, :], junk[:n, :].rearrange("p (h d) -> p h d", h=HA), ALU.add, rd=['junk'], wr=['qss'])
                    B.tsc('dve', qss[:n, :], qss[:n, :], kmaxb[:n, 0:1], None, ALU.mult, rd=['qss', 'kmaxb'], wr=['qss'])
                    B.act(qss[:n, :], qss[:n, :], AF.Sqrt, rd=['qss'], wr=['qss'])
                    B.tsc('dve', qpa[:n, :, QKR:QKR + 1], qss[:n, :].unsqueeze(2), -1.0, None, ALU.mult, rd=['qss'], wr=['qpa'])
                    B.cp('pool', qb[:n, :, :], qf[:n, :, 0:QKN], rd=['qf'], wr=['qb'])
                    cosb = cosT[:n, i, :].unsqueeze(1).broadcast_to([n, HA, 32])
                    sinb = sinT[:n, i, :].unsqueeze(1).broadcast_to([n, HA, 32])
                    _rope(B, 'pool', (qpa[:n, :, 0:32], qpa[:n, :, 32:64]),
                          (qf[:n, :, QKN:QKN + 32], qf[:n, :, QKN + 32:QKN + 64]),
                          cosb, sinb, (rta[:n], rtb[:n]), n, None, ['qf'], ['qpa'], 'rq')
                    for h in range(HA):
                        B.tr(pQT[:, h, :n], qb[:n, h, :], identb[:n, :n], rd=['qb', 'identb'], wr=['pQT'])
                    B.cp('act', QnT[:, :, r0:r0 + n], pQT[:, :, :n], rd=['pQT'], wr=['QnT'])
                    for h in range(HA):
                        B.tr(pQP[:, h, :n], qpa[:n, h, :], identb[:n, :n], rd=['qpa', 'identb'], wr=['pQP'])
                    B.cp('dve', QpT[:, :, r0:r0 + n], pQP[:, :, :n], rd=['pQP'], wr=['QpT'])
                B.barrier()

            with ExitStack() as st:
                gout = B.sb(st, "gout", [128, HA * VD], F32)
                B.dma('sp', gout[:], out_norm[li].partition_broadcast(128), wr=['gout'])
                yatt = B.sb(st, "yatt", [128, NT, HA * VD], BF16)
                PTs = [B.sb(st, f"PT{j}", [128, 512], BF16) for j in range(4)]
                ob = B.sb(st, "ob", [128, VD], F32)
                rden = B.sb(st, "rden", [128, 1], F32)
                oss = B.sb(st, "oss", [128, 1], F32)
                junk = B.sb(st, "junk3", [128, VD], F32)
                pS = [B.ps(st, f"pS{j}", [128, 512], F32) for j in range(4)]
                pO = [B.ps(st, f"pO{j}", [128, VD + 1], F32) for j in range(4)]
                obuf = [B.sb(st, f"obuf{j}", [128, VD + 1], F32) for j in range(4)]
                it = 0
                pend = None

                def emit_pv(p):
                    (h_, kt_, nk_, nqt_, wq_, PT_, kP_) = p
                    for jq in range(nqt_):
                        nq = min(128, wq_ - 128 * jq)
                        B.mm(pO[jq][:nq, :], PT_[:nk_, 128 * jq:128 * jq + nq], Vaug[:nk_, kt_, h_, :],
                             start=(kt_ == 0), stop=(kt_ == NT - 1), rd=[kP_, 'Vaug'], wr=[f"pO{jq}"])

                for h in range(HA):
                    for c in range(9):
                        q0 = 512 * c
                        wq = min(512, L - q0)
                        nqt = (wq + 127) // 128
                        for kt in range(NT):
                            nk = ts(kt)
                            k0 = 128 * kt
                            ps_ = pS[it % 4]
                            pk = f"pS{it % 4}"
                            PT = PTs[it % 4]
                            kP = f"PT{it % 4}"
                            it += 1
                            B.mm(ps_[:nk, :wq], KnT[:, h, k0:k0 + nk], QnT[:, h, q0:q0 + wq], start=True, stop=False,
                                 rd=['KnT', 'QnT'], wr=[pk])
                            B.mm(ps_[:nk, :wq], KpT[:, k0:k0 + nk], QpT[:, h, q0:q0 + wq], start=False, stop=True,
                                 rd=['KpT', 'QpT'], wr=[pk])
                            B.act(PT[:nk, :wq], ps_[:nk, :wq], AF.Exp, rd=[pk], wr=[kP], scale=ATT_SCALE)
                            if pend is not None:
                                emit_pv(pend)
                            pend = (h, kt, nk, nqt, wq, PT, kP)
                        emit_pv(pend)
                        pend = None
                        for jq in range(nqt):
                            nq = min(128, wq - 128 * jq)
                            B.cp('dve', obuf[jq][:nq, :], pO[jq][:nq, :], rd=[f"pO{jq}"], wr=[f"obuf{jq}"])
                        for jq in range(nqt):
                            nq = min(128, wq - 128 * jq)
                            ti = (q0 + 128 * jq) // 128
                            po = obuf[jq]
                            ko = f"obuf{jq}"
                            B.recip(rden[:nq, :], po[:nq, VD:VD + 1], rd=[ko], wr=['rden'])
                            B.tsc('dve', ob[:nq, :], po[:nq, 0:VD], rden[:nq, 0:1], None, ALU.mult,
                                  rd=[ko, 'rden'], wr=['ob'])
                            B.act(junk[:nq, :], ob[:nq, :], AF.Square, rd=['ob'], wr=['junk', 'oss'], accum=oss[:nq, :])
                            B.rsqrt_(oss[:nq, :], 1.0 / VD, RMS_EPS, 'oss')
                            B.stt(yatt[:nq, ti, h * VD:(h + 1) * VD], ob[:nq, :], oss[:nq, 0:1],
                                  gout[:nq, h * VD:(h + 1) * VD], ALU.mult, ALU.mult, rd=['ob', 'oss', 'gout'], wr=['yatt'])
                B.dma('sp', ymixD[0:128 * (NT - 1), 0:HA * VD].rearrange("(t p) c -> p t c", p=128),
                      yatt[:, 0:NT - 1, :], rd=['yatt'], wr=['ymixA'])
                B.dma('sp', ymixD[128 * (NT - 1):L, 0:HA * VD], yatt[:ts(NT - 1), NT - 1, :], rd=['yatt'], wr=['ymixA'])
                B.barrier()


DEC_C = -float(np.exp(-0.5))


def _rwkv(B, li, g):
    nc, S = B.nc, B.S
    projD, ymixD = g['projD'], g['ymixD']
    ident, identb = g['ident'], g['identb']
    R = g['rw_scr']
    opsD, vbD, gCD, yD, bonusD, gD, vfD = R['opsD'], R['vbD'], R['gCD'], R['yD'], R['bonusD'], R['gD'], R['vfD']
    cst = g['rw_consts']
    P = g['rw_params']

    with ExitStack() as st:
        def bc(name, ap, width):
            t = B.sb(st, name, [128, width], F32)
            B.dma('sp', t[:], ap.partition_broadcast(128), wr=[name])
            return t
        mp = bc("mp", P['mu_prev'][li], RWC)
        mn = bc("mn", P['mu_next'][li], RWC)
        c0 = B.sb(st, "c0", [128, RWC], F32)
        B.tt('dve', c0[:], mp[:], mn[:], ALU.add, rd=['mp', 'mn'], wr=['c0'])
        B.tsc('dve', c0[:], c0[:], -1.0, 1.0, ALU.mult, ALU.add, rd=['c0'], wr=['c0'])
        w0 = [bc(f"w0_{d}", P['w0'][li, d], RW) for d in range(2)]
        a0 = [bc(f"a0_{d}", P['a0'][li, d], RW) for d in range(2)]
        kkb = bc("kkb", P['k_k'][li], RW)
        kab = bc("kab", P['k_a'][li], RW)
        omka = B.sb(st, "omka", [128, RW], F32)
        B.tsc('dve', omka[:], kab[:], -1.0, 1.0, ALU.mult, ALU.add, rd=['kab'], wr=['omka'])
        rkb = bc("rkb", P['r_k'][li].rearrange("h n -> (h n)"), RW)
        w2 = B.sb(st, "w2", [128, RW], BF16)
        a2 = B.sb(st, "a2", [128, RW], BF16)
        g2 = B.sb(st, "g2", [128, RW], BF16)
        B.dma('pool', w2[:], P['w2'][li].rearrange("d k n -> (d k) n"), wr=['w2'])
        B.dma('pool', a2[:], P['a2'][li].rearrange("d k n -> (d k) n"), wr=['a2'])
        B.dma('pool', g2[:], P['g2'][li], wr=['g2'])
        if li > 0:
            v0b = bc("v0b", P['v0'][li - 1], RW)
            v1 = B.sb(st, "v1", [128, 4, 32], BF16)
            v2 = B.sb(st, "v2", [32, RW], BF16)
            B.dma('pool', v1[:], P['v1'][li - 1].rearrange("(c p) n -> p c n", p=128), wr=['v1'])
            B.dma('pool', v2[:], P['v2'][li - 1], wr=['v2'])
        ctr = [B.sb(st, f"ctr{j}", [128, RWC], F32) for j in range(2)]
        prv = [B.sb(st, f"prv{j}", [128, RWC], F32) for j in range(2)]
        nxt = [B.sb(st, f"nxt{j}", [128, RWC], F32) for j in range(2)]
        def two(name, shape, dt):
            return [B.sb(st, f"{name}_{q}", shape, dt) for q in range(2)]
        u_2 = two("u", [128, RWC], F32)
        lin_2 = two("lin", [128, 3, 128], BF16)
        linT_2 = two("linT", [128, 3, 128], BF16)
        kk_2 = two("kk", [128, RW], F32)
        t1_2 = two("t1", [128, RW], F32)
        t2_2 = two("t2", [128, RW], F32)
        t3_2 = two("t3", [128, RW], F32)
        rr_2 = two("rr", [128, RW], F32)
        sg_2 = [two(f"sg{d}", [128, RW], F32) for d in range(2)]
        av_2 = two("av", [128, RW], F32)
        kd_2 = two("kd", [128, RW], F32)
        ex_2 = two("ex", [128, 3, RW], F32)
        s8_2 = two("s8", [128, HB], F32)
        rks_2 = two("rks", [128, HB], F32)
        stg = [[B.sb(st, f"stg{d}{j}", [128, 4, RW], BF16) for j in range(2)] for d in range(2)]
        vb = [B.sb(st, f"vb{j}", [128, RW], BF16) for j in range(2)]
        gst = [B.sb(st, f"gst{j}", [128, RW], F32) for j in range(2)]
        bon = [B.sb(st, f"bon{j}", [128, RW], F32) for j in range(2)]
        gcs = [B.sb(st, f"gcs{j}", [64, 32], F32) for j in range(2)]
        if li > 0:
            vf = [B.sb(st, f"vf{j}", [128, RW], F32) for j in range(2)]
            vbt_2 = two("vbt", [128, RW], BF16)
            vT_2 = two("vT", [128, 4, 128], BF16)
            vl_2 = two("vl", [128, 32], BF16)
            vlT_2 = two("vlT", [32, 128], BF16)
        pL = B.ps(st, "pL", [128, 1024], BF16)
        pW = [B.ps(st, f"pW{j}", [128, RW], F32) for j in range(2)]
        pC = [B.ps(st, f"pC{j}", [128, RW], F32) for j in range(2)]
        pL2 = B.ps(st, "pL2", [128, 1024], BF16)
        pG = B.ps(st, "pG", [64, 32], F32)
        pV = B.ps(st, "pV", [128, RW], F32)

        deferred = []

        def defer_store(fn):
            deferred.append((S.sfx, fn))

        def flush_stores():
            cur = S.sfx
            for (sf, fn) in deferred:
                S.sfx = sf
                fn()
            S.sfx = cur
            del deferred[:]

        B.barrier()
        for i in range(NT):
            n = ts(i)
            j = i % 2
            r0 = 128 * i
            kc, kp_, kn = f"ctr{j}", f"prv{j}", f"nxt{j}"
            S.sfx = f"@{j}"
            S.nosfx = ('pL', 'pL2', 'pW0', 'pW1', 'pC0', 'pC1', 'pG', 'pV')
            u, lin, linT, kk, t1, t2, t3, rr, av, kd, ex, s8, rks = (x_[j] for x_ in (
                u_2, lin_2, linT_2, kk_2, t1_2, t2_2, t3_2, rr_2, av_2, kd_2, ex_2, s8_2, rks_2))
            sg = [sg_2[0][j], sg_2[1][j]]
            if li > 0:
                vbt, vT, vl, vlT = vbt_2[j], vT_2[j], vl_2[j], vlT_2[j]
            if n < 128:
                B.memset('pool', ctr[j][:], 0.0, wr=[kc])
                B.memset('pool', prv[j][:], 0.0, wr=[kp_])
                B.memset('pool', nxt[j][:], 0.0, wr=[kn])
            B.dma('sp', ctr[j][:n, :], projD[r0:r0 + n, INC - RWC:INC], wr=[kc])
            if i == 0:
                B.memset('pool', prv[j][0:32, :], 0.0, wr=[kp_])
                B.dma('sp', prv[j][1:128, :], projD[0:127, INC - RWC:INC], wr=[kp_])
            else:
                B.dma('sp', prv[j][:n, :], projD[r0 - 1:r0 - 1 + n, INC - RWC:INC], wr=[kp_])
            B.dma('sp', nxt[j][:n, :], projD[r0 + 1:r0 + 1 + n, INC - RWC:INC], wr=[kn])
            flush_stores()
            B.tt('pool', u[:], ctr[j][:], c0[:], ALU.mult, rd=[kc, 'c0'], wr=['u'])
            B.tt('dve', prv[j][:], prv[j][:], mp[:], ALU.mult, rd=[kp_, 'mp'], wr=[kp_])
            B.tt('pool', u[:], u[:], prv[j][:], ALU.add, rd=['u', kp_], wr=['u'])
            B.tt('dve', nxt[j][:], nxt[j][:], mn[:], ALU.mult, rd=[kn, 'mn'], wr=[kn])
            B.tt('pool', u[:], u[:], nxt[j][:], ALU.add, rd=['u', kn], wr=['u'])
            r_ = u[:, 0:RW]
            k_ = u[:, RW:2 * RW]
            v_ = u[:, 2 * RW:3 * RW]
            B.act(lin[:, 0, :], u[:, 1536:1664], AF.Tanh, rd=['u'], wr=['lin'])
            B.cp('pool', lin[:, 1, :], u[:, 1664:1792], rd=['u'], wr=['lin'])
            B.act(lin[:, 2, :], u[:, 1792:1920], AF.Sigmoid, rd=['u'], wr=['lin'])
            for c in range(3):
                B.tr(pL[:, c * 128:(c + 1) * 128], lin[:, c, :], identb[:], rd=['lin', 'identb'], wr=['pL'])
            B.cp('act', linT[:].rearrange("p c n -> p (c n)"), pL[:, 0:384], rd=['pL'], wr=['linT'])
            if li > 0:
                B.dma('sp', vf[j][:], vfD[r0:r0 + 128, :], wr=[f"vf{j}"])
                B.cp('pool', vbt[:], v_, rd=['u'], wr=['vbt'])
                for c in range(4):
                    B.tr(pL2[:, c * 128:(c + 1) * 128], vbt[:, c * 128:(c + 1) * 128], identb[:],
                         rd=['vbt', 'identb'], wr=['pL2'])
                B.cp('dve', vT[:].rearrange("p c n -> p (c n)"), pL2[:, 0:512], rd=['pL2'], wr=['vT'])
                for c in range(4):
                    B.mm(pV[:, 0:32], vT[:, c, :], v1[:, c, :], start=(c == 0), stop=(c == 3), rd=['vT', 'v1'], wr=['pV'])
                B.cp('act', vl[:], pV[:, 0:32], rd=['pV'], wr=['vl'])
                B.tr(pL2[0:32, 512:640], vl[:], identb[:], rd=['vl', 'identb'], wr=['pL2'])
                B.cp('act', vlT[:], pL2[0:32, 512:640], rd=['pL2'], wr=['vlT'])
                B.mm(pV[:], vlT[:], v2[:], rd=['vlT', 'v2'], wr=['pV'])
                B.tt('dve', t1[:], pV[:], v0b[:], ALU.add, rd=['pV', 'v0b'], wr=['t1'])
                B.act(t1[:], t1[:], AF.Sigmoid, rd=['t1'], wr=['t1'])
                B.tt('pool', t2[:], vf[j][:], v_, ALU.subtract, rd=[f"vf{j}", 'u'], wr=['t2'])
                B.tt('dve', t2[:], t2[:], t1[:], ALU.mult, rd=['t1', 't2'], wr=['t2'])
                B.tt('pool', v_, v_, t2[:], ALU.add, rd=['t2', 'u'], wr=['u'])
            else:
                defer_store(lambda i=i, j=j, r0=r0, v_=v_, **kw_: B.dma('sp', vfD[r0:r0 + 128, :], v_, rd=['u'], wr=[f"vfD{i}"]))
            B.cp('pool', vb[j][:], v_, rd=['u'], wr=[f"vb{j}"])
            defer_store(lambda i=i, j=j, r0=r0, **kw_: B.dma('sp', vbD[r0:r0 + 128, :], vb[j][:], rd=[f"vb{j}"], wr=[f"vbD{i}"]))
            B.mm(pW[0][:], linT[:, 2, :], g2[:], rd=['linT', 'g2'], wr=['pW0'])
            B.cp('act', gst[j][:], pW[0][:], rd=['pW0'], wr=[f"gst{j}"])
            defer_store(lambda i=i, j=j, r0=r0, **kw_: B.dma('sp', gD[r0:r0 + 128, :], gst[j][:], rd=[f"gst{j}"], wr=[f"gD{i}"]))
            B.tt('dve', kk[:], k_, kkb[:], ALU.mult, rd=['u', 'kkb'], wr=['kk'])
            B.act(t1[:], kk[:], AF.Square, rd=['kk'], wr=['t1'])
            B.red(s8[:], t1[:].rearrange("p (h n) -> p h n", h=HB), ALU.add, rd=['t1'], wr=['s8'])
            B.act(s8[:], s8[:], AF.Sqrt, rd=['s8'], wr=['s8'], bias=B.eps_ap(1e-12, 128), scale=1.0)
            B.recip(s8[:], s8[:], rd=['s8'], wr=['s8'])
            B.tt('dve', kk[:].rearrange("p (h n) -> p h n", h=HB), kk[:].rearrange("p (h n) -> p h n", h=HB),
                 s8[:].unsqueeze(2).broadcast_to([128, HB, NB]), ALU.mult, rd=['kk', 's8'], wr=['kk'])
            B.tt('pool', rr[:], r_, rkb[:], ALU.mult, rd=['u', 'rkb'], wr=['rr'])
            for d in range(2):
                B.mm(pW[0][:], linT[64 * d:64 * d + 64, 0, :], w2[64 * d:64 * d + 64, :], rd=['linT', 'w2'], wr=['pW0'])
                B.tt('dve', sg[d][:], pW[0][:], w0[d][:], ALU.add, rd=['pW0', f"w0_{d}"], wr=[f"sg{d}"])
                B.act(sg[d][:], sg[d][:], AF.Sigmoid, rd=[f"sg{d}"], wr=[f"sg{d}"])
                B.mm(pW[1][:], linT[64 * d:64 * d + 64, 1, :], a2[64 * d:64 * d + 64, :], rd=['linT', 'a2'], wr=['pW1'])
                B.tt('dve', av[:], pW[1][:], a0[d][:], ALU.add, rd=['pW1', f"a0_{d}"], wr=['av'])
                B.act(av[:], av[:], AF.Sigmoid, rd=['av'], wr=['av'])
                B.tt('pool', t2[:], av[:], kab[:], ALU.mult, rd=['av', 'kab'], wr=['t2'])
                B.tt('pool', t2[:], t2[:], omka[:], ALU.add, rd=['t2', 'omka'], wr=['t2'])
                B.tt('pool', kd[:], t2[:], k_, ALU.mult, rd=['t2', 'u'], wr=['kd'])
                B.tt('dve', t3[:], rr[:], kd[:], ALU.mult, rd=['rr', 'kd'], wr=['t3'])
                if d == 0:
                    B.red(rks[:], t3[:].rearrange("p (h n) -> p h n", h=HB), ALU.add, rd=['t3'], wr=['rks'])
                else:
                    B.red(s8[:], t3[:].rearrange("p (h n) -> p h n", h=HB), ALU.add, rd=['t3'], wr=['s8'])
                    B.tt('dve', rks[:], rks[:], s8[:], ALU.add, rd=['rks', 's8'], wr=['rks'])
                tin, tex = cst['tri'][d]
                B.mm(pC[0][:], tin[:], sg[d][:], rd=['tri', f"sg{d}"], wr=['pC0'])
                B.mm(pC[1][:], tex[:], sg[d][:], rd=['tri', f"sg{d}"], wr=['pC1'])
                B.act(ex[:, 0, :], pC[1][:], AF.Exp, rd=['pC1'], wr=['ex0'])
                B.act(ex[:, 1, :], pC[0][:], AF.Exp, rd=['pC0'], wr=['ex1'], scale=-1.0)
                B.act(ex[:, 2, :], pC[0][:], AF.Exp, rd=['pC0'], wr=['ex2'])
                sd = stg[d][j]
                ks = f"stg{d}{j}"
                B.tt('dve', sd[:, 0, :], kk[:], ex[:, 0, :], ALU.mult, rd=['kk', 'ex0'], wr=[ks])
                B.tt('pool', sd[:, 1, :], kd[:], ex[:, 1, :], ALU.mult, rd=['kd', 'ex1'], wr=[ks])
                B.tt('pool', t2[:], kk[:], av[:], ALU.mult, rd=['kk', 'av', 't2'], wr=['t2'])
                B.stt(sd[:, 2, :], t2[:], -1.0, ex[:, 1, :], ALU.mult, ALU.mult, rd=['t2', 'ex1'], wr=[ks])
                B.tt('dve', sd[:, 3, :], r_, ex[:, 2, :], ALU.mult, rd=['u', 'ex2'], wr=[ks])
                defer_store(lambda i=i, d=d, r0=r0, sd=sd, ks=ks, **kw_: B.dma('sp', opsD[d][r0:r0 + 128, :, :], sd[:], rd=[ks], wr=[f"opsD{d}_{i}"]))
                for half in range(2):
                    for h in range(HB):
                        col = d * 16 + half * 8 + h
                        B.mm(pG[:, col:col + 1], sg[d][64 * half:64 * half + 64, h * NB:(h + 1) * NB],
                             cst['negc'][64 * half:64 * half + 64, :], rd=[f"sg{d}", 'negc'], wr=['pG'])
            B.act(gcs[j][:], pG[:], AF.Exp, rd=['pG'], wr=[f"gcs{j}"])
            defer_store(lambda i=i, j=j, r0=r0, **kw_: B.dma('sp', gCD[i], gcs[j][:], rd=[f"gcs{j}"], wr=[f"gCD{i}"]))
            B.tt('dve', bon[j][:].rearrange("p (h n) -> p h n", h=HB), v_.rearrange("p (h n) -> p h n", h=HB),
                 rks[:].unsqueeze(2).broadcast_to([128, HB, NB]), ALU.mult, rd=['u', 'rks'], wr=[f"bon{j}"])
            defer_store(lambda i=i, j=j, r0=r0, **kw_: B.dma('sp', bonusD[r0:r0 + 128, :], bon[j][:], rd=[f"bon{j}"], wr=[f"bonusD{i}"]))
        flush_stores()
        S.sfx = ''
        B.barrier()
    if g.get('stop_after') == f"rwkvC{li}":
        return

    with ExitStack() as st:
        HH = 4
        pTr = B.ps(st, "pTr", [64, HH, 128], BF16)
        dbl = [B.ps(st, f"db{j}", [64, 2, HH, NB], F32) for j in range(4)]
        sgl = [B.ps(st, f"sgl{j}", [64, HH, NB], F32) for j in range(3)]
        bstate = {'d': 0, 's': 0}

        def dbank():
            j = bstate['d'] % 4
            bstate['d'] += 1
            return dbl[j], f"db{j}"

        def sbank():
            j = bstate['s'] % 3
            bstate['s'] += 1
            return sgl[j], f"sgb{j}"

        Mst = [B.sb(st, f"Mst{d}", [64, HH, NB], F32) for d in range(4)]
        Mbf = [B.sb(st, f"Mbf{d}", [64, HH, NB], BF16) for d in range(4)]
        Mtmp = [B.sb(st, f"Mtmp{d}", [64, HH, NB], F32) for d in range(4)]
        for d in range(4):
            B.memset('pool', Mst[d][:], 0.0, wr=[f"Mst{d}"])
            B.memset('pool', Mbf[d][:], 0.0, wr=[f"Mbf{d}"])
        slots = {}
        for d in range(2):
            for par in range(2):
                sl = {}
                tag = f"{d}{par}"
                sl['tag'] = tag
                sl['ops'] = B.sb(st, "ops" + tag, [128, 4, HH * NB], BF16)
                sl['opc'] = B.sb(st, "opc" + tag, [64, 2, 4, HH * NB], BF16)
                sl['V'] = B.sb(st, "V" + tag, [64, 2, HH, NB], BF16)
                sl['gC'] = B.sb(st, "gC" + tag, [64, 32], F32)
                sl['FT'] = B.sb(st, "FT" + tag, [64, 4, HH, 128], BF16)
                sl['A'] = [B.sb(st, f"A{q}" + tag, [64, 2, HH, NB], BF16) for q in range(3)]
                sl['P'] = [B.sb(st, f"P{q}" + tag, [64, 2, HH, NB], BF16) for q in range(2)]
                sl['Q'] = [B.sb(st, f"Q{q}" + tag, [64, 2, HH, NB], BF16) for q in range(2)]
                sl['TT'] = B.sb(st, "TT" + tag, [64, 2, HH, NB], BF16)
                sl['WmT'] = B.sb(st, "WmT" + tag, [64, 2, HH, NB], BF16)
                sl['X0'] = B.sb(st, "X0" + tag, [64, 2, HH, NB], BF16)
                sl['U0'] = B.sb(st, "U0" + tag, [64, 2, HH, NB], F32)
                sl['U'] = B.sb(st, "U" + tag, [64, 2, HH, NB], BF16)
                sl['y'] = B.sb(st, "y" + tag, [64, 2, HH, NB], F32)
                slots[(d, par)] = sl
        msk = cst['masks']
        i64 = cst['i64']
        f3 = lambda t_: t_.rearrange("p c h n -> p (c h) n")
        bh = lambda t_: t_[0:64, :].unsqueeze(1).broadcast_to([64, 2 * HH, NB])

        def problem(d, i, par):
            sl = slots[(d, par)]
            tag = sl['tag']
            K = lambda s_: s_ + tag
            r0 = 128 * i
            ops, opc, V, gC, FT, TT, WmT, X0, U0, U, y = (sl[x_] for x_ in ('ops', 'opc', 'V', 'gC', 'FT', 'TT', 'WmT', 'X0', 'U0', 'U', 'y'))
            A = sl['A']
            strict = msk['lt'] if d == 0 else msk['gt']
            incl = msk['le'] if d == 0 else msk['ge']
            strictT = msk['gt'] if d == 0 else msk['lt']
            hc = slice(par * HH * NB, (par + 1) * HH * NB)
            B.dma('sp', ops[:], opsD[d][r0:r0 + 128, :, hc], wr=[K('ops')])
            for c_ in range(2):
                B.dma('act', opc[:, c_], opsD[d][r0 + 64 * c_:r0 + 64 * c_ + 64, :, hc], wr=[K('opc')])
            B.dma('sp', V[:].rearrange("p c h n -> p c (h n)"), vbD[r0:r0 + 128, hc].rearrange("(c p) n -> p c n", p=64), wr=[K('V')])
            B.dma('act', gC[:], gCD[i], wr=[K('gC')])
            yield
            for kind in range(4):
                for h in range(HH):
                    B.tr(pTr[:, h, :], ops[:, kind, h * NB:(h + 1) * NB], identb[:],
                         rd=[K('ops'), 'identb'], wr=['pTr'])
                B.cp('act' if kind % 2 == 0 else 'dve', FT[:, kind, :, :], pTr[:], rd=['pTr'], wr=[K(f"FT{kind}")])
            yield
            P0, Q0 = sl['P'][0], sl['Q'][0]
            specs = [
                (1, 0, A[0], strict, K('A0')),
                (1, 3, A[1], incl, K('A1')),
                (2, 3, A[2], incl, K('A2')),
                (2, 0, P0, strict, K('P0')),
                (0, 2, Q0, strictT, K('Q0')),
            ]
            for qi, (lk, rk, dst, m, dk) in enumerate(specs):
                bk, kb = dbank()
                for c in range(2):
                    cs_ = slice(64 * c, 64 * c + 64)
                    for h in range(HH):
                        B.mm(bk[:, c, h, :], FT[:, lk, h, cs_], FT[:, rk, h, cs_], rd=[K(f"FT{lk}"), K(f"FT{rk}")], wr=[kb])
                B.tt('dve', f3(dst[:]), f3(bk[:]), bh(m), ALU.mult, rd=[kb, 'masks'], wr=[dk])
                if qi % 2 == 1:
                    yield
            B.tt('pool', f3(TT[:]), f3(P0[:]), bh(i64), ALU.add, rd=[K('P0'), 'masks'], wr=[K('TT')])
            yield
            cur = 0
            for lev in range(5):
                Pc, Qc = sl['P'][cur], sl['Q'][cur]
                Pn, Qn = sl['P'][1 - cur], sl['Q'][1 - cur]
                kPc, kQc, kPn, kQn = K(f"P{cur}"), K(f"Q{cur}"), K(f"P{1 - cur}"), K(f"Q{1 - cur}")
                bq, kbq = dbank()
                for c in range(2):
                    for h in range(HH):
                        B.mm(bq[:, c, h, :], Pc[:, c, h, :], Qc[:, c, h, :], rd=[kPc, kQc], wr=[kbq])
                B.cp('act', Qn[:], bq[:], rd=[kbq], wr=[kQn])
                if lev < 4:
                    bp, kbp = dbank()
                    for c in range(2):
                        for h in range(HH):
                            B.mm(bp[:, c, h, :], Qc[:, c, h, :], Pc[:, c, h, :], rd=[kPc, kQc], wr=[kbp])
                    B.cp('dve', Pn[:], bp[:], rd=[kbp], wr=[kPn])
                yield
                bt, kbt = dbank()
                for c in range(2):
                    for h in range(HH):
                        B.mm(bt[:, c, h, :], Qn[:, c, h, :], TT[:, c, h, :], rd=[kQn, K('TT')], wr=[kbt])
                B.tt('dve', TT[:], TT[:], bt[:], ALU.add, rd=[kbt, K('TT')], wr=[K('TT')])
                cur = 1 - cur
                yield
            bw, kbw = dbank()
            for c in range(2):
                for h in range(HH):
                    B.mm(bw[:, c, h, :], opc[:, c, 0, h * NB:(h + 1) * NB], TT[:, c, h, :], rd=[K('opc'), K('TT')], wr=[kbw])
            B.cp('act', WmT[:], bw[:], rd=[kbw], wr=[K('WmT')])
            bx, kbx = dbank()
            for c in range(2):
                for h in range(HH):
                    B.mm(bx[:, c, h, :], A[0][:, c, h, :], V[:, c, h, :], rd=[K('A0'), K('V')], wr=[kbx])
            B.cp('dve', X0[:], bx[:], rd=[kbx], wr=[K('X0')])
            yield
            bu, kbu = dbank()
            for c in range(2):
                for h in range(HH):
                    B.mm(bu[:, c, h, :], TT[:, c, h, :], X0[:, c, h, :], rd=[K('TT'), K('X0')], wr=[kbu])
            B.cp('act', U0[:], bu[:], rd=[kbu], wr=[K('U0')])
            yield
            dm = 2 * d + par
            kM, kMb, kMt = f"Mst{dm}", f"Mbf{dm}", f"Mtmp{dm}"
            for c in ((0, 1) if d == 0 else (1, 0)):
                cs_ = slice(64 * c, 64 * c + 64)
                bU, kbU = sbank()
                for h in range(HH):
                    B.mm(bU[:, h, :], WmT[:, c, h, :], Mbf[dm][:, h, :], rd=[K('WmT'), kMb], wr=[kbU])
                B.tt('dve', U[:, c], bU[:], U0[:, c], ALU.add, rd=[kbU, K('U0')], wr=[K('U')])
                bM, kbM = sbank()
                for h in range(HH):
                    B.mm(bM[:, h, :], opc[:, c, 1, h * NB:(h + 1) * NB], V[:, c, h, :], start=True, stop=False,
                         rd=[K('opc'), K('V')], wr=[kbM])
                    B.mm(bM[:, h, :], opc[:, c, 2, h * NB:(h + 1) * NB], U[:, c, h, :], start=False, stop=True,
                         rd=[K('opc'), K('U')], wr=[kbM])
                bY, kbY = sbank()
                for h in range(HH):
                    B.mm(bY[:, h, :], FT[:, 3, h, cs_], Mbf[dm][:, h, :], start=True, stop=False,
                         rd=[K('FT3'), kMb], wr=[kbY])
                    B.mm(bY[:, h, :], A[1][:, c, h, :], V[:, c, h, :], start=False, stop=False,
                         rd=[K('A1'), K('V')], wr=[kbY])
                    B.mm(bY[:, h, :], A[2][:, c, h, :], U[:, c, h, :], start=False, stop=True,
                         rd=[K('A2'), K('U')], wr=[kbY])
                B.cp('act', y[:, c], bY[:], rd=[kbY], wr=[K('y')])
                col = d * 16 + c * 8 + par * HH
                B.tt('dve', Mtmp[dm][:], Mst[dm][:], bM[:], ALU.add, rd=[kM, kbM], wr=[kMt])
                B.tt('pool', Mst[dm][:], Mtmp[dm][:], gC[:, col:col + HH].unsqueeze(2).broadcast_to([64, HH, NB]),
                     ALU.mult, rd=[kMt, K('gC')], wr=[kM])
                B.cp('act', Mbf[dm][:], Mst[dm][:], rd=[kM], wr=[kMb])
                yield
            B.dma('sp', yD[d][r0:r0 + 128, hc].rearrange("(c p) n -> p c n", p=64), y[:].rearrange("p c h n -> p c (h n)"),
                  rd=[K('y')], wr=[f"yD{d}_{i}_{par}"])
            yield

        FPm = g['ff_params']
        Mm = g['moe_scr']
        cw13f = B.sb(st, "cw13f", [128, 8, 2 * EH], F32)
        cw2f = B.sb(st, "cw2f", [128, 2, D], F32)
        cw13b = B.sb(st, "cw13b", [128, 8, 2 * EH], BF16)
        cw2b = B.sb(st, "cw2b", [128, 2, D], BF16)

        def conv_gen():
            for e in range(NE):
                B.dma('sp', cw13f[:, :, 0:EH], FPm['w1'][li, e].rearrange("(c p) n -> p c n", p=128), wr=['cw13f'])
                B.dma('sp', cw13f[:, :, EH:2 * EH], FPm['w3'][li, e].rearrange("(c p) n -> p c n", p=128), wr=['cw13f'])
                B.dma('sp', cw2f[:], FPm['w2'][li, e].rearrange("(c p) n -> p c n", p=128), wr=['cw2f'])
                B.cp('pool', cw13b[:], cw13f[:], rd=['cw13f'], wr=['cw13b'])
                B.cp('pool', cw2b[:], cw2f[:], rd=['cw2f'], wr=['cw2b'])
                B.dma('sp', Mm['W13D'][e * 128:(e + 1) * 128, :], cw13b[:].rearrange("p c n -> p (c n)"), rd=['cw13b'], wr=[f"W13D{e}"])
                B.dma('sp', Mm['W2D'][e * 128:(e + 1) * 128, :], cw2b[:].rearrange("p c n -> p (c n)"), rd=['cw2b'], wr=[f"W2D{e}"])
                yield

        conv = conv_gen()
        for step in range(NT):
            try:
                next(conv)
            except StopIteration:
                pass
            gens = [problem(0, step, 0), problem(1, NT - 1 - step, 0), problem(0, step, 1), problem(1, NT - 1 - step, 1)]
            alive = list(gens)
            while alive:
                for gq in list(alive):
                    try:
                        next(gq)
                    except StopIteration:
                        alive.remove(gq)
        B.barrier()

    with ExitStack() as st:
        def bc(name, ap, width):
            t = B.sb(st, name, [128, width], F32)
            B.dma('sp', t[:], ap.partition_broadcast(128), wr=[name])
            return t
        gng = bc("gng", P['gn_g'][li], RW)
        gnb = bc("gnb", P['gn_b'][li], RW)
        yf = [B.sb(st, f"yf{j}", [128, RW], F32) for j in range(2)]
        yb = [B.sb(st, f"yb{j}", [128, RW], F32) for j in range(2)]
        bo = [B.sb(st, f"bo{j}", [128, RW], F32) for j in range(2)]
        gg = [B.sb(st, f"gg{j}", [128, RW], F32) for j in range(2)]
        sq = B.sb(st, "sq", [128, RW], F32)
        s1 = B.sb(st, "s1", [128, HB], F32)
        s2 = B.sb(st, "s2", [128, HB], F32)
        m2 = B.sb(st, "m2", [128, HB], F32)
        yo = [B.sb(st, f"yo{j}", [128, RW], BF16) for j in range(2)]
        hv = lambda t_: t_.rearrange("p (h n) -> p h n", h=HB)
        b8 = lambda t_: t_.unsqueeze(2).broadcast_to([128, HB, NB])
        for i in range(NT):
            n = ts(i)
            j = i % 2
            r0 = 128 * i
            B.dma('sp', yf[j][:], yD[0][r0:r0 + 128, :], wr=[f"yf{j}"])
            B.dma('act', yb[j][:], yD[1][r0:r0 + 128, :], wr=[f"yb{j}"])
            B.dma('sp', bo[j][:], bonusD[r0:r0 + 128, :], wr=[f"bo{j}"])
            B.dma('act', gg[j][:], gD[r0:r0 + 128, :], wr=[f"gg{j}"])
            ky = f"yf{j}"
            B.tt('pool', yf[j][:], yf[j][:], yb[j][:], ALU.add, rd=[ky, f"yb{j}"], wr=[ky])
            B.red(s1[:], hv(yf[j][:]), ALU.add, rd=[ky], wr=['s1'])
            B.act(sq[:], yf[j][:], AF.Square, rd=[ky], wr=['sq'])
            B.red(s2[:], hv(sq[:]), ALU.add, rd=['sq'], wr=['s2'])
            B.tsc('dve', s1[:], s1[:], 1.0 / NB, None, ALU.mult, rd=['s1'], wr=['s1'])
            B.tt('dve', m2[:], s1[:], s1[:], ALU.mult, rd=['s1'], wr=['m2'])
            B.stt(s2[:], s2[:], 1.0 / NB, m2[:], ALU.mult, ALU.subtract, rd=['s2', 'm2'], wr=['s2'])
            B.act(s2[:], s2[:], AF.Sqrt, rd=['s2'], wr=['s2'], bias=B.eps_ap(GN_EPS, 128), scale=1.0)
            B.recip(s2[:], s2[:], rd=['s2'], wr=['s2'])
            B.tt('dve', hv(yf[j][:]), hv(yf[j][:]), b8(s1[:]), ALU.subtract, rd=[ky, 's1'], wr=[ky])
            B.tt('dve', hv(yf[j][:]), hv(yf[j][:]), b8(s2[:]), ALU.mult, rd=[ky, 's2'], wr=[ky])
            B.tt('pool', yf[j][:], yf[j][:], gng[:], ALU.mult, rd=[ky, 'gng'], wr=[ky])
            B.tt('pool', yf[j][:], yf[j][:], gnb[:], ALU.add, rd=[ky, 'gnb'], wr=[ky])
            B.tt('pool', yf[j][:], yf[j][:], bo[j][:], ALU.add, rd=[ky, f"bo{j}"], wr=[ky])
            B.tt('dve', yo[j][:], yf[j][:], gg[j][:], ALU.mult, rd=[ky, f"gg{j}"], wr=[f"yo{j}"])
            B.dma('sp', ymixD[r0:r0 + n, RW:2 * RW], yo[j][:n, :], rd=[f"yo{j}"], wr=[f"ymixB{i}"])
        B.barrier()


def _outproj_ln1(B, li, g):
    S = B.S
    hD, ymixD, identb = g['hD'], g['ymixD'], g['identb']
    FP = g['ff_params']
    with ExitStack() as st:
        wout = B.sb(st, "wout", [128, 8, D], BF16)
        for c in range(8):
            B.dma('pool', wout[:, c, :], FP['w_out'][li, c * 128:(c + 1) * 128, :], wr=['wout'])
        gbc = B.sb(st, "g1bc", [128, D], F32)
        bbc = B.sb(st, "b1bc", [128, D], F32)
        B.dma('sp', gbc[:], FP['ln1_g'][li].partition_broadcast(128), wr=['gbc'])
        B.dma('sp', bbc[:], FP['ln1_b'][li].partition_broadcast(128), wr=['bbc'])
        yms = [B.sb(st, f"ym{j}", [128, D], BF16) for j in range(2)]
        hts = [B.sb(st, f"hE{j}", [128, D], F32) for j in range(2)]
        ymT = B.sb(st, "ymT", [128, 8, 128], BF16)
        pTb = B.ps(st, "pTbE", [128, 8, 128], BF16)
        pO = [B.ps(st, f"pOE{j}", [128, 512], F32) for j in range(2)]
        lnw = _ln_work(B, st)
        for i in range(NT):
            n = ts(i)
            j = i % 2
            r0 = 128 * i
            ym, ht = yms[j], hts[j]
            kym, kh = f"ym{j}", f"hE{j}"
            B.dma('sp', ym[:n, :], ymixD[r0:r0 + n, :], wr=[kym])
            B.dma('act', ht[:n, :], hD[r0:r0 + n, :], wr=[kh])
            for c in range(8):
                B.tr(pTb[:, c, :n], ym[:n, c * 128:(c + 1) * 128], identb[:n, :n], rd=[kym, 'identb'], wr=['pTbE'])
            B.cp('act', ymT[:, :, :n], pTb[:, :, :n], rd=['pTbE'], wr=['ymT'])
            for half in range(2):
                for k in range(8):
                    B.mm(pO[half][:n, :], ymT[:, k, :n], wout[:, k, half * 512:(half + 1) * 512],
                         start=(k == 0), stop=(k == 7), rd=['ymT', 'wout'], wr=[f"pOE{half}"])
                B.stt(ht[:n, half * 512:(half + 1) * 512], ht[:n, half * 512:(half + 1) * 512], float(DN_ALPHA),
                      pO[half][:n, :], ALU.mult, ALU.add, rd=[kh, f"pOE{half}"], wr=[kh])
            _layer_norm(B, lnw, ht[:n, :], n, gbc, bbc, LN_EPS, kh)
            B.dma('sp', hD[r0:r0 + n, :], ht[:n, :], rd=[kh], wr=[f"hD{i}"])
        B.barrier()


TBLK = 11
MOE_SPARSE = True


def _moe_ln2(B, li, g, last):
    S = B.S
    hD, ident = g['hD'], g['ident']
    identb = g['identb']
    out = g['out']
    FP = g['ff_params']
    with ExitStack() as st:
        gbc = B.sb(st, "g2bc", [128, D], F32)
        bbc = B.sb(st, "b2bc", [128, D], F32)
        B.dma('sp', gbc[:], FP['ln2_g'][li].partition_broadcast(128), wr=['gbc'])
        B.dma('sp', bbc[:], FP['ln2_b'][li].partition_broadcast(128), wr=['bbc'])
        wr32 = B.sb(st, "wr32", [128, 8, 36], F32)
        for c in range(8):
            B.dma('sp', wr32[:, c, :], FP['wr'][li, c * 128:(c + 1) * 128, :], wr=['wr32'])
        brc = B.sb(st, "brc", [128, 36], F32)
        B.dma('sp', brc[:], FP['br'][li].partition_broadcast(128), wr=['brc'])
        acc = B.sb(st, "acc", [128, TBLK, D], F32)
        hTb = B.sb(st, "hTb", [128, TBLK, 8, 128], BF16)
        gate = B.sb(st, "gate", [128, TBLK, NE], F32)
        hT32 = B.sb(st, "hT32", [128, 8, 128], F32)
        hts = [B.sb(st, f"hF{j}", [128, D], F32) for j in range(2)]
        w13 = [B.sb(st, f"w13_{j}", [128, 8, 2 * EH], BF16) for j in range(2)]
        w2b = [B.sb(st, f"w2b_{j}", [128, 2, D], BF16) for j in range(2)]
        w13f = [B.sb(st, f"w13f_{j}", [128, 8, 2 * EH], F32) for j in range(2)]
        w2f = [B.sb(st, f"w2f_{j}", [128, 2, D], F32) for j in range(2)]
        lg = B.sb(st, "lg", [128, 36], F32)
        gex = B.sb(st, "gex", [128, 4], F32)
        gmk = B.sb(st, "gmk", [128, 4], F32)
        eex = B.sb(st, "eex", [128, NE], F32)
        sel = B.sb(st, "sel", [128, NE], F32)
        top8 = B.sb(st, "top8", [128, 8], F32)
        sc = {k_: B.sb(st, "sc_" + k_, [128, 1], F32) for k_ in ('gmax', 'gsum', 'emax', 'esum', 'coef')}
        sa = B.sb(st, "sa", [128, EH], F32)
        hid = B.sb(st, "hid", [128, EH], BF16)
        hidT = B.sb(st, "hidT", [128, 2, 128], BF16)
        bk = [B.ps(st, f"bkF{j}", [128, 512], F32) for j in range(7)]
        pTh = B.ps(st, "pTh", [128, 2, 128], BF16)
        lnw = _ln_work(B, st)
        wcount = 0
        for blk in range(NT // TBLK):
            tiles = list(range(blk * TBLK, (blk + 1) * TBLK))
            for tl, i in enumerate(tiles):
                n = ts(i)
                j = i % 2
                r0 = 128 * i
                ht, kh = hts[j], f"hF{j}"
                B.dma('sp', ht[:n, :], hD[r0:r0 + n, :], wr=[kh])
                for half in range(2):
                    pt = bk[half]
                    ptv = pt[:].rearrange("p (c n) -> p c n", c=4)
                    for c in range(4):
                        B.tr(ptv[:, c, :n], ht[:n, (half * 4 + c) * 128:(half * 4 + c + 1) * 128], ident[:n, :n],
                             rd=[kh, 'ident'], wr=[f"bkF{half}"])
                    B.cp('act', hT32[:, half * 4:half * 4 + 4, :n], ptv[:, :, :n], rd=[f"bkF{half}"], wr=['hT32'])
                    B.cp('pool', hTb[:, tl, half * 4:half * 4 + 4, :n], hT32[:, half * 4:half * 4 + 4, :n], rd=['hT32'], wr=['hTb'])
                for k in range(8):
                    B.mm(bk[2][:n, 0:36], hT32[:, k, :n], wr32[:, k, :], start=(k == 0), stop=(k == 7),
                         rd=['hT32', 'wr32'], wr=['bkF2'])
                B.tt('dve', lg[:n, :], bk[2][:n, 0:36], brc[:n, :], ALU.add, rd=['bkF2', 'brc'], wr=['lg'])
                B.red(sc['gmax'][:n, :], lg[:n, 0:4], ALU.max, rd=['lg'], wr=['gmax'])
                B.tsc('dve', gmk[:n, :], lg[:n, 0:4], sc['gmax'][:n, 0:1], None, ALU.is_ge, rd=['lg', 'gmax'], wr=['gmk'])
                B.tsc('dve', sc['gmax'][:n, :], sc['gmax'][:n, :], -1.0, None, ALU.mult, rd=['gmax', 'gmk'], wr=['gmax'])
                B.act(gex[:n, :], lg[:n, 0:4], AF.Exp, rd=['lg', 'gmax'], wr=['gex', 'gsum'], bias=sc['gmax'][:n, 0:1],
                      scale=1.0, accum=sc['gsum'][:n, :])
                B.tsc('dve', gmk[:n, :], gmk[:n, :], -1.0, 1e30, ALU.add, ALU.mult, rd=['gmk'], wr=['gmk'])
                lev = lg[:n, 4:36].rearrange("p (g e) -> p g e", g=NG)
                B.tt('dve', lev, lev, gmk[:n, :].unsqueeze(2).broadcast_to([n, NG, EPG]), ALU.add, rd=['lg', 'gmk'], wr=['lg'])
                B.red(sc['emax'][:n, :], lg[:n, 4:36], ALU.max, rd=['lg'], wr=['emax'])
                B.tsc('dve', sc['emax'][:n, :], sc['emax'][:n, :], -1.0, None, ALU.mult, rd=['emax'], wr=['emax'])
                B.act(eex[:n, :], lg[:n, 4:36], AF.Exp, rd=['lg', 'emax'], wr=['eex', 'esum'], bias=sc['emax'][:n, 0:1],
                      scale=1.0, accum=sc['esum'][:n, :])
                S.op('dve', (lambda n=n: lambda e: e.max(out=top8[:n, :], in_=eex[:n, :]))(), reads=['eex'], writes=['top8'])
                B.tsc('dve', sel[:n, :], eex[:n, :], top8[:n, 1:2], None, ALU.is_ge, rd=['eex', 'top8'], wr=['sel'])
                B.tt('dve', sc['coef'][:n, :], sc['gsum'][:n, :], sc['esum'][:n, :], ALU.mult, rd=['gsum', 'esum'], wr=['coef'])
                B.recip(sc['coef'][:n, :], sc['coef'][:n, :], rd=['coef'], wr=['coef'])
                B.stt(gate[:n, tl, :], eex[:n, :], sc['coef'][:n, 0:1], sel[:n, :], ALU.mult, ALU.mult,
                      rd=['eex', 'coef', 'sel'], wr=['gate'])
            for e in range(NE):
                wj = wcount % 2
                wcount += 1
                wa, wb = w13[wj], w2b[wj]
                kwa, kwb = f"w13_{wj}", f"w2b_{wj}"
                waf, wbf = w13f[wj], w2f[wj]
                kwaf, kwbf = f"w13f_{wj}", f"w2f_{wj}"
                B.dma('sp', waf[:, :, 0:EH], FP['w1'][li, e].rearrange("(c p) n -> p c n", p=128), wr=[kwaf])
                B.dma('sp', waf[:, :, EH:2 * EH], FP['w3'][li, e].rearrange("(c p) n -> p c n", p=128), wr=[kwaf])
                B.dma('sp', wbf[:], FP['w2'][li, e].rearrange("(c p) n -> p c n", p=128), wr=[kwbf])
                B.cp('pool', wa[:], waf[:], rd=[kwaf], wr=[kwa])
                B.cp('pool', wb[:], wbf[:], rd=[kwbf], wr=[kwb])
                for tl, i in enumerate(tiles):
                    n = ts(i)
                    pH, kpH = bk[tl % 2], f"bkF{tl % 2}"
                    for k in range(8):
                        B.mm(pH[:n, :], hTb[:, tl, k, :n], wa[:, k, :], start=(k == 0), stop=(k == 7),
                             rd=['hTb', kwa], wr=[kpH])
                    B.act(sa[:n, :], pH[:n, 0:EH], AF.Silu, rd=[kpH], wr=['sa'])
                    B.stt(hid[:n, :], pH[:n, EH:2 * EH], gate[:n, tl, e:e + 1], sa[:n, :], ALU.mult, ALU.mult,
                          rd=[kpH, 'gate', 'sa'], wr=['hid'])
                    for c in range(2):
                        B.tr(pTh[:, c, :n], hid[:n, c * 128:(c + 1) * 128], identb[:n, :n], rd=['hid', 'identb'], wr=['pTh'])
                    B.cp('act', hidT[:, :, :n], pTh[:, :, :n], rd=['pTh'], wr=['hidT'])
                    for half in range(2):
                        po, kpo = bk[3 + 2 * (tl % 2) + half], f"bkF{3 + 2 * (tl % 2) + half}"
                        for c in range(2):
                            B.mm(po[:n, :], hidT[:, c, :n], wb[:, c, half * 512:(half + 1) * 512],
                                 start=(c == 0), stop=(c == 1), rd=['hidT', kwb], wr=[kpo])
                        dst = acc[:n, tl, half * 512:(half + 1) * 512]
                        if e == 0:
                            B.cp('dve', dst, po[:n, :], rd=[kpo], wr=[f"acc{tl}"])
                        else:
                            B.tt('dve', dst, dst, po[:n, :], ALU.add, rd=[kpo, f"acc{tl}"], wr=[f"acc{tl}"])
            for tl, i in enumerate(tiles):
                n = ts(i)
                j = i % 2
                r0 = 128 * i
                ht, kh = hts[j], f"hF{j}"
                B.dma('sp', ht[:n, :], hD[r0:r0 + n, :], wr=[kh])
                B.stt(ht[:n, :], ht[:n, :], float(DN_ALPHA), acc[:n, tl, :], ALU.mult, ALU.add, rd=[kh, f"acc{tl}"], wr=[kh])
                _layer_norm(B, lnw, ht[:n, :], n, gbc, bbc, LN_EPS, kh)
                if not last:
                    B.dma('sp', hD[r0:r0 + n, :], ht[:n, :], rd=[kh], wr=[f"hD{i}"])
                else:
                    if i == 0:
                        o_ = B.dma('sp', out[0:128 - NMETA, :], ht[NMETA:128, :], rd=[kh], wr=[f"out{i}"])
                    else:
                        o_ = B.dma('sp', out[r0 - NMETA:r0 - NMETA + n, :], ht[:n, :], rd=[kh], wr=[f"out{i}"])
                    B.out_dmas.append(o_)
        B.barrier()


_IN_NAMES = ['meta_tokens', 'emb_ln_g', 'emb_ln_b', 'w_in', 'mla_q_norm', 'mla_kv_norm', 'mla_w_uq', 'mla_w_ukv',
             'mla_out_norm', 'rwkv_mu_prev', 'rwkv_mu_next', 'rwkv_w0', 'rwkv_w2', 'rwkv_a0', 'rwkv_a2', 'rwkv_g2',
             'rwkv_k_k', 'rwkv_k_a', 'rwkv_r_k', 'rwkv_gn_g', 'rwkv_gn_b', 'rwkv_v0', 'rwkv_v1', 'rwkv_v2', 'w_out',
             'ln1_g', 'ln1_b', 'moe_w1', 'moe_w3', 'moe_w2', 'ln2_g', 'ln2_b']


def kernel(**inputs):
    inputs = {k: np.asarray(v) for k, v in inputs.items()}
    nc, _ = build_program(nlayers=DEPTH)
    common = {k: np.ascontiguousarray(inputs[k], dtype=np.float32) for k in _IN_NAMES}
    common.update(host_consts())
    common.update(host_layout(inputs))
    nb = inputs['x'].shape[0]
    in_maps = []
    for b in range(nb):
        m = dict(common)
        m['x'] = np.ascontiguousarray(inputs['x'][b], dtype=np.float32)
        m['positions'] = np.ascontiguousarray(inputs['positions'][b].reshape(-1, 1).astype(np.int32))
        in_maps.append(m)
    res = run_bass_kernel_spmd(nc, in_maps, core_ids=list(range(nb)))
    return np.stack([np.asarray(res.results[b]['out'], dtype=np.float32) for b in range(nb)], axis=0)


NBLK = 96
NSLOT = NBLK * 128


def _moe_sparse_ln2(B, li, g, last):
    S = B.S
    nc = B.nc
    hD, ident, identb, out = g['hD'], g['ident'], g['identb'], g['out']
    FP = g['ff_params']
    M = g['moe_scr']
    W13D, W2D, XgD, YgD = M['W13D'], M['W2D'], M['XgD'], M['YgD']
    IOA = bass.IndirectOffsetOnAxis

    with ExitStack() as pst:
        S1 = B.sb(pst, "S1", [128, NT, NE], F32)
        S2 = B.sb(pst, "S2", [128, NT, NE], F32)
        PC = B.sb(pst, "PC", [128, NT, NE], F32)
        G = B.sb(pst, "Gt", [128, NT, 2], F32)
        SL = B.sb(pst, "SL", [128, NT * 2], I32)
        widx = B.sb(pst, "widx", [128, NBLK], I32)
        pstart = B.sb(pst, "pstart", [128, NE], F32)
        B.memset('pool', S1[:], 0.0, wr=['S1'])
        B.memset('pool', S2[:], 0.0, wr=['S2'])
        if not hasattr(B, 'breg'):
            def _mkreg(e):
                B.breg = e.to_reg(NSLOT - 1)
                return e.nop()
            S.op('pool', _mkreg)
            B.breg = None

        with ExitStack() as st:
            zt = B.sb(st, "zt", [128, 8 * D], BF16)
            B.memset('pool', zt[:], 0.0, wr=['zt'])
            for zb in range(NSLOT // 1024):
                B.dma('act', XgD[zb * 1024:(zb + 1) * 1024, :].rearrange("(p r) c -> p (r c)", p=128), zt[:],
                      rd=['zt'], wr=[f"XgZ{zb}"])
            wr32 = B.sb(st, "wr32", [128, 8, 36], F32)
            for c in range(8):
                B.dma('sp', wr32[:, c, :], FP['wr'][li, c * 128:(c + 1) * 128, :], wr=['wr32'])
            brc = B.sb(st, "brc", [128, 36], F32)
            B.dma('sp', brc[:], FP['br'][li].partition_broadcast(128), wr=['brc'])
            cm = B.sb(st, "cmoe", [128, 33 + NBLK], F32)
            B.dma('sp', cm[:], g['cmoe_in'].partition_broadcast(128), wr=['cm'])
            trix = B.sb(st, "trix", [128, 128], BF16)
            onesb = B.sb(st, "onesb", [128, 128], BF16)
            B.dma('pool', trix[:], g['trix_in'], wr=['trix'])
            B.memset('pool', onesb[:], 1.0, wr=['onesb'])
            carry = B.sb(st, "carry", [128, NE], F32)
            B.memset('pool', carry[:], 0.0, wr=['carry'])
            hT32 = B.sb(st, "hT32", [128, 8, 128], F32)
            hts = [B.sb(st, f"hF{j}", [128, D], F32) for j in range(2)]
            hbs = [B.sb(st, f"hB{j}", [128, D], BF16) for j in range(2)]
            lg = B.sb(st, "lg", [128, 36], F32)
            gex = B.sb(st, "gex", [128, 4], F32)
            gmk = B.sb(st, "gmk", [128, 4], F32)
            eex = B.sb(st, "eex", [128, NE], F32)
            selb = B.sb(st, "selb", [128, NE], BF16)
            selt = B.sb(st, "selt", [128, NE], F32)
            top8 = B.sb(st, "top8", [128, 8], F32)
            sc = {k_: B.sb(st, "sc_" + k_, [128, 1], F32) for k_ in ('gmax', 'gsum', 'emax', 'esum', 'coef')}
            bk = [B.ps(st, f"bkF{j}", [128, 512], F32) for j in range(4)]
            B.memset('pool', selb[:], 0.0, wr=['selb'])
            for i in range(NT):
                n = ts(i)
                j = i % 2
                r0 = 128 * i
                ht, kh = hts[j], f"hF{j}"
                B.dma('sp', ht[:n, :], hD[r0:r0 + n, :], wr=[kh])
                for half in range(2):
                    pt = bk[half]
                    ptv = pt[:].rearrange("p (c n) -> p c n", c=4)
                    for c in range(4):
                        B.tr(ptv[:, c, :n], ht[:n, (half * 4 + c) * 128:(half * 4 + c + 1) * 128], ident[:n, :n],
                             rd=[kh, 'ident'], wr=[f"bkF{half}"])
                    B.cp('act', hT32[:, half * 4:half * 4 + 4, :n], ptv[:, :, :n], rd=[f"bkF{half}"], wr=['hT32'])
                for k in range(8):
                    B.mm(bk[2][:n, 0:36], hT32[:, k, :n], wr32[:, k, :], start=(k == 0), stop=(k == 7),
                         rd=['hT32', 'wr32'], wr=['bkF2'])
                B.tt('dve', lg[:n, :], bk[2][:n, 0:36], brc[:n, :], ALU.add, rd=['bkF2', 'brc'], wr=['lg'])
                B.red(sc['gmax'][:n, :], lg[:n, 0:4], ALU.max, rd=['lg'], wr=['gmax'])
                B.tsc('dve', gmk[:n, :], lg[:n, 0:4], sc['gmax'][:n, 0:1], None, ALU.is_ge, rd=['lg', 'gmax'], wr=['gmk'])
                B.tsc('dve', sc['gmax'][:n, :], sc['gmax'][:n, :], -1.0, None, ALU.mult, rd=['gmax', 'gmk'], wr=['gmax'])
                B.act(gex[:n, :], lg[:n, 0:4], AF.Exp, rd=['lg', 'gmax'], wr=['gex', 'gsum'], bias=sc['gmax'][:n, 0:1],
                      scale=1.0, accum=sc['gsum'][:n, :])
                B.tsc('dve', gmk[:n, :], gmk[:n, :], -1.0, 1e30, ALU.add, ALU.mult, rd=['gmk'], wr=['gmk'])
                lev = lg[:n, 4:36].rearrange("p (g e) -> p g e", g=NG)
                B.tt('dve', lev, lev, gmk[:n, :].unsqueeze(2).broadcast_to([n, NG, EPG]), ALU.add, rd=['lg', 'gmk'], wr=['lg'])
                B.red(sc['emax'][:n, :], lg[:n, 4:36], ALU.max, rd=['lg'], wr=['emax'])
                B.tsc('dve', sc['emax'][:n, :], sc['emax'][:n, :], -1.0, None, ALU.mult, rd=['emax'], wr=['emax'])
                B.act(eex[:n, :], lg[:n, 4:36], AF.Exp, rd=['lg', 'emax'], wr=['eex', 'esum'], bias=sc['emax'][:n, 0:1],
                      scale=1.0, accum=sc['esum'][:n, :])
                S.op('dve', (lambda n=n: lambda e: e.max(out=top8[:n, :], in_=eex[:n, :]))(), reads=['eex'], writes=['top8'])
                B.tsc('dve', S1[:n, i, :], eex[:n, :], top8[:n, 0:1], None, ALU.is_ge, rd=['eex', 'top8'], wr=['S1'])
                B.tsc('dve', selt[:n, :], eex[:n, :], top8[:n, 1:2], None, ALU.is_ge, rd=['eex', 'top8'], wr=['selt'])
                B.tt('dve', S2[:n, i, :], selt[:n, :], S1[:n, i, :], ALU.subtract, rd=['selt', 'S1'], wr=['S2'])
                B.cp('dve', selb[:n, :], selt[:n, :], rd=['selt'], wr=['selb'])
                B.tt('dve', sc['coef'][:n, :], sc['gsum'][:n, :], sc['esum'][:n, :], ALU.mult, rd=['gsum', 'esum'], wr=['coef'])
                B.recip(sc['coef'][:n, :], sc['coef'][:n, :], rd=['coef'], wr=['coef'])
                B.tsc('dve', G[:n, i, :], top8[:n, 0:2], sc['coef'][:n, 0:1], None, ALU.mult, rd=['top8', 'coef'], wr=['G'])
                B.mm(bk[3][:, 0:NE], trix[:], selb[:], rd=['trix', 'selb'], wr=['bkF3'])
                B.tt('dve', PC[:, i, :], bk[3][:, 0:NE], carry[:], ALU.add, rd=['bkF3', 'carry'], wr=['PC'])
                B.mm(bk[3][:, 64:64 + NE], onesb[:], selb[:], rd=['onesb', 'selb', 'PC'], wr=['bkF3'])
                B.tt('dve', carry[:], carry[:], bk[3][:, 64:64 + NE], ALU.add, rd=['bkF3', 'carry'], wr=['carry'])
                B.cp('pool', hbs[j][:n, :], ht[:n, :], rd=[kh], wr=[f"hB{j}"])
                B.dma('act', g['hbD'][r0:r0 + n, :], hbs[j][:n, :], rd=[f"hB{j}"], wr=[f"hbD{i}"])

            cmp1 = B.sb(st, "cmp1", [128, NE, 33], F32)
            nblk = B.sb(st, "nblk", [128, NE], F32)
            pend = B.sb(st, "pend", [128, NE], F32)
            onesf = B.sb(st, "onesf", [128, NE], F32)
            cmp2 = B.sb(st, "cmp2", [128, NBLK, NE], F32)
            bke = B.sb(st, "bke", [128, NBLK], F32)
            pidx = B.sb(st, "pidx", [128, 1], I32)
            pidf = B.sb(st, "pidf", [128, 1], F32)
            B.memset('pool', onesf[:], 1.0, wr=['onesf'])
            B.tt('dve', cmp1[:], carry[:].unsqueeze(2).broadcast_to([128, NE, 33]),
                 cm[:, 0:33].unsqueeze(1).broadcast_to([128, NE, 33]), ALU.is_gt, rd=['carry', 'cm'], wr=['cmp1'])
            B.red(nblk[:], cmp1[:], ALU.add, rd=['cmp1'], wr=['nblk'])
            B.tsc('dve', nblk[:], nblk[:], 128.0, None, ALU.mult, rd=['nblk'], wr=['nblk'])
            S.op('dve', lambda e: e.tensor_tensor_scan(out=pend[:], data0=onesf[:], data1=nblk[:], initial=0.0,
                                                        op0=ALU.mult, op1=ALU.add), reads=['onesf', 'nblk'], writes=['pend'])
            B.tt('dve', pstart[:], pend[:], nblk[:], ALU.subtract, rd=['pend', 'nblk'], wr=['pstart'])
            B.tt('dve', cmp2[:], pend[:].unsqueeze(1).broadcast_to([128, NBLK, NE]),
                 cm[:, 33:33 + NBLK].unsqueeze(2).broadcast_to([128, NBLK, NE]), ALU.is_le, rd=['pend', 'cm'], wr=['cmp2'])
            B.red(bke[:], cmp2[:], ALU.add, rd=['cmp2'], wr=['bke'])
            B.tsc('dve', bke[:], bke[:], float(NE - 1), None, ALU.min, rd=['bke'], wr=['bke'])
            S.op('pool', lambda e: e.iota(pidx[:], pattern=[[0, 1]], base=0, channel_multiplier=1), writes=['pidx'])
            B.cp('dve', pidf[:], pidx[:], rd=['pidx'], wr=['pidf'])
            B.tsc('dve', bke[:], bke[:], 128.0, pidf[:, 0:1], ALU.mult, ALU.add, rd=['bke', 'pidf'], wr=['bke'])
            B.cp('dve', widx[:], bke[:], rd=['bke'], wr=['widx'])

            slf = B.sb(st, "slf", [128, NT * 2], F32)
            tmpa = B.sb(st, "tmpa", [128, NE], F32)
            tmpb = B.sb(st, "tmpb", [128, NE], F32)
            B.memset('pool', slf[:], 1.0e9, wr=['slf'])
            for i in range(NT):
                n = ts(i)
                B.tt('dve', tmpa[:n, :], PC[:n, i, :], pstart[:n, :], ALU.add, rd=['PC', 'pstart'], wr=['tmpa'])
                B.tt('dve', tmpb[:n, :], tmpa[:n, :], S1[:n, i, :], ALU.mult, rd=['tmpa', 'S1'], wr=['tmpb'])
                B.red(slf[:n, 2 * i:2 * i + 1], tmpb[:n, :], ALU.add, rd=['tmpb', 'slf'], wr=['slf'])
                B.tt('dve', tmpb[:n, :], tmpa[:n, :], S2[:n, i, :], ALU.mult, rd=['tmpa', 'S2', 'slf'], wr=['tmpb'])
                B.red(slf[:n, 2 * i + 1:2 * i + 2], tmpb[:n, :], ALU.add, rd=['tmpb', 'slf'], wr=['slf'])
            B.cp('dve', SL[:], slf[:], rd=['slf'], wr=['SL'])
            hb2 = [B.sb(st, f"hb2_{j}", [128, D], BF16) for j in range(2)]
            for i in range(NT):
                n = ts(i)
                j = i % 2
                r0 = 128 * i
                if n < 128:
                    B.memset('pool', hb2[j][:], 0.0, wr=[f"hb2_{j}"])
                B.dma('sp', hb2[j][:n, :], g['hbD'][r0:r0 + n, :], rd=[f"hbD{i}"], wr=[f"hb2_{j}"])
                for q in range(2):
                    S.op('pool', (lambda j=j, i=i, q=q: lambda e: e.indirect_dma_start(
                        out=XgD[:, :], out_offset=IOA(ap=SL[:, 2 * i + q:2 * i + q + 1], axis=0), in_=hb2[j][:, :], in_offset=None,
                        bounds_check=B.breg, oob_is_err=False))(), reads=[f"hb2_{j}", 'SL'] + [f"XgZ{zb}" for zb in range(NSLOT // 1024)], writes=[f"Xg{i}_{q}"], dma=True)
            B.barrier()

        with ExitStack() as st:
            w13 = [B.sb(st, f"bw13_{j}", [128, 8, 2 * EH], BF16) for j in range(2)]
            w2 = [B.sb(st, f"bw2_{j}", [128, 2, D], BF16) for j in range(2)]
            xb = [B.sb(st, f"xb{j}", [128, D], BF16) for j in range(2)]
            xT = B.sb(st, "xT", [128, 8, 128], BF16)
            sa = B.sb(st, "sa", [128, EH], F32)
            hid = B.sb(st, "hid", [128, EH], BF16)
            hidT = B.sb(st, "hidT", [128, 2, 128], BF16)
            yb = [B.sb(st, f"yb{j}", [128, D], F32) for j in range(2)]
            pX = B.ps(st, "pX", [128, 8, 128], BF16)
            pH = [B.ps(st, f"pH{j}", [128, 512], F32) for j in range(2)]
            pTh = B.ps(st, "pTh", [128, 2, 128], BF16)
            pO = [B.ps(st, f"pOm{j}", [128, 512], F32) for j in range(4)]
            for b in range(NBLK):
                j = b % 2
                S.op('pool', (lambda j=j, b=b: lambda e: e.indirect_dma_start(
                    out=w13[j][:].rearrange("p c n -> p (c n)"), out_offset=None, in_=W13D[:, :],
                    in_offset=IOA(ap=widx[:, b:b + 1], axis=0)))(), reads=['widx'], writes=[f"bw13_{j}"], dma=True)
                S.op('pool', (lambda j=j, b=b: lambda e: e.indirect_dma_start(
                    out=w2[j][:].rearrange("p c n -> p (c n)"), out_offset=None, in_=W2D[:, :],
                    in_offset=IOA(ap=widx[:, b:b + 1], axis=0)))(), reads=['widx'], writes=[f"bw2_{j}"], dma=True)
                B.dma('sp', xb[j][:], XgD[b * 128:(b + 1) * 128, :], wr=[f"xb{j}"])
                for c in range(8):
                    B.tr(pX[:, c, :], xb[j][:, c * 128:(c + 1) * 128], identb[:], rd=[f"xb{j}", 'identb'], wr=['pX'])
                B.cp('act', xT[:], pX[:], rd=['pX'], wr=['xT'])
                ph, kph = pH[j], f"pH{j}"
                for k in range(8):
                    B.mm(ph[:], xT[:, k, :], w13[j][:, k, :], start=(k == 0), stop=(k == 7), rd=['xT', f"bw13_{j}"], wr=[kph])
                B.act(sa[:], ph[:, 0:EH], AF.Silu, rd=[kph], wr=['sa'])
                B.tt('dve', hid[:], ph[:, EH:2 * EH], sa[:], ALU.mult, rd=[kph, 'sa'], wr=['hid'])
                for c in range(2):
                    B.tr(pTh[:, c, :], hid[:, c * 128:(c + 1) * 128], identb[:], rd=['hid', 'identb'], wr=['pTh'])
                B.cp('act', hidT[:], pTh[:], rd=['pTh'], wr=['hidT'])
                for half in range(2):
                    po, kpo = pO[2 * j + half], f"pOm{2 * j + half}"
                    for c in range(2):
                        B.mm(po[:], hidT[:, c, :], w2[j][:, c, half * 512:(half + 1) * 512], start=(c == 0), stop=(c == 1),
                             rd=['hidT', f"bw2_{j}"], wr=[kpo])
                    B.cp('dve' if half == 0 else 'act', yb[j][:, half * 512:(half + 1) * 512], po[:], rd=[kpo], wr=[f"yb{j}"])
                B.dma('sp', YgD[b * 128:(b + 1) * 128, :], yb[j][:], rd=[f"yb{j}"], wr=[f"YgD{b}"])
            B.barrier()

        with ExitStack() as st:
            gbc = B.sb(st, "g2bc", [128, D], F32)
            bbc = B.sb(st, "b2bc", [128, D], F32)
            B.dma('sp', gbc[:], FP['ln2_g'][li].partition_broadcast(128), wr=['gbc'])
            B.dma('sp', bbc[:], FP['ln2_b'][li].partition_broadcast(128), wr=['bbc'])
            hts = [B.sb(st, f"hG{j}", [128, D], F32) for j in range(2)]
            y1 = [B.sb(st, f"y1_{j}", [128, D], F32) for j in range(2)]
            y2 = [B.sb(st, f"y2_{j}", [128, D], F32) for j in range(2)]
            lnw = _ln_work(B, st)
            for i in range(NT):
                n = ts(i)
                j = i % 2
                r0 = 128 * i
                ht, kh = hts[j], f"hG{j}"
                B.dma('sp', ht[:n, :], hD[r0:r0 + n, :], wr=[kh])
                for q, yy in ((0, y1), (1, y2)):
                    S.op('pool', (lambda j=j, i=i, q=q, yy=yy: lambda e: e.indirect_dma_start(
                        out=yy[j][:, :], out_offset=None, in_=YgD[:, :], in_offset=IOA(ap=SL[:, 2 * i + q:2 * i + q + 1], axis=0),
                        bounds_check=B.breg, oob_is_err=False))(), reads=['SL'], writes=[f"y{q + 1}_{j}"], dma=True)
                B.tsc('dve', y1[j][:n, :], y1[j][:n, :], G[:n, i, 0:1], None, ALU.mult, rd=[f"y1_{j}", 'G'], wr=[f"y1_{j}"])
                B.stt(y1[j][:n, :], y2[j][:n, :], G[:n, i, 1:2], y1[j][:n, :], ALU.mult, ALU.add, rd=[f"y1_{j}", f"y2_{j}", 'G'], wr=[f"y1_{j}"])
                B.stt(ht[:n, :], ht[:n, :], float(DN_ALPHA), y1[j][:n, :], ALU.mult, ALU.add, rd=[kh, f"y1_{j}"], wr=[kh])
                _layer_norm(B, lnw, ht[:n, :], n, gbc, bbc, LN_EPS, kh)
                if not last:
                    B.dma('sp', hD[r0:r0 + n, :], ht[:n, :], rd=[kh], wr=[f"hD{i}"])
                else:
                    if i == 0:
                        o_ = B.dma('sp', out[0:128 - NMETA, :], ht[NMETA:128, :], rd=[kh], wr=[f"out{i}"])
                    else:
                        o_ = B.dma('sp', out[r0 - NMETA:r0 - NMETA + n, :], ht[:n, :], rd=[kh], wr=[f"out{i}"])
                    B.out_dmas.append(o_)
            B.barrier()
```

```python
from contextlib import ExitStack
import numpy as np
import concourse.bass as bass
import concourse.mybir as mybir
from concourse.bass_utils import run_bass_kernel_spmd

F32 = mybir.dt.float32
BF16 = mybir.dt.bfloat16
I32 = mybir.dt.int32
AF = mybir.ActivationFunctionType
ALU = mybir.AluOpType
AX = mybir.AxisListType

D = 1024
SEQ = 4096
DEPTH = 4
NMETA = 16
L = SEQ + NMETA
NT = 33
LP = NT * 128
HA = 4
QKN = 128
QKR = 64
VD = 128
QL = 256
KVL = 128
HB = 8
NB = 64
RW = 512
INC = 2368
RWC = 1920
NG = 4
EPG = 8
NE = 32
EH = 256
DN_ALPHA = (2 * DEPTH) ** 0.25
LN_EPS = 1e-5
RMS_EPS = 1e-6
GN_EPS = 64e-5
ATT_SCALE = (QKN + QKR) ** -0.5
CH = 64
NCH = 65


def ts(i):
    return 128 if i < NT - 1 else L - 128 * (NT - 1)


COMPUTE = ('pe', 'act', 'dve', 'pool')
N_DMA_SEMS = 32
N_SW_SEMS = 4
SEM_WRAP = 20000


class _Buf:
    __slots__ = ('w', 'r')

    def __init__(self):
        self.w = None
        self.r = []


class _Op:
    __slots__ = ('eng', 'fn', 'deps', 'is_dma', 'needed', 'tok', 'idx')


class Sched:
    def __init__(self, nc):
        self.nc = nc
        self.engs = {'pe': nc.tensor, 'act': nc.scalar, 'dve': nc.vector,
                     'pool': nc.gpsimd, 'sp': nc.sync}
        self.ops = []
        self.bufs = {}
        self.last = {}
        self.dmas_since_bar = []
        self.sfx = ''
        self.nosfx = ()

    def _b(self, name):
        b = self.bufs.get(name)
        if b is None:
            b = self.bufs[name] = _Buf()
        return b

    def op(self, eng, fn, reads=(), writes=(), dma=False):
        o = _Op()
        o.eng = eng
        o.fn = fn
        o.is_dma = dma
        o.needed = False
        o.tok = None
        o.idx = len(self.ops)
        deps = set()
        if self.sfx:
            reads = [x if x in self.nosfx else x + self.sfx for x in reads]
            writes = [x if x in self.nosfx else x + self.sfx for x in writes]
        rb = [self._b(x) for x in reads]
        wb = [self._b(x) for x in writes]
        for b in rb:
            if b.w is not None:
                deps.add(b.w)
        for b in wb:
            if b.w is not None:
                deps.add(b.w)
            deps.update(b.r)
        for b in rb:
            b.r.append(o.idx)
        for b in wb:
            b.w = o.idx
            b.r = []
        deps.discard(o.idx)
        o.deps = deps
        self.ops.append(o)
        if dma:
            self.dmas_since_bar.append(o.idx)
        else:
            self.last[eng] = o.idx
        return o

    def barrier(self):
        pend = set(self.dmas_since_bar) | set(self.last.values())
        self.dmas_since_bar = []
        for e in ('pe', 'act', 'dve', 'pool', 'sp'):
            o = self.op(e, lambda en: en.nop())
            o.deps |= {p for p in pend if p != o.idx}
        self.bufs = {}

    def emit(self):
        nc = self.nc
        ops = self.ops
        for o in ops:
            nd = set()
            best = {}
            for d in o.deps:
                p = ops[d]
                if p.is_dma:
                    nd.add(d)
                    continue
                if (not o.is_dma) and p.eng == 'pe' and o.eng == 'pe':
                    continue
                if best.get(p.eng, -1) < d:
                    best[p.eng] = d
            nd.update(best.values())
            for d in nd:
                ops[d].needed = True
            o.deps = nd
        csem = {e: nc.alloc_semaphore(name=f"c_{e}_0") for e in COMPUTE}
        csem['sp'] = nc.alloc_semaphore(name="c_sp_0")
        ccnt = {e: 0 for e in csem}
        cgen = {e: 0 for e in csem}
        dsems = [nc.alloc_semaphore(name=f"d_{i}") for i in range(N_DMA_SEMS + N_SW_SEMS)]
        dcnt = [0] * (N_DMA_SEMS + N_SW_SEMS)
        dnext = 0
        swnext = 0
        waited = {}
        n_wait = 0
        for o in ops:
            e = self.engs[o.eng]
            for d in sorted(o.deps):
                p = ops[d]
                sem, val = p.tok
                key = (o.eng, sem.num)
                if waited.get(key, 0) >= val:
                    continue
                e.wait_ge(sem, val)
                n_wait += 1
                waited[key] = val
            if o.is_dma:
                if o.eng == 'pool':
                    si = N_DMA_SEMS + swnext
                    swnext = (swnext + 1) % N_SW_SEMS
                else:
                    si = dnext
                    dnext = (dnext + 1) % N_DMA_SEMS
                sem = dsems[si]
                if dcnt[si] > 0:
                    key = (o.eng, sem.num)
                    if waited.get(key, 0) < dcnt[si]:
                        e.wait_ge(sem, dcnt[si])
                        waited[key] = dcnt[si]
                        n_wait += 1
                ins = o.fn(e)
                dcnt[si] += 16
                ins.then_inc(sem, 16)
                o.tok = (sem, dcnt[si])
            else:
                ins = o.fn(e)
                if o.needed:
                    if ccnt[o.eng] >= SEM_WRAP:
                        cgen[o.eng] += 1
                        csem[o.eng] = nc.alloc_semaphore(name=f"c_{o.eng}_{cgen[o.eng]}")
                        ccnt[o.eng] = 0
                    ccnt[o.eng] += 1
                    sem = csem[o.eng]
                    ins.then_inc(sem, 1)
                    o.tok = (sem, ccnt[o.eng])
        return dict(n_ops=len(ops), n_wait=n_wait, ccnt=dict(ccnt), cgen=dict(cgen), dcnt_max=max(dcnt))


class Builder:
    def __init__(self, nc, dbg=()):
        self.nc = nc
        self.S = Sched(nc)
        self.dbg = set(dbg)
        self.out_dmas = []
        self._uid = 0

    def sb(self, stack, name, shape, dt):
        self._uid += 1
        return stack.enter_context(self.nc.sbuf_tensor(f"{name}_u{self._uid}", list(shape), dt))

    def ps(self, stack, name, shape, dt=F32):
        self._uid += 1
        return stack.enter_context(self.nc.psum_tensor(f"{name}_u{self._uid}", list(shape), dt))

    def dram(self, name, shape, dt, kind=None):
        if kind is None:
            kind = "ExternalOutput" if name in self.dbg else "Internal"
        return self.nc.dram_tensor(name, list(shape), dt, kind=kind).ap()

    def mm(self, out, lhsT, rhs, start=True, stop=True, rd=(), wr=()):
        return self.S.op('pe', lambda e: e.matmul(out, lhsT=lhsT, rhs=rhs, start=start, stop=stop),
                         reads=rd, writes=wr)

    def tr(self, out, in_, ident, rd=(), wr=()):
        return self.S.op('pe', lambda e: e.transpose(out=out, in_=in_, identity=ident),
                         reads=rd, writes=wr)

    def act(self, out, in_, func, rd=(), wr=(), bias=None, scale=None, accum=None, eng='act'):
        kw = {}
        if bias is not None:
            kw['bias'] = bias
        if scale is not None:
            kw['scale'] = scale
        if accum is not None:
            kw['accum_out'] = accum
        return self.S.op(eng, lambda e: e.activation(out=out, in_=in_, func=func, **kw),
                         reads=rd, writes=wr)

    def tt(self, eng, out, in0, in1, op, rd=(), wr=()):
        return self.S.op(eng, lambda e: e.tensor_tensor(out=out, in0=in0, in1=in1, op=op),
                         reads=rd, writes=wr)

    def tsc(self, eng, out, in0, s1, s2, op0, op1=None, rd=(), wr=()):
        if op1 is None:
            return self.S.op(eng, lambda e: e.tensor_scalar(out=out, in0=in0, scalar1=s1, scalar2=None,
                                                            op0=op0), reads=rd, writes=wr)
        return self.S.op(eng, lambda e: e.tensor_scalar(out=out, in0=in0, scalar1=s1, scalar2=s2,
                                                        op0=op0, op1=op1), reads=rd, writes=wr)

    def stt(self, out, in0, scalar, in1, op0, op1, rd=(), wr=()):
        return self.S.op('dve', lambda e: e.scalar_tensor_tensor(out=out, in0=in0, scalar=scalar, in1=in1,
                                                                 op0=op0, op1=op1), reads=rd, writes=wr)

    def cp(self, eng, out, in_, rd=(), wr=()):
        if eng == 'act':
            return self.S.op('act', lambda e: e.copy(out=out, in_=in_), reads=rd, writes=wr)
        return self.S.op(eng, lambda e: e.tensor_copy(out=out, in_=in_), reads=rd, writes=wr)

    def recip(self, out, in_, rd=(), wr=()):
        return self.S.op('dve', lambda e: e.reciprocal(out=out, in_=in_), reads=rd, writes=wr)

    def red(self, out, in_, op, rd=(), wr=(), axis=AX.X):
        return self.S.op('dve', lambda e: e.tensor_reduce(out=out, in_=in_, axis=axis, op=op),
                         reads=rd, writes=wr)

    def memset(self, eng, ap, val, wr=()):
        return self.S.op(eng, lambda e: e.memset(ap, val), writes=wr)

    def dma(self, q, out, in_, rd=(), wr=()):
        return self.S.op(q, lambda e: e.dma_start(out=out, in_=in_), reads=rd, writes=wr, dma=True)

    def barrier(self):
        self.S.barrier()

    def rsqrt_(self, ap, mul, eps, key):
        self.act(ap, ap, AF.Sqrt, rd=[key], wr=[key], bias=self.eps_ap(eps, ap.shape[0]), scale=mul)
        self.recip(ap, ap, rd=[key], wr=[key])

    def eps_ap(self, eps, n):
        return self.epst[eps][:n, :]


def host_consts():
    c = {}
    c['c_ident'] = np.eye(128, dtype=np.float32)
    c['c_invf'] = (10000.0 ** (-np.arange(0, 64, 2, dtype=np.float32) / 64) / (2 * np.pi)).astype(np.float32)
    p = np.arange(128)
    same = (p[:, None] // 64) == (p[None, :] // 64)
    dc = -float(np.exp(-0.5))
    tri = np.zeros((4, 128, 128), np.float32)
    tri[0] = dc * (same & (p[:, None] <= p[None, :]))
    tri[1] = dc * (same & (p[:, None] < p[None, :]))
    tri[2] = dc * (same & (p[:, None] >= p[None, :]))
    tri[3] = dc * (same & (p[:, None] > p[None, :]))
    c['c_tri'] = tri
    r = (p % 64)[:, None]
    cc = np.arange(64)[None, :]
    c['c_masks'] = np.stack([r < cc, r <= cc, r > cc, r >= cc, r == cc]).astype(np.float32)
    c['c_moe'] = np.concatenate([128.0 * np.arange(33), 128.0 * np.arange(96)]).astype(np.float32)
    c['c_trix'] = (p[:, None] < p[None, :]).astype(np.float32)
    return c


def host_layout(inputs):
    m = {}
    m['moe_wr'] = np.ascontiguousarray(np.concatenate([inputs['moe_w_group'], inputs['moe_w_expert']], axis=-1))
    m['moe_br'] = np.ascontiguousarray(np.concatenate([inputs['moe_b_group'], inputs['moe_b_expert']], axis=-1))
    return m


def build_program(nlayers=DEPTH, dbg=(), stop_after=None):
    nc = bass.Bass("TRN2", target_bir_lowering=False)
    B = Builder(nc, dbg)
    S = B.S

    def inp(name, shape, dt=F32):
        return nc.dram_tensor(name, list(shape), dt, kind="ExternalInput").ap()

    x = inp("x", [SEQ, D])
    pos_in = inp("positions", [SEQ, 1], I32)
    meta = inp("meta_tokens", [NMETA, D])
    emb_g = inp("emb_ln_g", [D])
    emb_b = inp("emb_ln_b", [D])
    w_in = inp("w_in", [DEPTH, D, INC])
    q_norm = inp("mla_q_norm", [DEPTH, QL])
    kv_norm = inp("mla_kv_norm", [DEPTH, KVL])
    w_uq = inp("mla_w_uq", [DEPTH, QL, HA * (QKN + QKR)])
    w_ukv = inp("mla_w_ukv", [DEPTH, KVL, HA * (QKN + VD)])
    out_norm = inp("mla_out_norm", [DEPTH, HA * VD])
    ident_in = inp("c_ident", [128, 128])
    tri_in = inp("c_tri", [4, 128, 128])
    cmoe_in = inp("c_moe", [33 + 96])
    trix_in = inp("c_trix", [128, 128])
    masks_in = inp("c_masks", [5, 128, 64])
    ff_params = dict(
        w_out=inp("w_out", [DEPTH, D, D]), ln1_g=inp("ln1_g", [DEPTH, D]), ln1_b=inp("ln1_b", [DEPTH, D]),
        wr=inp("moe_wr", [DEPTH, D, NG + NE]), br=inp("moe_br", [DEPTH, NG + NE]),
        w1=inp("moe_w1", [DEPTH, NE, D, EH]), w3=inp("moe_w3", [DEPTH, NE, D, EH]), w2=inp("moe_w2", [DEPTH, NE, EH, D]),
        ln2_g=inp("ln2_g", [DEPTH, D]), ln2_b=inp("ln2_b", [DEPTH, D]))
    rw_params = dict(
        mu_prev=inp("rwkv_mu_prev", [DEPTH, RWC]), mu_next=inp("rwkv_mu_next", [DEPTH, RWC]),
        w0=inp("rwkv_w0", [DEPTH, 2, RW]), w2=inp("rwkv_w2", [DEPTH, 2, 64, RW]),
        a0=inp("rwkv_a0", [DEPTH, 2, RW]), a2=inp("rwkv_a2", [DEPTH, 2, 64, RW]),
        g2=inp("rwkv_g2", [DEPTH, 128, RW]), k_k=inp("rwkv_k_k", [DEPTH, RW]), k_a=inp("rwkv_k_a", [DEPTH, RW]),
        r_k=inp("rwkv_r_k", [DEPTH, HB, NB]), gn_g=inp("rwkv_gn_g", [DEPTH, RW]), gn_b=inp("rwkv_gn_b", [DEPTH, RW]),
        v0=inp("rwkv_v0", [DEPTH - 1, RW]), v1=inp("rwkv_v1", [DEPTH - 1, RW, 32]), v2=inp("rwkv_v2", [DEPTH - 1, 32, RW]))
    invf_in = inp("c_invf", [32])
    out = nc.dram_tensor("out", [SEQ, D], F32, kind="ExternalOutput").ap()

    hD = B.dram("hD", [LP, D], F32)
    projD = B.dram("projD", [LP, INC], F32)
    ymixD = B.dram("ymixD", [LP, D], BF16)
    moe_scr = dict(W13D=B.dram("W13D", [NE * 128, 8 * 2 * EH], BF16), W2D=B.dram("W2D", [NE * 128, 2 * D], BF16),
                   XgD=B.dram("XgD", [96 * 128, D], BF16), YgD=B.dram("YgD", [96 * 128, D], F32))
    hbD = B.dram("hbD", [LP, D], BF16)
    rw_scr = dict(opsD=[B.dram(f"opsD{d}", [LP, 4, RW], BF16) for d in range(2)],
                  vbD=B.dram("vbD", [LP, RW], BF16), gCD=B.dram("gCD", [NT, 64, 32], F32),
                  yD=[B.dram(f"yD{d}", [LP, RW], F32) for d in range(2)],
                  bonusD=B.dram("bonusD", [LP, RW], F32), gD=B.dram("gD", [LP, RW], F32),
                  vfD=B.dram("vfD", [LP, RW], F32))

    with ExitStack() as gs:
        ident = B.sb(gs, "ident", [128, 128], F32)
        identb = B.sb(gs, "identb", [128, 128], BF16)
        cosT = B.sb(gs, "cosT", [128, NT, 32], F32)
        sinT = B.sb(gs, "sinT", [128, NT, 32], F32)
        B.epst = {}
        for ev in (LN_EPS, RMS_EPS, GN_EPS, 1e-12, 1e-30):
            t = B.sb(gs, f"eps{len(B.epst)}", [128, 1], F32)
            B.memset('pool', t[:], ev, wr=['eps'])
            B.epst[ev] = t
        B.dma('sp', ident[:], ident_in, wr=['ident'])
        tri_t = [B.sb(gs, f"tri{q}", [128, 128], F32) for q in range(4)]
        for q in range(4):
            B.dma('sp', tri_t[q][:], tri_in[q], wr=['tri'])
        mk_t = [B.sb(gs, f"mk{q}", [128, 64], F32) for q in range(5)]
        for q in range(5):
            B.dma('sp', mk_t[q][:], masks_in[q], wr=['masks'])
        negc = B.sb(gs, "negc", [128, 1], F32)
        B.memset('pool', negc[:], DEC_C, wr=['negc'])
        rw_consts = dict(tri=[(tri_t[0], tri_t[1]), (tri_t[2], tri_t[3])], negc=negc,
                         masks=dict(lt=mk_t[0], le=mk_t[1], gt=mk_t[2], ge=mk_t[3]), i64=mk_t[4])
        B.cp('dve', identb[:], ident[:], rd=['ident'], wr=['identb'])

        with ExitStack() as st:
            posi = B.sb(st, "posi", [128, NT], I32)
            posf = B.sb(st, "posf", [128, NT], F32)
            invf = B.sb(st, "invf", [128, 32], F32)
            fr = B.sb(st, "fr", [128, NT, 32], F32)
            fri = B.sb(st, "fri", [128, NT, 32], I32)
            frf = B.sb(st, "frf", [128, NT, 32], F32)
            msk = B.sb(st, "msk", [128, NT, 32], F32)
            B.memset('pool', posi[:], 0, wr=['posi'])
            B.dma('sp', posi[16:128, 0:1], pos_in[0:112, :], rd=[], wr=['posi'])
            for i in range(1, NT):
                n = ts(i)
                B.dma('sp', posi[:n, i:i + 1], pos_in[128 * i - 16:128 * i - 16 + n, :], wr=['posi'])
            B.dma('sp', invf[:], invf_in.partition_broadcast(128), wr=['invf'])
            B.cp('dve', posf[:], posi[:], rd=['posi'], wr=['posf'])
            B.tsc('dve', posf[:], posf[:], float(NMETA), None, ALU.add, rd=['posf'], wr=['posf'])
            S.op('pool', lambda e: e.iota(posi[0:16, 0:1], pattern=[[0, 1]], base=0, channel_multiplier=1),
                 reads=[], writes=['posi'])
            B.cp('dve', posf[0:16, 0:1], posi[0:16, 0:1], rd=['posi', 'posf'], wr=['posf'])
            for (tab, off) in ((sinT, 0.0), (cosT, 0.25)):
                B.tt('dve', fr[:], posf[:].unsqueeze(2).broadcast_to([128, NT, 32]),
                     invf[:].unsqueeze(1).broadcast_to([128, NT, 32]), ALU.mult,
                     rd=['posf', 'invf'], wr=['fr'])
                if off:
                    B.tsc('dve', fr[:], fr[:], off, None, ALU.add, rd=['fr'], wr=['fr'])
                B.cp('dve', fri[:], fr[:], rd=['fr'], wr=['fri'])
                B.cp('dve', frf[:], fri[:], rd=['fri'], wr=['frf'])
                B.tt('dve', fr[:], fr[:], frf[:], ALU.subtract, rd=['fr', 'frf'], wr=['fr'])
                B.tsc('dve', msk[:], fr[:], 0.5, None, ALU.is_gt, rd=['fr'], wr=['msk'])
                B.tt('dve', fr[:], fr[:], msk[:], ALU.subtract, rd=['fr', 'msk'], wr=['fr'])
                B.tsc('dve', msk[:], fr[:], -0.5, None, ALU.is_lt, rd=['fr'], wr=['msk'])
                B.tt('dve', fr[:], fr[:], msk[:], ALU.add, rd=['fr', 'msk'], wr=['fr'])
                B.act(tab[:], fr[:], AF.Sin, rd=['fr'], wr=['tab'], scale=2.0 * np.pi)
            B.barrier()

        with ExitStack() as st:
            gbc = B.sb(st, "gbc", [128, D], F32)
            bbc = B.sb(st, "bbc", [128, D], F32)
            B.dma('sp', gbc[:], emb_g.partition_broadcast(128), wr=['gbc'])
            B.dma('sp', bbc[:], emb_b.partition_broadcast(128), wr=['bbc'])
            xts = [B.sb(st, f"xt{j}", [128, D], F32) for j in range(2)]
            lnw = _ln_work(B, st)
            for i in range(NT):
                n = ts(i)
                xt = xts[i % 2]
                k = f"xt{i % 2}"
                if i == 0:
                    B.dma('sp', xt[0:NMETA, :], meta, wr=[k])
                    B.dma('sp', xt[NMETA:128, :], x[0:128 - NMETA, :], wr=[k])
                else:
                    B.dma('sp', xt[:n, :], x[128 * i - NMETA:128 * i - NMETA + n, :], wr=[k])
                _layer_norm(B, lnw, xt[:n, :], n, gbc, bbc, LN_EPS, k)
                B.dma('act', hD[128 * i:128 * i + n, :], xt[:n, :], rd=[k], wr=[f"hD{i}"])
            B.barrier()

        with ExitStack() as st:
            z = B.sb(st, "zpad", [128, INC], F32)
            B.memset('pool', z[:], 0.0, wr=['z'])
            B.dma('sp', projD[L:LP, :], z[0:LP - L, :], rd=['z'], wr=['projpad'])
            B.barrier()

        for li in range(nlayers):
            _mixer_attention(B, li, locals())
            if stop_after == f"att{li}":
                break
            _rwkv(B, li, locals())
            if stop_after in (f"rwkv{li}", f"rwkvC{li}"):
                break
            _outproj_ln1(B, li, locals())
            if stop_after == f"ln1_{li}":
                break
            if MOE_SPARSE:
                _moe_sparse_ln2(B, li, locals(), last=(li == nlayers - 1))
            else:
                _moe_ln2(B, li, locals(), last=(li == nlayers - 1))
            if stop_after == f"moe{li}":
                break

        if 'ymixD' in B.dbg or True:
            pass

    fin = S.op('sp', lambda e: e.nop())
    fin.deps |= set(S.dmas_since_bar)
    stats = S.emit()
    return nc, stats


def _ln_work(B, st):
    return dict(stats=B.sb(st, "ln_st", [128, 2, 6], F32), mv=B.sb(st, "ln_mv", [128, 2], F32),
                rs=B.sb(st, "ln_rs", [128, 1], F32), nb=B.sb(st, "ln_nb", [128, 1], F32))


def _layer_norm(B, w, xap, n, gbc, bbc, eps, key):
    S = B.S
    stt_, mv, rs, nb = w['stats'], w['mv'], w['rs'], w['nb']
    for c in range(2):
        S.op('dve', (lambda c: lambda e: e.bn_stats(out=stt_[:n, c, :], in_=xap[:, c * 512:(c + 1) * 512]))(c),
             reads=[key], writes=['ln_st'])
    S.op('dve', lambda e: e.bn_aggr(out=mv[:n, :], in_=stt_[:n, :, :]), reads=['ln_st'], writes=['ln_mv'])
    B.act(rs[:n, :], mv[:n, 1:2], AF.Sqrt, rd=['ln_mv'], wr=['ln_rs'], bias=B.eps_ap(eps, n), scale=1.0)
    B.recip(rs[:n, :], rs[:n, :], rd=['ln_rs'], wr=['ln_rs'])
    B.tsc('dve', xap, xap, mv[:n, 0:1], rs[:n, 0:1], ALU.subtract, ALU.mult, rd=[key, 'ln_mv', 'ln_rs'], wr=[key])
    B.tt('pool', xap, xap, gbc[:n, :], ALU.mult, rd=[key, 'gbc'], wr=[key])
    B.tt('pool', xap, xap, bbc[:n, :], ALU.add, rd=[key, 'bbc'], wr=[key])


def _rope(B, eng, dst, src, cos_ap, sin_ap, tmp, n, shape, rd, wr, tkey):
    s1, s2 = src
    d1, d2 = dst
    ta, tb = tmp
    B.tt(eng, ta, s1, cos_ap, ALU.mult, rd=rd + ['tab'], wr=[tkey + 'a'])
    B.tt(eng, tb, s2, sin_ap, ALU.mult, rd=rd + ['tab'], wr=[tkey + 'b'])
    B.tt(eng, d1, ta, tb, ALU.subtract, rd=[tkey + 'a', tkey + 'b'], wr=wr)
    B.tt(eng, ta, s2, cos_ap, ALU.mult, rd=rd + ['tab', tkey + 'a'], wr=[tkey + 'a'])
    B.tt(eng, tb, s1, sin_ap, ALU.mult, rd=rd + ['tab', tkey + 'b'], wr=[tkey + 'b'])
    B.tt(eng, d2, ta, tb, ALU.add, rd=[tkey + 'a', tkey + 'b'], wr=wr)


def _mixer_attention(B, li, g):
    nc, S = B.nc, B.S
    hD, projD, ymixD = g['hD'], g['projD'], g['ymixD']
    ident, identb, cosT, sinT = g['ident'], g['identb'], g['cosT'], g['sinT']
    w_in, w_uq, w_ukv = g['w_in'], g['w_uq'], g['w_ukv']
    q_norm, kv_norm, out_norm = g['q_norm'], g['kv_norm'], g['out_norm']

    with ExitStack() as rs_:
        KnT = B.sb(rs_, "KnT", [128, HA, LP], BF16)
        KpT = B.sb(rs_, "KpT", [65, LP], BF16)
        Vaug = B.sb(rs_, "Vaug", [128, NT, HA, VD + 1], BF16)
        kmaxb = B.sb(rs_, "kmaxb", [128, 1], F32)
        with ExitStack() as st:
            win = B.sb(st, "win", [128, 8, INC], BF16)
            wukv = B.sb(st, "wukv", [128, HA, 2, 128], BF16)
            gkv = B.sb(st, "gkv", [128, KVL], F32)
            kmax = B.sb(st, "kmax", [128, HA], F32)
            hts = [B.sb(st, f"ht{j}", [128, D], F32) for j in range(2)]
            hTs = [B.sb(st, f"hT{j}", [128, 8, 128], BF16) for j in range(2)]
            pjs = [B.sb(st, f"pj{j}", [128, INC], F32) for j in range(2)]
            ckvn = B.sb(st, "ckvn", [128, KVL], BF16)
            ckvT = B.sb(st, "ckvT", [128, 128], BF16)
            ssq = B.sb(st, "ssq", [128, 1], F32)
            junk = B.sb(st, "junk", [128, 512], F32)
            kss = B.sb(st, "kss", [128, HA], F32)
            kpss = B.sb(st, "kpss", [128, 1], F32)
            kpe = B.sb(st, "kpe", [128, QKR], BF16)
            rta = B.sb(st, "rta", [128, 32], F32)
            rtb = B.sb(st, "rtb", [128, 32], F32)
            pT = [B.ps(st, f"pT{j}", [128, 4, 128], F32) for j in range(2)]
            pP = [B.ps(st, f"pP{j}", [128, 512], F32) for j in range(2)]
            pKV = B.ps(st, "pKV", [128, 2, 512], F32)
            pK = B.ps(st, "pK", [128, HA, 128], F32)
            pTb = B.ps(st, "pTb", [128, 2, 128], BF16)

            for c in range(8):
                B.dma('pool', win[:, c, :], w_in[li, c * 128:(c + 1) * 128, :], wr=['win'])
            B.dma('pool', wukv[:].rearrange("p h t d -> p (h t d)"), w_ukv[li], wr=['wukv'])
            B.dma('sp', gkv[:], kv_norm[li].partition_broadcast(128), wr=['gkv'])
            B.memset('pool', kmax[:], 0.0, wr=['kmax'])
            B.memset('pool', Vaug[:], 0.0, wr=['Vaug'])
            for i in range(NT):
                B.memset('pool', Vaug[:ts(i), i, :, VD:VD + 1], 1.0, wr=['Vaug'])
            B.memset('pool', KpT[64:65, :], 1.0, wr=['KpT'])

            for i in range(NT):
                n = ts(i)
                j = i % 2
                ht, hT, pj = hts[j], hTs[j], pjs[j]
                kh, khT, kpj = f"ht{j}", f"hT{j}", f"pj{j}"
                r0 = 128 * i
                B.dma('sp', ht[:n, :], hD[r0:r0 + n, :], rd=[f"hD{i}"], wr=[kh])
                for half in range(2):
                    pt = pT[half]
                    for c in range(4):
                        B.tr(pt[:, c, :n], ht[:n, (half * 4 + c) * 128:(half * 4 + c + 1) * 128], ident[:n, :n],
                             rd=[kh, 'ident'], wr=[f"pT{half}"])
                    B.cp('act' if half == 0 else 'dve', hT[:, half * 4:half * 4 + 4, :n], pt[:, :, :n],
                         rd=[f"pT{half}"], wr=[khT])
                c0 = 0
                ci = 0
                while c0 < INC:
                    w = min(512, INC - c0)
                    pp = pP[ci % 2]
                    for k in range(8):
                        B.mm(pp[:n, :w], hT[:, k, :n], win[:, k, c0:c0 + w], start=(k == 0), stop=(k == 7),
                             rd=[khT, 'win'], wr=[f"pP{ci % 2}"])
                    B.cp('act' if ci % 2 == 0 else 'dve', pj[:n, c0:c0 + w], pp[:n, :w], rd=[f"pP{ci % 2}"], wr=[kpj])
                    c0 += w
                    ci += 1
                B.dma('act', projD[r0:r0 + n, :], pj[:n, :], rd=[kpj], wr=[f"projD{i}"])
                ckv = pj[:n, QL:QL + KVL]
                B.act(junk[:n, :KVL], ckv, AF.Square, rd=[kpj], wr=['junk', 'ssq'], accum=ssq[:n, :])
                B.rsqrt_(ssq[:n, :], 1.0 / KVL, RMS_EPS, 'ssq')
                B.stt(ckvn[:n, :], ckv, ssq[:n, 0:1], gkv[:n, :], ALU.mult, ALU.mult, rd=[kpj, 'ssq', 'gkv'], wr=['ckvn'])
                B.tr(pTb[:, 0, :n], ckvn[:n, :], identb[:n, :n], rd=['ckvn', 'identb'], wr=['pTb0'])
                B.cp('act', ckvT[:, :n], pTb[:, 0, :n], rd=['pTb0'], wr=['ckvT'])
                for t_ in range(2):
                    B.mm(pKV[:n, t_, :].rearrange("p (h d) -> p h d", h=HA), ckvT[:, :n], wukv[:, :, t_, :],
                         rd=['ckvT', 'wukv'], wr=[f"pKV{t_}"])
                B.cp('dve', Vaug[:n, i, :, 0:VD], pKV[:n, 1, :].rearrange("p (h d) -> p h d", h=HA),
                     rd=['pKV1'], wr=['Vaug'])
                B.act(junk[:n, :], pKV[:n, 0, :], AF.Square, rd=['pKV0'], wr=['junk'])
                B.red(kss[:n, :], junk[:n, :].rearrange("p (h d) -> p h d", h=HA), ALU.add, rd=['junk'], wr=['kss'])
                kp = pj[:n, QL + KVL:QL + KVL + QKR]
                B.act(junk[:n, :QKR], kp, AF.Square, rd=[kpj, 'kss'], wr=['junk', 'kpss'], accum=kpss[:n, :])
                B.tsc('dve', kss[:n, :], kss[:n, :], kpss[:n, 0:1], None, ALU.add, rd=['kss', 'kpss'], wr=['kss'])
                B.tt('dve', kmax[:n, :], kmax[:n, :], kss[:n, :], ALU.max, rd=['kmax', 'kss'], wr=['kmax'])
                _rope(B, 'pool', (kpe[:n, 0:32], kpe[:n, 32:64]), (kp[:, 0:32], kp[:, 32:64]),
                      cosT[:n, i, :], sinT[:n, i, :], (rta[:n, :], rtb[:n, :]), n, None, [kpj], ['kpe'], 'rt')
                B.tr(pTb[0:QKR, 1, :n], kpe[:n, :], identb[:n, :n], rd=['kpe', 'identb'], wr=['pTb1'])
                B.cp('act', KpT[0:QKR, r0:r0 + n], pTb[0:QKR, 1, :n], rd=['pTb1'], wr=['KpT'])
                for h in range(HA):
                    B.mm(pK[:, h, :n], wukv[:, h, 0, :], ckvT[:, :n], rd=['wukv', 'ckvT'], wr=['pK'])
                B.cp('dve', KnT[:, :, r0:r0 + n], pK[:, :, :n], rd=['pK'], wr=['KnT'])
            kmr = B.sb(st, "kmr", [128, 1], F32)
            kmT = B.sb(st, "kmT", [1, 128], F32)
            km1 = B.sb(st, "km1", [1, 1], F32)
            ones1 = B.sb(st, "ones1", [1, 128], F32)
            B.memset('pool', ones1[:], 1.0, wr=['ones1'])
            B.red(kmr[:], kmax[:], ALU.max, rd=['kmax'], wr=['kmr'])
            B.tr(pT[0][0:1, 0, :], kmr[:, 0:1], ident[:, :], rd=['kmr', 'ident'], wr=['pT0'])
            B.cp('act', kmT[:], pT[0][0:1, 0, :], rd=['pT0'], wr=['kmT'])
            B.red(km1[:], kmT[:], ALU.max, rd=['kmT'], wr=['km1'])
            B.mm(pT[1][:, 0, 0:1], ones1[:], km1[:], rd=['ones1', 'km1'], wr=['pT1'])
            B.cp('act', kmaxb[:], pT[1][:, 0, 0:1], rd=['pT1'], wr=['kmaxb'])
            B.barrier()

        with ExitStack() as rq_:
            QnT = B.sb(rq_, "QnT", [128, HA, LP], BF16)
            QpT = B.sb(rq_, "QpT", [65, HA, LP], BF16)
            with ExitStack() as st:
                wuq = B.sb(st, "wuq", [128, 2, HA * (QKN + QKR)], BF16)
                gq = B.sb(st, "gq", [128, QL], F32)
                cqs = [B.sb(st, f"cq{j}", [128, QL], F32) for j in range(2)]
                cqn = B.sb(st, "cqn", [128, QL], BF16)
                cqT = B.sb(st, "cqT", [128, 2, 128], BF16)
                qf = B.sb(st, "qf", [128, HA, QKN + QKR], F32)
                qb = B.sb(st, "qb", [128, HA, QKN], BF16)
                qpa = B.sb(st, "qpa", [128, HA, QKR + 1], BF16)
                ssq = B.sb(st, "ssq2", [128, 1], F32)
                qss = B.sb(st, "qss", [128, HA], F32)
                junk = B.sb(st, "junk2", [128, HA * (QKN + QKR)], F32)
                rta = B.sb(st, "rta2", [128, HA, 32], F32)
                rtb = B.sb(st, "rtb2", [128, HA, 32], F32)
                pTb = B.ps(st, "pTb2", [128, 2, 128], BF16)
                pQ = B.ps(st, "pQ", [128, 2, 512], F32)
                pQT = B.ps(st, "pQT", [128, HA, 128], BF16)
                pQP = B.ps(st, "pQP", [65, HA, 128], BF16)
                for c in range(2):
                    B.dma('pool', wuq[:, c, :], w_uq[li, c * 128:(c + 1) * 128, :], wr=['wuq'])
                B.dma('sp', gq[:], q_norm[li].partition_broadcast(128), wr=['gq'])
                for i in range(NT):
                    n = ts(i)
                    j = i % 2
                    cq, kcq = cqs[j], f"cq{j}"
                    r0 = 128 * i
                    B.dma('sp', cq[:n, :], projD[r0:r0 + n, 0:QL], rd=[f"projD{i}"], wr=[kcq])
                    B.act(junk[:n, :QL], cq[:n, :], AF.Square, rd=[kcq], wr=['junk', 'ssq'], accum=ssq[:n, :])
                    B.rsqrt_(ssq[:n, :], 1.0 / QL, RMS_EPS, 'ssq')
                    B.stt(cqn[:n, :], cq[:n, :], ssq[:n, 0:1], gq[:n, :], ALU.mult, ALU.mult, rd=[kcq, 'ssq', 'gq'], wr=['cqn'])
                    for c in range(2):
                        B.tr(pTb[:, c, :n], cqn[:n, c * 128:(c + 1) * 128], identb[:n, :n], rd=['cqn', 'identb'], wr=['pTb'])
                    B.cp('act', cqT[:, :, :n], pTb[:, :, :n], rd=['pTb'], wr=['cqT'])
                    for cc in range(2):
                        for k in range(2):
                            B.mm(pQ[:n, cc, 0:384], cqT[:, k, :n], wuq[:, k, cc * 384:(cc + 1) * 384],
                                 start=(k == 0), stop=(k == 1), rd=['cqT', 'wuq'], wr=[f"pQ{cc}"])
                    qff = qf[:].rearrange("p h d -> p (h d)")
                    B.cp('act', qff[:n, 0:384], pQ[:n, 0, 0:384], rd=['pQ0'], wr=['qf'])
                    B.cp('dve', qff[:n, 384:768], pQ[:n, 1, 0:384], rd=['pQ1'], wr=['qf'])
                    B.act(junk[:n, :], qff[:n, :], AF.Square, rd=['qf'], wr=['junk'])
                    B.red(qss[:n, :], junk[:n, :].rearrange("p (h d) -> p h d", h=HA), ALU.add, rd=['junk'], wr=['qss'])
                    B.tsc('dve', qss[:n, :], qss[:n, :], kmaxb[:n, 0:1], None, ALU.mult, rd=['qss', 'kmaxb'], wr=['qss'])
                    B.act(qss[:n, :], qss[:n, :], AF.Sqrt, rd=['qss'], wr=['qss'])
                    B.tsc('dve', qpa[:n, :, QKR:QKR + 1], qss[:n, :].unsqueeze(2), -1.0, None, ALU.mult, rd=['qss'], wr=['qpa'])
                    B.cp('pool', qb[:n, :, :], qf[:n, :, 0:QKN], rd=['qf'], wr=['qb'])
                    cosb = cosT[:n, i, :].unsqueeze(1).broadcast_to([n, HA, 32])
                    sinb = sinT[:n, i, :].unsqueeze(1).broadcast_to([n, HA, 32])
                    _rope(B, 'pool', (qpa[:n, :, 0:32], qpa[:n, :, 32:64]),
                          (qf[:n, :, QKN:QKN + 32], qf[:n, :, QKN + 32:QKN + 64]),
                          cosb, sinb, (rta[:n], rtb[:n]), n, None, ['qf'], ['qpa'], 'rq')
                    for h in range(HA):
                        B.tr(pQT[:, h, :n], qb[:n, h, :], identb[:n, :n], rd=['qb', 'identb'], wr=['pQT'])
                    B.cp('act', QnT[:, :, r0:r0 + n], pQT[:, :, :n], rd=['pQT'], wr=['QnT'])
                    for h in range(HA):
                        B.tr(pQP[:, h, :n], qpa[:n, h, :], identb[:n, :n], rd=['qpa', 'identb'], wr=['pQP'])
                    B.cp('dve', QpT[:, :, r0:r0 + n], pQP[:, :, :n], rd=['pQP'], wr=['QpT'])
                B.barrier()

            with ExitStack() as st:
                gout = B.sb(st, "gout", [128, HA * VD], F32)
                B.dma('sp', gout[:], out_norm[li].partition_broadcast(128), wr=['gout'])
                yatt = B.sb(st, "yatt", [128, NT, HA * VD], BF16)
                PTs = [B.sb(st, f"PT{j}", [128, 512], BF16) for j in range(4)]
                ob = B.sb(st, "ob", [128, VD], F32)
                rden = B.sb(st, "rden", [128, 1], F32)
                oss = B.sb(st, "oss", [128, 1], F32)
                junk = B.sb(st, "junk3", [128, VD], F32)
                pS = [B.ps(st, f"pS{j}", [128, 512], F32) for j in range(4)]
                pO = [B.ps(st, f"pO{j}", [128, VD + 1], F32) for j in range(4)]
                obuf = [B.sb(st, f"obuf{j}", [128, VD + 1], F32) for j in range(4)]
                it = 0
                pend = None

                def emit_pv(p):
                    (h_, kt_, nk_, nqt_, wq_, PT_, kP_) = p
                    for jq in range(nqt_):
                        nq = min(128, wq_ - 128 * jq)
                        B.mm(pO[jq][:nq, :], PT_[:nk_, 128 * jq:128 * jq + nq], Vaug[:nk_, kt_, h_, :],
                             start=(kt_ == 0), stop=(kt_ == NT - 1), rd=[kP_, 'Vaug'], wr=[f"pO{jq}"])

                for h in range(HA):
                    for c in range(9):
                        q0 = 512 * c
                        wq = min(512, L - q0)
                        nqt = (wq + 127) // 128
                        for kt in range(NT):
                            nk = ts(kt)
                            k0 = 128 * kt
                            ps_ = pS[it % 4]
                            pk = f"pS{it % 4}"
                            PT = PTs[it % 4]
                            kP = f"PT{it % 4}"
                            it += 1
                            B.mm(ps_[:nk, :wq], KnT[:, h, k0:k0 + nk], QnT[:, h, q0:q0 + wq], start=True, stop=False,
                                 rd=['KnT', 'QnT'], wr=[pk])
                            B.mm(ps_[:nk, :wq], KpT[:, k0:k0 + nk], QpT[:, h, q0:q0 + wq], start=False, stop=True,
                                 rd=['KpT', 'QpT'], wr=[pk])
                            B.act(PT[:nk, :wq], ps_[:nk, :wq], AF.Exp, rd=[pk], wr=[kP], scale=ATT_SCALE)
                            if pend is not None:
                                emit_pv(pend)
                            pend = (h, kt, nk, nqt, wq, PT, kP)
                        emit_pv(pend)
                        pend = None
                        for jq in range(nqt):
                            nq = min(128, wq - 128 * jq)
                            B.cp('dve', obuf[jq][:nq, :], pO[jq][:nq, :], rd=[f"pO{jq}"], wr=[f"obuf{jq}"])
                        for jq in range(nqt):
                            nq = min(128, wq - 128 * jq)
                            ti = (q0 + 128 * jq) // 128
                            po = obuf[jq]
                            ko = f"obuf{jq}"
                            B.recip(rden[:nq, :], po[:nq, VD:VD + 1], rd=[ko], wr=['rden'])
                            B.tsc('dve', ob[:nq, :], po[:nq, 0:VD], rden[:nq, 0:1], None, ALU.mult,
                                  rd=[ko, 'rden'], wr=['ob'])
                            B.act(junk[:nq, :], ob[:nq, :], AF.Square, rd=['ob'], wr=['junk', 'oss'], accum=oss[:nq, :])
                            B.rsqrt_(oss[:nq, :], 1.0 / VD, RMS_EPS, 'oss')
                            B.stt(yatt[:nq, ti, h * VD:(h + 1) * VD], ob[:nq, :], oss[:nq, 0:1],
                                  gout[:nq, h * VD:(h + 1) * VD], ALU.mult, ALU.mult, rd=['ob', 'oss', 'gout'], wr=['yatt'])
                B.dma('sp', ymixD[0:128 * (NT - 1), 0:HA * VD].rearrange("(t p) c -> p t c", p=128),
                      yatt[:, 0:NT - 1, :], rd=['yatt'], wr=['ymixA'])
                B.dma('sp', ymixD[128 * (NT - 1):L, 0:HA * VD], yatt[:ts(NT - 1), NT - 1, :], rd=['yatt'], wr=['ymixA'])
                B.barrier()


DEC_C = -float(np.exp(-0.5))


def _rwkv(B, li, g):
    nc, S = B.nc, B.S
    projD, ymixD = g['projD'], g['ymixD']
    ident, identb = g['ident'], g['identb']
    R = g['rw_scr']
    opsD, vbD, gCD, yD, bonusD, gD, vfD = R['opsD'], R['vbD'], R['gCD'], R['yD'], R['bonusD'], R['gD'], R['vfD']
    cst = g['rw_consts']
    P = g['rw_params']

    with ExitStack() as st:
        def bc(name, ap, width):
            t = B.sb(st, name, [128, width], F32)
            B.dma('sp', t[:], ap.partition_broadcast(128), wr=[name])
            return t
        mp = bc("mp", P['mu_prev'][li], RWC)
        mn = bc("mn", P['mu_next'][li], RWC)
        c0 = B.sb(st, "c0", [128, RWC], F32)
        B.tt('dve', c0[:], mp[:], mn[:], ALU.add, rd=['mp', 'mn'], wr=['c0'])
        B.tsc('dve', c0[:], c0[:], -1.0, 1.0, ALU.mult, ALU.add, rd=['c0'], wr=['c0'])
        w0 = [bc(f"w0_{d}", P['w0'][li, d], RW) for d in range(2)]
        a0 = [bc(f"a0_{d}", P['a0'][li, d], RW) for d in range(2)]
        kkb = bc("kkb", P['k_k'][li], RW)
        kab = bc("kab", P['k_a'][li], RW)
        omka = B.sb(st, "omka", [128, RW], F32)
        B.tsc('dve', omka[:], kab[:], -1.0, 1.0, ALU.mult, ALU.add, rd=['kab'], wr=['omka'])
        rkb = bc("rkb", P['r_k'][li].rearrange("h n -> (h n)"), RW)
        w2 = B.sb(st, "w2", [128, RW], BF16)
        a2 = B.sb(st, "a2", [128, RW], BF16)
        g2 = B.sb(st, "g2", [128, RW], BF16)
        B.dma('pool', w2[:], P['w2'][li].rearrange("d k n -> (d k) n"), wr=['w2'])
        B.dma('pool', a2[:], P['a2'][li].rearrange("d k n -> (d k) n"), wr=['a2'])
        B.dma('pool', g2[:], P['g2'][li], wr=['g2'])
        if li > 0:
            v0b = bc("v0b", P['v0'][li - 1], RW)
            v1 = B.sb(st, "v1", [128, 4, 32], BF16)
            v2 = B.sb(st, "v2", [32, RW], BF16)
            B.dma('pool', v1[:], P['v1'][li - 1].rearrange("(c p) n -> p c n", p=128), wr=['v1'])
            B.dma('pool', v2[:], P['v2'][li - 1], wr=['v2'])
        ctr = [B.sb(st, f"ctr{j}", [128, RWC], F32) for j in range(2)]
        prv = [B.sb(st, f"prv{j}", [128, RWC], F32) for j in range(2)]
        nxt = [B.sb(st, f"nxt{j}", [128, RWC], F32) for j in range(2)]
        def two(name, shape, dt):
            return [B.sb(st, f"{name}_{q}", shape, dt) for q in range(2)]
        u_2 = two("u", [128, RWC], F32)
        lin_2 = two("lin", [128, 3, 128], BF16)
        linT_2 = two("linT", [128, 3, 128], BF16)
        kk_2 = two("kk", [128, RW], F32)
        t1_2 = two("t1", [128, RW], F32)
        t2_2 = two("t2", [128, RW], F32)
        t3_2 = two("t3", [128, RW], F32)
        rr_2 = two("rr", [128, RW], F32)
        sg_2 = [two(f"sg{d}", [128, RW], F32) for d in range(2)]
        av_2 = two("av", [128, RW], F32)
        kd_2 = two("kd", [128, RW], F32)
        ex_2 = two("ex", [128, 3, RW], F32)
        s8_2 = two("s8", [128, HB], F32)
        rks_2 = two("rks", [128, HB], F32)
        stg = [[B.sb(st, f"stg{d}{j}", [128, 4, RW], BF16) for j in range(2)] for d in range(2)]
        vb = [B.sb(st, f"vb{j}", [128, RW], BF16) for j in range(2)]
        gst = [B.sb(st, f"gst{j}", [128, RW], F32) for j in range(2)]
        bon = [B.sb(st, f"bon{j}", [128, RW], F32) for j in range(2)]
        gcs = [B.sb(st, f"gcs{j}", [64, 32], F32) for j in range(2)]
        if li > 0:
            vf = [B.sb(st, f"vf{j}", [128, RW], F32) for j in range(2)]
            vbt_2 = two("vbt", [128, RW], BF16)
            vT_2 = two("vT", [128, 4, 128], BF16)
            vl_2 = two("vl", [128, 32], BF16)
            vlT_2 = two("vlT", [32, 128], BF16)
        pL = B.ps(st, "pL", [128, 1024], BF16)
        pW = [B.ps(st, f"pW{j}", [128, RW], F32) for j in range(2)]
        pC = [B.ps(st, f"pC{j}", [128, RW], F32) for j in range(2)]
        pL2 = B.ps(st, "pL2", [128, 1024], BF16)
        pG = B.ps(st, "pG", [64, 32], F32)
        pV = B.ps(st, "pV", [128, RW], F32)

        deferred = []

        def defer_store(fn):
            deferred.append((S.sfx, fn))

        def flush_stores():
            cur = S.sfx
            for (sf, fn) in deferred:
                S.sfx = sf
                fn()
            S.sfx = cur
            del deferred[:]

        B.barrier()
        for i in range(NT):
            n = ts(i)
            j = i % 2
            r0 = 128 * i
            kc, kp_, kn = f"ctr{j}", f"prv{j}", f"nxt{j}"
            S.sfx = f"@{j}"
            S.nosfx = ('pL', 'pL2', 'pW0', 'pW1', 'pC0', 'pC1', 'pG', 'pV')
            u, lin, linT, kk, t1, t2, t3, rr, av, kd, ex, s8, rks = (x_[j] for x_ in (
                u_2, lin_2, linT_2, kk_2, t1_2, t2_2, t3_2, rr_2, av_2, kd_2, ex_2, s8_2, rks_2))
            sg = [sg_2[0][j], sg_2[1][j]]
            if li > 0:
                vbt, vT, vl, vlT = vbt_2[j], vT_2[j], vl_2[j], vlT_2[j]
            if n < 128:
                B.memset('pool', ctr[j][:], 0.0, wr=[kc])
                B.memset('pool', prv[j][:], 0.0, wr=[kp_])
                B.memset('pool', nxt[j][:], 0.0, wr=[kn])
            B.dma('sp', ctr[j][:n, :], projD[r0:r0 + n, INC - RWC:INC], wr=[kc])
            if i == 0:
                B.memset('pool', prv[j][0:32, :], 0.0, wr=[kp_])
                B.dma('sp', prv[j][1:128, :], projD[0:127, INC - RWC:INC], wr=[kp_])
            else:
                B.dma('sp', prv[j][:n, :], projD[r0 - 1:r0 - 1 + n, INC - RWC:INC], wr=[kp_])
            B.dma('sp', nxt[j][:n, :], projD[r0 + 1:r0 + 1 + n, INC - RWC:INC], wr=[kn])
            flush_stores()
            B.tt('pool', u[:], ctr[j][:], c0[:], ALU.mult, rd=[kc, 'c0'], wr=['u'])
            B.tt('dve', prv[j][:], prv[j][:], mp[:], ALU.mult, rd=[kp_, 'mp'], wr=[kp_])
            B.tt('pool', u[:], u[:], prv[j][:], ALU.add, rd=['u', kp_], wr=['u'])
            B.tt('dve', nxt[j][:], nxt[j][:], mn[:], ALU.mult, rd=[kn, 'mn'], wr=[kn])
            B.tt('pool', u[:], u[:], nxt[j][:], ALU.add, rd=['u', kn], wr=['u'])
            r_ = u[:, 0:RW]
            k_ = u[:, RW:2 * RW]
            v_ = u[:, 2 * RW:3 * RW]
            B.act(lin[:, 0, :], u[:, 1536:1664], AF.Tanh, rd=['u'], wr=['lin'])
            B.cp('pool', lin[:, 1, :], u[:, 1664:1792], rd=['u'], wr=['lin'])
            B.act(lin[:, 2, :], u[:, 1792:1920], AF.Sigmoid, rd=['u'], wr=['lin'])
            for c in range(3):
                B.tr(pL[:, c * 128:(c + 1) * 128], lin[:, c, :], identb[:], rd=['lin', 'identb'], wr=['pL'])
            B.cp('act', linT[:].rearrange("p c n -> p (c n)"), pL[:, 0:384], rd=['pL'], wr=['linT'])
            if li > 0:
                B.dma('sp', vf[j][:], vfD[r0:r0 + 128, :], wr=[f"vf{j}"])
                B.cp('pool', vbt[:], v_, rd=['u'], wr=['vbt'])
                for c in range(4):
                    B.tr(pL2[:, c * 128:(c + 1) * 128], vbt[:, c * 128:(c + 1) * 128], identb[:],
                         rd=['vbt', 'identb'], wr=['pL2'])
                B.cp('dve', vT[:].rearrange("p c n -> p (c n)"), pL2[:, 0:512], rd=['pL2'], wr=['vT'])
                for c in range(4):
                    B.mm(pV[:, 0:32], vT[:, c, :], v1[:, c, :], start=(c == 0), stop=(c == 3), rd=['vT', 'v1'], wr=['pV'])
                B.cp('act', vl[:], pV[:, 0:32], rd=['pV'], wr=['vl'])
                B.tr(pL2[0:32, 512:640], vl[:], identb[:], rd=['vl', 'identb'], wr=['pL2'])
                B.cp('act', vlT[:], pL2[0:32, 512:640], rd=['pL2'], wr=['vlT'])
                B.mm(pV[:], vlT[:], v2[:], rd=['vlT', 'v2'], wr=['pV'])
                B.tt('dve', t1[:], pV[:], v0b[:], ALU.add, rd=['pV', 'v0b'], wr=['t1'])
                B.act(t1[:], t1[:], AF.Sigmoid, rd=['t1'], wr=['t1'])
                B.tt('pool', t2[:], vf[j][:], v_, ALU.subtract, rd=[f"vf{j}", 'u'], wr=['t2'])
                B.tt('dve', t2[:], t2[:], t1[:], ALU.mult, rd=['t1', 't2'], wr=['t2'])
                B.tt('pool', v_, v_, t2[:], ALU.add, rd=['t2', 'u'], wr=['u'])
            else:
                defer_store(lambda i=i, j=j, r0=r0, v_=v_, **kw_: B.dma('sp', vfD[r0:r0 + 128, :], v_, rd=['u'], wr=[f"vfD{i}"]))
            B.cp('pool', vb[j][:], v_, rd=['u'], wr=[f"vb{j}"])
            defer_store(lambda i=i, j=j, r0=r0, **kw_: B.dma('sp', vbD[r0:r0 + 128, :], vb[j][:], rd=[f"vb{j}"], wr=[f"vbD{i}"]))
            B.mm(pW[0][:], linT[:, 2, :], g2[:], rd=['linT', 'g2'], wr=['pW0'])
            B.cp('act', gst[j][:], pW[0][:], rd=['pW0'], wr=[f"gst{j}"])
            defer_store(lambda i=i, j=j, r0=r0, **kw_: B.dma('sp', gD[r0:r0 + 128, :], gst[j][:], rd=[f"gst{j}"], wr=[f"gD{i}"]))
            B.tt('dve', kk[:], k_, kkb[:], ALU.mult, rd=['u', 'kkb'], wr=['kk'])
            B.act(t1[:], kk[:], AF.Square, rd=['kk'], wr=['t1'])
            B.red(s8[:], t1[:].rearrange("p (h n) -> p h n", h=HB), ALU.add, rd=['t1'], wr=['s8'])
            B.act(s8[:], s8[:], AF.Sqrt, rd=['s8'], wr=['s8'], bias=B.eps_ap(1e-12, 128), scale=1.0)
            B.recip(s8[:], s8[:], rd=['s8'], wr=['s8'])
            B.tt('dve', kk[:].rearrange("p (h n) -> p h n", h=HB), kk[:].rearrange("p (h n) -> p h n", h=HB),
                 s8[:].unsqueeze(2).broadcast_to([128, HB, NB]), ALU.mult, rd=['kk', 's8'], wr=['kk'])
            B.tt('pool', rr[:], r_, rkb[:], ALU.mult, rd=['u', 'rkb'], wr=['rr'])
            for d in range(2):
                B.mm(pW[0][:], linT[64 * d:64 * d + 64, 0, :], w2[64 * d:64 * d + 64, :], rd=['linT', 'w2'], wr=['pW0'])
                B.tt('dve', sg[d][:], pW[0][:], w0[d][:], ALU.add, rd=['pW0', f"w0_{d}"], wr=[f"sg{d}"])
                B.act(sg[d][:], sg[d][:], AF.Sigmoid, rd=[f"sg{d}"], wr=[f"sg{d}"])
                B.mm(pW[1][:], linT[64 * d:64 * d + 64, 1, :], a2[64 * d:64 * d + 64, :], rd=['linT', 'a2'], wr=['pW1'])
                B.tt('dve', av[:], pW[1][:], a0[d][:], ALU.add, rd=['pW1', f"a0_{d}"], wr=['av'])
                B.act(av[:], av[:], AF.Sigmoid, rd=['av'], wr=['av'])
                B.tt('pool', t2[:], av[:], kab[:], ALU.mult, rd=['av', 'kab'], wr=['t2'])
                B.tt('pool', t2[:], t2[:], omka[:], ALU.add, rd=['t2', 'omka'], wr=['t2'])
                B.tt('pool', kd[:], t2[:], k_, ALU.mult, rd=['t2', 'u'], wr=['kd'])
                B.tt('dve', t3[:], rr[:], kd[:], ALU.mult, rd=['rr', 'kd'], wr=['t3'])
                if d == 0:
                    B.red(rks[:], t3[:].rearrange("p (h n) -> p h n", h=HB), ALU.add, rd=['t3'], wr=['rks'])
                else:
                    B.red(s8[:], t3[:].rearrange("p (h n) -> p h n", h=HB), ALU.add, rd=['t3'], wr=['s8'])
                    B.tt('dve', rks[:], rks[:], s8[:], ALU.add, rd=['rks', 's8'], wr=['rks'])
                tin, tex = cst['tri'][d]
                B.mm(pC[0][:], tin[:], sg[d][:], rd=['tri', f"sg{d}"], wr=['pC0'])
                B.mm(pC[1][:], tex[:], sg[d][:], rd=['tri', f"sg{d}"], wr=['pC1'])
                B.act(ex[:, 0, :], pC[1][:], AF.Exp, rd=['pC1'], wr=['ex0'])
                B.act(ex[:, 1, :], pC[0][:], AF.Exp, rd=['pC0'], wr=['ex1'], scale=-1.0)
                B.act(ex[:, 2, :], pC[0][:], AF.Exp, rd=['pC0'], wr=['ex2'])
                sd = stg[d][j]
                ks = f"stg{d}{j}"
                B.tt('dve', sd[:, 0, :], kk[:], ex[:, 0, :], ALU.mult, rd=['kk', 'ex0'], wr=[ks])
                B.tt('pool', sd[:, 1, :], kd[:], ex[:, 1, :], ALU.mult, rd=['kd', 'ex1'], wr=[ks])
                B.tt('pool', t2[:], kk[:], av[:], ALU.mult, rd=['kk', 'av', 't2'], wr=['t2'])
                B.stt(sd[:, 2, :], t2[:], -1.0, ex[:, 1, :], ALU.mult, ALU.mult, rd=['t2', 'ex1'], wr=[ks])
                B.tt('dve', sd[:, 3, :], r_, ex[:, 2, :], ALU.mult, rd=['u', 'ex2'], wr=[ks])
                defer_store(lambda i=i, d=d, r0=r0, sd=sd, ks=ks, **kw_: B.dma('sp', opsD[d][r0:r0 + 128, :, :], sd[:], rd=[ks], wr=[f"opsD{d}_{i}"]))
                for half in range(2):
                    for h in range(HB):
                        col = d * 16 + half * 8 + h
                        B.mm(pG[:, col:col + 1], sg[d][64 * half:64 * half + 64, h * NB:(h + 1) * NB],
                             cst['negc'][64 * half:64 * half + 64, :], rd=[f"sg{d}", 'negc'], wr=['pG'])
            B.act(gcs[j][:], pG[:], AF.Exp, rd=['pG'], wr=[f"gcs{j}"])
            defer_store(lambda i=i, j=j, r0=r0, **kw_: B.dma('sp', gCD[i], gcs[j][:], rd=[f"gcs{j}"], wr=[f"gCD{i}"]))
            B.tt('dve', bon[j][:].rearrange("p (h n) -> p h n", h=HB), v_.rearrange("p (h n) -> p h n", h=HB),
                 rks[:].unsqueeze(2).broadcast_to([128, HB, NB]), ALU.mult, rd=['u', 'rks'], wr=[f"bon{j}"])
            defer_store(lambda i=i, j=j, r0=r0, **kw_: B.dma('sp', bonusD[r0:r0 + 128, :], bon[j][:], rd=[f"bon{j}"], wr=[f"bonusD{i}"]))
        flush_stores()
        S.sfx = ''
        B.barrier()
    if g.get('stop_after') == f"rwkvC{li}":
        return

    with ExitStack() as st:
        HH = 4
        pTr = B.ps(st, "pTr", [64, HH, 128], BF16)
        dbl = [B.ps(st, f"db{j}", [64, 2, HH, NB], F32) for j in range(4)]
        sgl = [B.ps(st, f"sgl{j}", [64, HH, NB], F32) for j in range(3)]
        bstate = {'d': 0, 's': 0}

        def dbank():
            j = bstate['d'] % 4
            bstate['d'] += 1
            return dbl[j], f"db{j}"

        def sbank():
            j = bstate['s'] % 3
            bstate['s'] += 1
            return sgl[j], f"sgb{j}"

        Mst = [B.sb(st, f"Mst{d}", [64, HH, NB], F32) for d in range(4)]
        Mbf = [B.sb(st, f"Mbf{d}", [64, HH, NB], BF16) for d in range(4)]
        Mtmp = [B.sb(st, f"Mtmp{d}", [64, HH, NB], F32) for d in range(4)]
        for d in range(4):
            B.memset('pool', Mst[d][:], 0.0, wr=[f"Mst{d}"])
            B.memset('pool', Mbf[d][:], 0.0, wr=[f"Mbf{d}"])
        slots = {}
        for d in range(2):
            for par in range(2):
                sl = {}
                tag = f"{d}{par}"
                sl['tag'] = tag
                sl['ops'] = B.sb(st, "ops" + tag, [128, 4, HH * NB], BF16)
                sl['opc'] = B.sb(st, "opc" + tag, [64, 2, 4, HH * NB], BF16)
                sl['V'] = B.sb(st, "V" + tag, [64, 2, HH, NB], BF16)
                sl['gC'] = B.sb(st, "gC" + tag, [64, 32], F32)
                sl['FT'] = B.sb(st, "FT" + tag, [64, 4, HH, 128], BF16)
                sl['A'] = [B.sb(st, f"A{q}" + tag, [64, 2, HH, NB], BF16) for q in range(3)]
                sl['P'] = [B.sb(st, f"P{q}" + tag, [64, 2, HH, NB], BF16) for q in range(2)]
                sl['Q'] = [B.sb(st, f"Q{q}" + tag, [64, 2, HH, NB], BF16) for q in range(2)]
                sl['TT'] = B.sb(st, "TT" + tag, [64, 2, HH, NB], BF16)
                sl['WmT'] = B.sb(st, "WmT" + tag, [64, 2, HH, NB], BF16)
                sl['X0'] = B.sb(st, "X0" + tag, [64, 2, HH, NB], BF16)
                sl['U0'] = B.sb(st, "U0" + tag, [64, 2, HH, NB], F32)
                sl['U'] = B.sb(st, "U" + tag, [64, 2, HH, NB], BF16)
                sl['y'] = B.sb(st, "y" + tag, [64, 2, HH, NB], F32)
                slots[(d, par)] = sl
        msk = cst['masks']
        i64 = cst['i64']
        f3 = lambda t_: t_.rearrange("p c h n -> p (c h) n")
        bh = lambda t_: t_[0:64, :].unsqueeze(1).broadcast_to([64, 2 * HH, NB])

        def problem(d, i, par):
            sl = slots[(d, par)]
            tag = sl['tag']
            K = lambda s_: s_ + tag
            r0 = 128 * i
            ops, opc, V, gC, FT, TT, WmT, X0, U0, U, y = (sl[x_] for x_ in ('ops', 'opc', 'V', 'gC', 'FT', 'TT', 'WmT', 'X0', 'U0', 'U', 'y'))
            A = sl['A']
            strict = msk['lt'] if d == 0 else msk['gt']
            incl = msk['le'] if d == 0 else msk['ge']
            strictT = msk['gt'] if d == 0 else msk['lt']
            hc = slice(par * HH * NB, (par + 1) * HH * NB)
            B.dma('sp', ops[:], opsD[d][r0:r0 + 128, :, hc], wr=[K('ops')])
            for c_ in range(2):
                B.dma('act', opc[:, c_], opsD[d][r0 + 64 * c_:r0 + 64 * c_ + 64, :, hc], wr=[K('opc')])
            B.dma('sp', V[:].rearrange("p c h n -> p c (h n)"), vbD[r0:r0 + 128, hc].rearrange("(c p) n -> p c n", p=64), wr=[K('V')])
            B.dma('act', gC[:], gCD[i], wr=[K('gC')])
            yield
            for kind in range(4):
                for h in range(HH):
                    B.tr(pTr[:, h, :], ops[:, kind, h * NB:(h + 1) * NB], identb[:],
                         rd=[K('ops'), 'identb'], wr=['pTr'])
                B.cp('act' if kind % 2 == 0 else 'dve', FT[:, kind, :, :], pTr[:], rd=['pTr'], wr=[K(f"FT{kind}")])
            yield
            P0, Q0 = sl['P'][0], sl['Q'][0]
            specs = [
                (1, 0, A[0], strict, K('A0')),
                (1, 3, A[1], incl, K('A1')),
                (2, 3, A[2], incl, K('A2')),
                (2, 0, P0, strict, K('P0')),
                (0, 2, Q0, strictT, K('Q0')),
            ]
            for qi, (lk, rk, dst, m, dk) in enumerate(specs):
                bk, kb = dbank()
                for c in range(2):
                    cs_ = slice(64 * c, 64 * c + 64)
                    for h in range(HH):
                        B.mm(bk[:, c, h, :], FT[:, lk, h, cs_], FT[:, rk, h, cs_], rd=[K(f"FT{lk}"), K(f"FT{rk}")], wr=[kb])
                B.tt('dve', f3(dst[:]), f3(bk[:]), bh(m), ALU.mult, rd=[kb, 'masks'], wr=[dk])
                if qi % 2 == 1:
                    yield
            B.tt('pool', f3(TT[:]), f3(P0[:]), bh(i64), ALU.add, rd=[K('P0'), 'masks'], wr=[K('TT')])
            yield
            cur = 0
            for lev in range(5):
                Pc, Qc = sl['P'][cur], sl['Q'][cur]
                Pn, Qn = sl['P'][1 - cur], sl['Q'][1 - cur]
                kPc, kQc, kPn, kQn = K(f"P{cur}"), K(f"Q{cur}"), K(f"P{1 - cur}"), K(f"Q{1 - cur}")
                bq, kbq = dbank()
                for c in range(2):
                    for h in range(HH):
                        B.mm(bq[:, c, h, :], Pc[:, c, h, :], Qc[:, c, h, :], rd=[kPc, kQc], wr=[kbq])
                B.cp('act', Qn[:], bq[:], rd=[kbq], wr=[kQn])
                if lev < 4:
                    bp, kbp = dbank()
                    for c in range(2):
                        for h in range(HH):
                            B.mm(bp[:, c, h, :], Qc[:, c, h, :], Pc[:, c, h, :], rd=[kPc, kQc], wr=[kbp])
                    B.cp('dve', Pn[:], bp[:], rd=[kbp], wr=[kPn])
                yield
                bt, kbt = dbank()
                for c in range(2):
                    for h in range(HH):
                        B.mm(bt[:, c, h, :], Qn[:, c, h, :], TT[:, c, h, :], rd=[kQn, K('TT')], wr=[kbt])
                B.tt('dve', TT[:], TT[:], bt[:], ALU.add, rd=[kbt, K('TT')], wr=[K('TT')])
                cur = 1 - cur
                yield
            bw, kbw = dbank()
            for c in range(2):
                for h in range(HH):
                    B.mm(bw[:, c, h, :], opc[:, c, 0, h * NB:(h + 1) * NB], TT[:, c, h, :], rd=[K('opc'), K('TT')], wr=[kbw])
            B.cp('act', WmT[:], bw[:], rd=[kbw], wr=[K('WmT')])
            bx, kbx = dbank()
            for c in range(2):
                for h in range(HH):
                    B.mm(bx[:, c, h, :], A[0][:, c, h, :], V[:, c, h, :], rd=[K('A0'), K('V')], wr=[kbx])
            B.cp('dve', X0[:], bx[:], rd=[kbx], wr=[K('X0')])
            yield
            bu, kbu = dbank()
            for c in range(2):
                for h in range(HH):
                    B.mm(bu[:, c, h, :], TT[:, c, h, :], X0[:, c, h, :], rd=[K('TT'), K('X0')], wr=[kbu])
            B.cp('act', U0[:], bu[:], rd=[kbu], wr=[K('U0')])
            yield
            dm = 2 * d + par
            kM, kMb, kMt = f"Mst{dm}", f"Mbf{dm}", f"Mtmp{dm}"
            for c in ((0, 1) if d == 0 else (1, 0)):
                cs_ = slice(64 * c, 64 * c + 64)
                bU, kbU = sbank()
                for h in range(HH):
                    B.mm(bU[:, h, :], WmT[:, c, h, :], Mbf[dm][:, h, :], rd=[K('WmT'), kMb], wr=[kbU])
                B.tt('dve', U[:, c], bU[:], U0[:, c], ALU.add, rd=[kbU, K('U0')], wr=[K('U')])
                bM, kbM = sbank()
                for h in range(HH):
                    B.mm(bM[:, h, :], opc[:, c, 1, h * NB:(h + 1) * NB], V[:, c, h, :], start=True, stop=False,
                         rd=[K('opc'), K('V')], wr=[kbM])
                    B.mm(bM[:, h, :], opc[:, c, 2, h * NB:(h + 1) * NB], U[:, c, h, :], start=False, stop=True,
                         rd=[K('opc'), K('U')], wr=[kbM])
                bY, kbY = sbank()
                for h in range(HH):
                    B.mm(bY[:, h, :], FT[:, 3, h, cs_], Mbf[dm][:, h, :], start=True, stop=False,
                         rd=[K('FT3'), kMb], wr=[kbY])
                    B.mm(bY[:, h, :], A[1][:, c, h, :], V[:, c, h, :], start=False, stop=False,
                         rd=[K('A1'), K('V')], wr=[kbY])
                    B.mm(bY[:, h, :], A[2][:, c, h, :], U[:, c, h, :], start=False, stop=True,
                         rd=[K('A2'), K('U')], wr=[kbY])
                B.cp('act', y[:, c], bY[:], rd=[kbY], wr=[K('y')])
                col = d * 16 + c * 8 + par * HH
                B.tt('dve', Mtmp[dm][:], Mst[dm][:], bM[:], ALU.add, rd=[kM, kbM], wr=[kMt])
                B.tt('pool', Mst[dm][:], Mtmp[dm][:], gC[:, col:col + HH].unsqueeze(2).broadcast_to([64, HH, NB]),
                     ALU.mult, rd=[kMt, K('gC')], wr=[kM])
                B.cp('act', Mbf[dm][:], Mst[dm][:], rd=[kM], wr=[kMb])
                yield
            B.dma('sp', yD[d][r0:r0 + 128, hc].rearrange("(c p) n -> p c n", p=64), y[:].rearrange("p c h n -> p c (h n)"),
                  rd=[K('y')], wr=[f"yD{d}_{i}_{par}"])
            yield

        FPm = g['ff_params']
        Mm = g['moe_scr']
        cw13f = B.sb(st, "cw13f", [128, 8, 2 * EH], F32)
        cw2f = B.sb(st, "cw2f", [128, 2, D], F32)
        cw13b = B.sb(st, "cw13b", [128, 8, 2 * EH], BF16)
        cw2b = B.sb(st, "cw2b", [128, 2, D], BF16)

        def conv_gen():
            for e in range(NE):
                B.dma('sp', cw13f[:, :, 0:EH], FPm['w1'][li, e].rearrange("(c p) n -> p c n", p=128), wr=['cw13f'])
                B.dma('sp', cw13f[:, :, EH:2 * EH], FPm['w3'][li, e].rearrange("(c p) n -> p c n", p=128), wr=['cw13f'])
                B.dma('sp', cw2f[:], FPm['w2'][li, e].rearrange("(c p) n -> p c n", p=128), wr=['cw2f'])
                B.cp('pool', cw13b[:], cw13f[:], rd=['cw13f'], wr=['cw13b'])
                B.cp('pool', cw2b[:], cw2f[:], rd=['cw2f'], wr=['cw2b'])
                B.dma('sp', Mm['W13D'][e * 128:(e + 1) * 128, :], cw13b[:].rearrange("p c n -> p (c n)"), rd=['cw13b'], wr=[f"W13D{e}"])
                B.dma('sp', Mm['W2D'][e * 128:(e + 1) * 128, :], cw2b[:].rearrange("p c n -> p (c n)"), rd=['cw2b'], wr=[f"W2D{e}"])
                yield

        conv = conv_gen()
        for step in range(NT):
            try:
                next(conv)
            except StopIteration:
                pass
            gens = [problem(0, step, 0), problem(1, NT - 1 - step, 0), problem(0, step, 1), problem(1, NT - 1 - step, 1)]
            alive = list(gens)
            while alive:
                for gq in list(alive):
                    try:
                        next(gq)
                    except StopIteration:
                        alive.remove(gq)
        B.barrier()

    with ExitStack() as st:
        def bc(name, ap, width):
            t = B.sb(st, name, [128, width], F32)
            B.dma('sp', t[:], ap.partition_broadcast(128), wr=[name])
            return t
        gng = bc("gng", P['gn_g'][li], RW)
        gnb = bc("gnb", P['gn_b'][li], RW)
        yf = [B.sb(st, f"yf{j}", [128, RW], F32) for j in range(2)]
        yb = [B.sb(st, f"yb{j}", [128, RW], F32) for j in range(2)]
        bo = [B.sb(st, f"bo{j}", [128, RW], F32) for j in range(2)]
        gg = [B.sb(st, f"gg{j}", [128, RW], F32) for j in range(2)]
        sq = B.sb(st, "sq", [128, RW], F32)
        s1 = B.sb(st, "s1", [128, HB], F32)
        s2 = B.sb(st, "s2", [128, HB], F32)
        m2 = B.sb(st, "m2", [128, HB], F32)
        yo = [B.sb(st, f"yo{j}", [128, RW], BF16) for j in range(2)]
        hv = lambda t_: t_.rearrange("p (h n) -> p h n", h=HB)
        b8 = lambda t_: t_.unsqueeze(2).broadcast_to([128, HB, NB])
        for i in range(NT):
            n = ts(i)
            j = i % 2
            r0 = 128 * i
            B.dma('sp', yf[j][:], yD[0][r0:r0 + 128, :], wr=[f"yf{j}"])
            B.dma('act', yb[j][:], yD[1][r0:r0 + 128, :], wr=[f"yb{j}"])
            B.dma('sp', bo[j][:], bonusD[r0:r0 + 128, :], wr=[f"bo{j}"])
            B.dma('act', gg[j][:], gD[r0:r0 + 128, :], wr=[f"gg{j}"])
            ky = f"yf{j}"
            B.tt('pool', yf[j][:], yf[j][:], yb[j][:], ALU.add, rd=[ky, f"yb{j}"], wr=[ky])
            B.red(s1[:], hv(yf[j][:]), ALU.add, rd=[ky], wr=['s1'])
            B.act(sq[:], yf[j][:], AF.Square, rd=[ky], wr=['sq'])
            B.red(s2[:], hv(sq[:]), ALU.add, rd=['sq'], wr=['s2'])
            B.tsc('dve', s1[:], s1[:], 1.0 / NB, None, ALU.mult, rd=['s1'], wr=['s1'])
            B.tt('dve', m2[:], s1[:], s1[:], ALU.mult, rd=['s1'], wr=['m2'])
            B.stt(s2[:], s2[:], 1.0 / NB, m2[:], ALU.mult, ALU.subtract, rd=['s2', 'm2'], wr=['s2'])
            B.act(s2[:], s2[:], AF.Sqrt, rd=['s2'], wr=['s2'], bias=B.eps_ap(GN_EPS, 128), scale=1.0)
            B.recip(s2[:], s2[:], rd=['s2'], wr=['s2'])
            B.tt('dve', hv(yf[j][:]), hv(yf[j][:]), b8(s1[:]), ALU.subtract, rd=[ky, 's1'], wr=[ky])
            B.tt('dve', hv(yf[j][:]), hv(yf[j][:]), b8(s2[:]), ALU.mult, rd=[ky, 's2'], wr=[ky])
            B.tt('pool', yf[j][:], yf[j][:], gng[:], ALU.mult, rd=[ky, 'gng'], wr=[ky])
            B.tt('pool', yf[j][:], yf[j][:], gnb[:], ALU.add, rd=[ky, 'gnb'], wr=[ky])
            B.tt('pool', yf[j][:], yf[j][:], bo[j][:], ALU.add, rd=[ky, f"bo{j}"], wr=[ky])
            B.tt('dve', yo[j][:], yf[j][:], gg[j][:], ALU.mult, rd=[ky, f"gg{j}"], wr=[f"yo{j}"])
            B.dma('sp', ymixD[r0:r0 + n, RW:2 * RW], yo[j][:n, :], rd=[f"yo{j}"], wr=[f"ymixB{i}"])
        B.barrier()


def _outproj_ln1(B, li, g):
    S = B.S
    hD, ymixD, identb = g['hD'], g['ymixD'], g['identb']
    FP = g['ff_params']
    with ExitStack() as st:
        wout = B.sb(st, "wout", [128, 8, D], BF16)
        for c in range(8):
            B.dma('pool', wout[:, c, :], FP['w_out'][li, c * 128:(c + 1) * 128, :], wr=['wout'])
        gbc = B.sb(st, "g1bc", [128, D], F32)
        bbc = B.sb(st, "b1bc", [128, D], F32)
        B.dma('sp', gbc[:], FP['ln1_g'][li].partition_broadcast(128), wr=['gbc'])
        B.dma('sp', bbc[:], FP['ln1_b'][li].partition_broadcast(128), wr=['bbc'])
        yms = [B.sb(st, f"ym{j}", [128, D], BF16) for j in range(2)]
        hts = [B.sb(st, f"hE{j}", [128, D], F32) for j in range(2)]
        ymT = B.sb(st, "ymT", [128, 8, 128], BF16)
        pTb = B.ps(st, "pTbE", [128, 8, 128], BF16)
        pO = [B.ps(st, f"pOE{j}", [128, 512], F32) for j in range(2)]
        lnw = _ln_work(B, st)
        for i in range(NT):
            n = ts(i)
            j = i % 2
            r0 = 128 * i
            ym, ht = yms[j], hts[j]
            kym, kh = f"ym{j}", f"hE{j}"
            B.dma('sp', ym[:n, :], ymixD[r0:r0 + n, :], wr=[kym])
            B.dma('act', ht[:n, :], hD[r0:r0 + n, :], wr=[kh])
            for c in range(8):
                B.tr(pTb[:, c, :n], ym[:n, c * 128:(c + 1) * 128], identb[:n, :n], rd=[kym, 'identb'], wr=['pTbE'])
            B.cp('act', ymT[:, :, :n], pTb[:, :, :n], rd=['pTbE'], wr=['ymT'])
            for half in range(2):
                for k in range(8):
                    B.mm(pO[half][:n, :], ymT[:, k, :n], wout[:, k, half * 512:(half + 1) * 512],
                         start=(k == 0), stop=(k == 7), rd=['ymT', 'wout'], wr=[f"pOE{half}"])
                B.stt(ht[:n, half * 512:(half + 1) * 512], ht[:n, half * 512:(half + 1) * 512], float(DN_ALPHA),
                      pO[half][:n, :], ALU.mult, ALU.add, rd=[kh, f"pOE{half}"], wr=[kh])
            _layer_norm(B, lnw, ht[:n, :], n, gbc, bbc, LN_EPS, kh)
            B.dma('sp', hD[r0:r0 + n, :], ht[:n, :], rd=[kh], wr=[f"hD{i}"])
        B.barrier()


TBLK = 11
MOE_SPARSE = True


def _moe_ln2(B, li, g, last):
    S = B.S
    hD, ident = g['hD'], g['ident']
    identb = g['identb']
    out = g['out']
    FP = g['ff_params']
    with ExitStack() as st:
        gbc = B.sb(st, "g2bc", [128, D], F32)
        bbc = B.sb(st, "b2bc", [128, D], F32)
        B.dma('sp', gbc[:], FP['ln2_g'][li].partition_broadcast(128), wr=['gbc'])
        B.dma('sp', bbc[:], FP['ln2_b'][li].partition_broadcast(128), wr=['bbc'])
        wr32 = B.sb(st, "wr32", [128, 8, 36], F32)
        for c in range(8):
            B.dma('sp', wr32[:, c, :], FP['wr'][li, c * 128:(c + 1) * 128, :], wr=['wr32'])
        brc = B.sb(st, "brc", [128, 36], F32)
        B.dma('sp', brc[:], FP['br'][li].partition_broadcast(128), wr=['brc'])
        acc = B.sb(st, "acc", [128, TBLK, D], F32)
        hTb = B.sb(st, "hTb", [128, TBLK, 8, 128], BF16)
        gate = B.sb(st, "gate", [128, TBLK, NE], F32)
        hT32 = B.sb(st, "hT32", [128, 8, 128], F32)
        hts = [B.sb(st, f"hF{j}", [128, D], F32) for j in range(2)]
        w13 = [B.sb(st, f"w13_{j}", [128, 8, 2 * EH], BF16) for j in range(2)]
        w2b = [B.sb(st, f"w2b_{j}", [128, 2, D], BF16) for j in range(2)]
        w13f = [B.sb(st, f"w13f_{j}", [128, 8, 2 * EH], F32) for j in range(2)]
        w2f = [B.sb(st, f"w2f_{j}", [128, 2, D], F32) for j in range(2)]
        lg = B.sb(st, "lg", [128, 36], F32)
        gex = B.sb(st, "gex", [128, 4], F32)
        gmk = B.sb(st, "gmk", [128, 4], F32)
        eex = B.sb(st, "eex", [128, NE], F32)
        sel = B.sb(st, "sel", [128, NE], F32)
        top8 = B.sb(st, "top8", [128, 8], F32)
        sc = {k_: B.sb(st, "sc_" + k_, [128, 1], F32) for k_ in ('gmax', 'gsum', 'emax', 'esum', 'coef')}
        sa = B.sb(st, "sa", [128, EH], F32)
        hid = B.sb(st, "hid", [128, EH], BF16)
        hidT = B.sb(st, "hidT", [128, 2, 128], BF16)
        bk = [B.ps(st, f"bkF{j}", [128, 512], F32) for j in range(7)]
        pTh = B.ps(st, "pTh", [128, 2, 128], BF16)
        lnw = _ln_work(B, st)
        wcount = 0
        for blk in range(NT // TBLK):
            tiles = list(range(blk * TBLK, (blk + 1) * TBLK))
            for tl, i in enumerate(tiles):
                n = ts(i)
                j = i % 2
                r0 = 128 * i
                ht, kh = hts[j], f"hF{j}"
                B.dma('sp', ht[:n, :], hD[r0:r0 + n, :], wr=[kh])
                for half in range(2):
                    pt = bk[half]
                    ptv = pt[:].rearrange("p (c n) -> p c n", c=4)
                    for c in range(4):
                        B.tr(ptv[:, c, :n], ht[:n, (half * 4 + c) * 128:(half * 4 + c + 1) * 128], ident[:n, :n],
                             rd=[kh, 'ident'], wr=[f"bkF{half}"])
                    B.cp('act', hT32[:, half * 4:half * 4 + 4, :n], ptv[:, :, :n], rd=[f"bkF{half}"], wr=['hT32'])
                    B.cp('pool', hTb[:, tl, half * 4:half * 4 + 4, :n], hT32[:, half * 4:half * 4 + 4, :n], rd=['hT32'], wr=['hTb'])
                for k in range(8):
                    B.mm(bk[2][:n, 0:36], hT32[:, k, :n], wr32[:, k, :], start=(k == 0), stop=(k == 7),
                         rd=['hT32', 'wr32'], wr=['bkF2'])
                B.tt('dve', lg[:n, :], bk[2][:n, 0:36], brc[:n, :], ALU.add, rd=['bkF2', 'brc'], wr=['lg'])
                B.red(sc['gmax'][:n, :], lg[:n, 0:4], ALU.max, rd=['lg'], wr=['gmax'])
                B.tsc('dve', gmk[:n, :], lg[:n, 0:4], sc['gmax'][:n, 0:1], None, ALU.is_ge, rd=['lg', 'gmax'], wr=['gmk'])
                B.tsc('dve', sc['gmax'][:n, :], sc['gmax'][:n, :], -1.0, None, ALU.mult, rd=['gmax', 'gmk'], wr=['gmax'])
                B.act(gex[:n, :], lg[:n, 0:4], AF.Exp, rd=['lg', 'gmax'], wr=['gex', 'gsum'], bias=sc['gmax'][:n, 0:1],
                      scale=1.0, accum=sc['gsum'][:n, :])
                B.tsc('dve', gmk[:n, :], gmk[:n, :], -1.0, 1e30, ALU.add, ALU.mult, rd=['gmk'], wr=['gmk'])
                lev = lg[:n, 4:36].rearrange("p (g e) -> p g e", g=NG)
                B.tt('dve', lev, lev, gmk[:n, :].unsqueeze(2).broadcast_to([n, NG, EPG]), ALU.add, rd=['lg', 'gmk'], wr=['lg'])
                B.red(sc['emax'][:n, :], lg[:n, 4:36], ALU.max, rd=['lg'], wr=['emax'])
                B.tsc('dve', sc['emax'][:n, :], sc['emax'][:n, :], -1.0, None, ALU.mult, rd=['emax'], wr=['emax'])
                B.act(eex[:n, :], lg[:n, 4:36], AF.Exp, rd=['lg', 'emax'], wr=['eex', 'esum'], bias=sc['emax'][:n, 0:1],
                      scale=1.0, accum=sc['esum'][:n, :])
                S.op('dve', (lambda n=n: lambda e: e.max(out=top8[:n, :], in_=eex[:n, :]))(), reads=['eex'], writes=['top8'])
                B.tsc('dve', sel[:n, :], eex[:n, :], top8[:n, 1:2], None, ALU.is_ge, rd=['eex', 'top8'], wr=['sel'])
                B.tt('dve', sc['coef'][:n, :], sc['gsum'][:n, :], sc['esum'][:n, :], ALU.mult, rd=['gsum', 'esum'], wr=['coef'])
                B.recip(sc['coef'][:n, :], sc['coef'][:n, :], rd=['coef'], wr=['coef'])
                B.stt(gate[:n, tl, :], eex[:n, :], sc['coef'][:n, 0:1], sel[:n, :], ALU.mult, ALU.mult,
                      rd=['eex', 'coef', 'sel'], wr=['gate'])
            for e in range(NE):
                wj = wcount % 2
                wcount += 1
                wa, wb = w13[wj], w2b[wj]
                kwa, kwb = f"w13_{wj}", f"w2b_{wj}"
                waf, wbf = w13f[wj], w2f[wj]
                kwaf, kwbf = f"w13f_{wj}", f"w2f_{wj}"
                B.dma('sp', waf[:, :, 0:EH], FP['w1'][li, e].rearrange("(c p) n -> p c n", p=128), wr=[kwaf])
                B.dma('sp', waf[:, :, EH:2 * EH], FP['w3'][li, e].rearrange("(c p) n -> p c n", p=128), wr=[kwaf])
                B.dma('sp', wbf[:], FP['w2'][li, e].rearrange("(c p) n -> p c n", p=128), wr=[kwbf])
                B.cp('pool', wa[:], waf[:], rd=[kwaf], wr=[kwa])
                B.cp('pool', wb[:], wbf[:], rd=[kwbf], wr=[kwb])
                for tl, i in enumerate(tiles):
                    n = ts(i)
                    pH, kpH = bk[tl % 2], f"bkF{tl % 2}"
                    for k in range(8):
                        B.mm(pH[:n, :], hTb[:, tl, k, :n], wa[:, k, :], start=(k == 0), stop=(k == 7),
                             rd=['hTb', kwa], wr=[kpH])
                    B.act(sa[:n, :], pH[:n, 0:EH], AF.Silu, rd=[kpH], wr=['sa'])
                    B.stt(hid[:n, :], pH[:n, EH:2 * EH], gate[:n, tl, e:e + 1], sa[:n, :], ALU.mult, ALU.mult,
                          rd=[kpH, 'gate', 'sa'], wr=['hid'])
                    for c in range(2):
                        B.tr(pTh[:, c, :n], hid[:n, c * 128:(c + 1) * 128], identb[:n, :n], rd=['hid', 'identb'], wr=['pTh'])
                    B.cp('act', hidT[:, :, :n], pTh[:, :, :n], rd=['pTh'], wr=['hidT'])
                    for half in range(2):
                        po, kpo = bk[3 + 2 * (tl % 2) + half], f"bkF{3 + 2 * (tl % 2) + half}"
                        for c in range(2):
                            B.mm(po[:n, :], hidT[:, c, :n], wb[:, c, half * 512:(half + 1) * 512],
                                 start=(c == 0), stop=(c == 1), rd=['hidT', kwb], wr=[kpo])
                        dst = acc[:n, tl, half * 512:(half + 1) * 512]
                        if e == 0:
                            B.cp('dve', dst, po[:n, :], rd=[kpo], wr=[f"acc{tl}"])
                        else:
                            B.tt('dve', dst, dst, po[:n, :], ALU.add, rd=[kpo, f"acc{tl}"], wr=[f"acc{tl}"])
            for tl, i in enumerate(tiles):
                n = ts(i)
                j = i % 2
                r0 = 128 * i
                ht, kh = hts[j], f"hF{j}"
                B.dma('sp', ht[:n, :], hD[r0:r0 + n, :], wr=[kh])
                B.stt(ht[:n, :], ht[:n, :], float(DN_ALPHA), acc[:n, tl, :], ALU.mult, ALU.add, rd=[kh, f"acc{tl}"], wr=[kh])
                _layer_norm(B, lnw, ht[:n, :], n, gbc, bbc, LN_EPS, kh)
                if not last:
                    B.dma('sp', hD[r0:r0 + n, :], ht[:n, :], rd=[kh], wr=[f"hD{i}"])
                else:
                    if i == 0:
                        o_ = B.dma('sp', out[0:128 - NMETA, :], ht[NMETA:128, :], rd=[kh], wr=[f"out{i}"])
                    else:
                        o_ = B.dma('sp', out[r0 - NMETA:r0 - NMETA + n, :], ht[:n, :], rd=[kh], wr=[f"out{i}"])
                    B.out_dmas.append(o_)
        B.barrier()


_IN_NAMES = ['meta_tokens', 'emb_ln_g', 'emb_ln_b', 'w_in', 'mla_q_norm', 'mla_kv_norm', 'mla_w_uq', 'mla_w_ukv',
             'mla_out_norm', 'rwkv_mu_prev', 'rwkv_mu_next', 'rwkv_w0', 'rwkv_w2', 'rwkv_a0', 'rwkv_a2', 'rwkv_g2',
             'rwkv_k_k', 'rwkv_k_a', 'rwkv_r_k', 'rwkv_gn_g', 'rwkv_gn_b', 'rwkv_v0', 'rwkv_v1', 'rwkv_v2', 'w_out',
             'ln1_g', 'ln1_b', 'moe_w1', 'moe_w3', 'moe_w2', 'ln2_g', 'ln2_b']


def kernel(**inputs):
    inputs = {k: np.asarray(v) for k, v in inputs.items()}
    nc, _ = build_program(nlayers=DEPTH)
    common = {k: np.ascontiguousarray(inputs[k], dtype=np.float32) for k in _IN_NAMES}
    common.update(host_consts())
    common.update(host_layout(inputs))
    nb = inputs['x'].shape[0]
    in_maps = []
    for b in range(nb):
        m = dict(common)
        m['x'] = np.ascontiguousarray(inputs['x'][b], dtype=np.float32)
        m['positions'] = np.ascontiguousarray(inputs['positions'][b].reshape(-1, 1).astype(np.int32))
        in_maps.append(m)
    res = run_bass_kernel_spmd(nc, in_maps, core_ids=list(range(nb)))
    return np.stack([np.asarray(res.results[b]['out'], dtype=np.float32) for b in range(nb)], axis=0)


NBLK = 96
NSLOT = NBLK * 128


def _moe_sparse_ln2(B, li, g, last):
    S = B.S
    nc = B.nc
    hD, ident, identb, out = g['hD'], g['ident'], g['identb'], g['out']
    FP = g['ff_params']
    M = g['moe_scr']
    W13D, W2D, XgD, YgD = M['W13D'], M['W2D'], M['XgD'], M['YgD']
    IOA = bass.IndirectOffsetOnAxis

    with ExitStack() as pst:
        S1 = B.sb(pst, "S1", [128, NT, NE], F32)
        S2 = B.sb(pst, "S2", [128, NT, NE], F32)
        PC = B.sb(pst, "PC", [128, NT, NE], F32)
        G = B.sb(pst, "Gt", [128, NT, 2], F32)
        SL = B.sb(pst, "SL", [128, NT * 2], I32)
        widx = B.sb(pst, "widx", [128, NBLK], I32)
        pstart = B.sb(pst, "pstart", [128, NE], F32)
        B.memset('pool', S1[:], 0.0, wr=['S1'])
        B.memset('pool', S2[:], 0.0, wr=['S2'])
        if not hasattr(B, 'breg'):
            def _mkreg(e):
                B.breg = e.to_reg(NSLOT - 1)
                return e.nop()
            S.op('pool', _mkreg)
            B.breg = None

        with ExitStack() as st:
            zt = B.sb(st, "zt", [128, 8 * D], BF16)
            B.memset('pool', zt[:], 0.0, wr=['zt'])
            for zb in range(NSLOT // 1024):
                B.dma('act', XgD[zb * 1024:(zb + 1) * 1024, :].rearrange("(p r) c -> p (r c)", p=128), zt[:],
                      rd=['zt'], wr=[f"XgZ{zb}"])
            wr32 = B.sb(st, "wr32", [128, 8, 36], F32)
            for c in range(8):
                B.dma('sp', wr32[:, c, :], FP['wr'][li, c * 128:(c + 1) * 128, :], wr=['wr32'])
            brc = B.sb(st, "brc", [128, 36], F32)
            B.dma('sp', brc[:], FP['br'][li].partition_broadcast(128), wr=['brc'])
            cm = B.sb(st, "cmoe", [128, 33 + NBLK], F32)
            B.dma('sp', cm[:], g['cmoe_in'].partition_broadcast(128), wr=['cm'])
            trix = B.sb(st, "trix", [128, 128], BF16)
            onesb = B.sb(st, "onesb", [128, 128], BF16)
            B.dma('pool', trix[:], g['trix_in'], wr=['trix'])
            B.memset('pool', onesb[:], 1.0, wr=['onesb'])
            carry = B.sb(st, "carry", [128, NE], F32)
            B.memset('pool', carry[:], 0.0, wr=['carry'])
            hT32 = B.sb(st, "hT32", [128, 8, 128], F32)
            hts = [B.sb(st, f"hF{j}", [128, D], F32) for j in range(2)]
            hbs = [B.sb(st, f"hB{j}", [128, D], BF16) for j in range(2)]
            lg = B.sb(st, "lg", [128, 36], F32)
            gex = B.sb(st, "gex", [128, 4], F32)
            gmk = B.sb(st, "gmk", [128, 4], F32)
            eex = B.sb(st, "eex", [128, NE], F32)
            selb = B.sb(st, "selb", [128, NE], BF16)
            selt = B.sb(st, "selt", [128, NE], F32)
            top8 = B.sb(st, "top8", [128, 8], F32)
            sc = {k_: B.sb(st, "sc_" + k_, [128, 1], F32) for k_ in ('gmax', 'gsum', 'emax', 'esum', 'coef')}
            bk = [B.ps(st, f"bkF{j}", [128, 512], F32) for j in range(4)]
            B.memset('pool', selb[:], 0.0, wr=['selb'])
            for i in range(NT):
                n = ts(i)
                j = i % 2
                r0 = 128 * i
                ht, kh = hts[j], f"hF{j}"
                B.dma('sp', ht[:n, :], hD[r0:r0 + n, :], wr=[kh])
                for half in range(2):
                    pt = bk[half]
                    ptv = pt[:].rearrange("p (c n) -> p c n", c=4)
                    for c in range(4):
                        B.tr(ptv[:, c, :n], ht[:n, (half * 4 + c) * 128:(half * 4 + c + 1) * 128], ident[:n, :n],
                             rd=[kh, 'ident'], wr=[f"bkF{half}"])
                    B.cp('act', hT32[:, half * 4:half * 4 + 4, :n], ptv[:, :, :n], rd=[f"bkF{half}"], wr=['hT32'])
                for k in range(8):
                    B.mm(bk[2][:n, 0:36], hT32[:, k, :n], wr32[:, k, :], start=(k == 0), stop=(k == 7),
                         rd=['hT32', 'wr32'], wr=['bkF2'])
                B.tt('dve', lg[:n, :], bk[2][:n, 0:36], brc[:n, :], ALU.add, rd=['bkF2', 'brc'], wr=['lg'])
                B.red(sc['gmax'][:n, :], lg[:n, 0:4], ALU.max, rd=['lg'], wr=['gmax'])
                B.tsc('dve', gmk[:n, :], lg[:n, 0:4], sc['gmax'][:n, 0:1], None, ALU.is_ge, rd=['lg', 'gmax'], wr=['gmk'])
                B.tsc('dve', sc['gmax'][:n, :], sc['gmax'][:n, :], -1.0, None, ALU.mult, rd=['gmax', 'gmk'], wr=['gmax'])
                B.act(gex[:n, :], lg[:n, 0:4], AF.Exp, rd=['lg', 'gmax'], wr=['gex', 'gsum'], bias=sc['gmax'][:n, 0:1],
                      scale=1.0, accum=sc['gsum'][:n, :])
                B.tsc('dve', gmk[:n, :], gmk[:n, :], -1.0, 1e30, ALU.add, ALU.mult, rd=['gmk'], wr=['gmk'])
                lev = lg[:n, 4:36].rearrange("p (g e) -> p g e", g=NG)
                B.tt('dve', lev, lev, gmk[:n, :].unsqueeze(2).broadcast_to([n, NG, EPG]), ALU.add, rd=['lg', 'gmk'], wr=['lg'])
                B.red(sc['emax'][:n, :], lg[:n, 4:36], ALU.max, rd=['lg'], wr=['emax'])
                B.tsc('dve', sc['emax'][:n, :], sc['emax'][:n, :], -1.0, None, ALU.mult, rd=['emax'], wr=['emax'])
                B.act(eex[:n, :], lg[:n, 4:36], AF.Exp, rd=['lg', 'emax'], wr=['eex', 'esum'], bias=sc['emax'][:n, 0:1],
                      scale=1.0, accum=sc['esum'][:n, :])
                S.op('dve', (lambda n=n: lambda e: e.max(out=top8[:n, :], in_=eex[:n, :]))(), reads=['eex'], writes=['top8'])
                B.tsc('dve', S1[:n, i, :], eex[:n, :], top8[:n, 0:1], None, ALU.is_ge, rd=['eex', 'top8'], wr=['S1'])
                B.tsc('dve', selt[:n, :], eex[:n, :], top8[:n, 1:2], None, ALU.is_ge, rd=['eex', 'top8'], wr=['selt'])
                B.tt('dve', S2[:n, i, :], selt[:n, :], S1[:n, i, :], ALU.subtract, rd=['selt', 'S1'], wr=['S2'])
                B.cp('dve', selb[:n, :], selt[:n, :], rd=['selt'], wr=['selb'])
                B.tt('dve', sc['coef'][:n, :], sc['gsum'][:n, :], sc['esum'][:n, :], ALU.mult, rd=['gsum', 'esum'], wr=['coef'])
                B.recip(sc['coef'][:n, :], sc['coef'][:n, :], rd=['coef'], wr=['coef'])
                B.tsc('dve', G[:n, i, :], top8[:n, 0:2], sc['coef'][:n, 0:1], None, ALU.mult, rd=['top8', 'coef'], wr=['G'])
                B.mm(bk[3][:, 0:NE], trix[:], selb[:], rd=['trix', 'selb'], wr=['bkF3'])
                B.tt('dve', PC[:, i, :], bk[3][:, 0:NE], carry[:], ALU.add, rd=['bkF3', 'carry'], wr=['PC'])
                B.mm(bk[3][:, 64:64 + NE], onesb[:], selb[:], rd=['onesb', 'selb', 'PC'], wr=['bkF3'])
                B.tt('dve', carry[:], carry[:], bk[3][:, 64:64 + NE], ALU.add, rd=['bkF3', 'carry'], wr=['carry'])
                B.cp('pool', hbs[j][:n, :], ht[:n, :], rd=[kh], wr=[f"hB{j}"])
                B.dma('act', g['hbD'][r0:r0 + n, :], hbs[j][:n, :], rd=[f"hB{j}"], wr=[f"hbD{i}"])

            cmp1 = B.sb(st, "cmp1", [128, NE, 33], F32)
            nblk = B.sb(st, "nblk", [128, NE], F32)
            pend = B.sb(st, "pend", [128, NE], F32)
            onesf = B.sb(st, "onesf", [128, NE], F32)
            cmp2 = B.sb(st, "cmp2", [128, NBLK, NE], F32)
            bke = B.sb(st, "bke", [128, NBLK], F32)
            pidx = B.sb(st, "pidx", [128, 1], I32)
            pidf = B.sb(st, "pidf", [128, 1], F32)
            B.memset('pool', onesf[:], 1.0, wr=['onesf'])
            B.tt('dve', cmp1[:], carry[:].unsqueeze(2).broadcast_to([128, NE, 33]),
                 cm[:, 0:33].unsqueeze(1).broadcast_to([128, NE, 33]), ALU.is_gt, rd=['carry', 'cm'], wr=['cmp1'])
            B.red(nblk[:], cmp1[:], ALU.add, rd=['cmp1'], wr=['nblk'])
            B.tsc('dve', nblk[:], nblk[:], 128.0, None, ALU.mult, rd=['nblk'], wr=['nblk'])
            S.op('dve', lambda e: e.tensor_tensor_scan(out=pend[:], data0=onesf[:], data1=nblk[:], initial=0.0,
                                                        op0=ALU.mult, op1=ALU.add), reads=['onesf', 'nblk'], writes=['pend'])
            B.tt('dve', pstart[:], pend[:], nblk[:], ALU.subtract, rd=['pend', 'nblk'], wr=['pstart'])
            B.tt('dve', cmp2[:], pend[:].unsqueeze(1).broadcast_to([128, NBLK, NE]),
                 cm[:, 33:33 + NBLK].unsqueeze(2).broadcast_to([128, NBLK, NE]), ALU.is_le, rd=['pend', 'cm'], wr=['cmp2'])
            B.red(bke[:], cmp2[:], ALU.add, rd=['cmp2'], wr=['bke'])
            B.tsc('dve', bke[:], bke[:], float(NE - 1), None, ALU.min, rd=['bke'], wr=['bke'])
            S.op('pool', lambda e: e.iota(pidx[:], pattern=[[0, 1]], base=0, channel_multiplier=1), writes=['pidx'])
            B.cp('dve', pidf[:], pidx[:], rd=['pidx'], wr=['pidf'])
            B.tsc('dve', bke[:], bke[:], 128.0, pidf[:, 0:1], ALU.mult, ALU.add, rd=['bke', 'pidf'], wr=['bke'])
            B.cp('dve', widx[:], bke[:], rd=['bke'], wr=['widx'])

            slf = B.sb(st, "slf", [128, NT * 2], F32)
            tmpa = B.sb(st, "tmpa", [128, NE], F32)
            tmpb = B.sb(st, "tmpb", [128, NE], F32)
            B.memset('pool', slf[:], 1.0e9, wr=['slf'])
            for i in range(NT):
                n = ts(i)
                B.tt('dve', tmpa[:n, :], PC[:n, i, :], pstart[:n, :], ALU.add, rd=['PC', 'pstart'], wr=['tmpa'])
                B.tt('dve', tmpb[:n, :], tmpa[:n, :], S1[:n, i, :], ALU.mult, rd=['tmpa', 'S1'], wr=['tmpb'])
                B.red(slf[:n, 2 * i:2 * i + 1], tmpb[:n, :], ALU.add, rd=['tmpb', 'slf'], wr=['slf'])
                B.tt('dve', tmpb[:n, :], tmpa[:n, :], S2[:n, i, :], ALU.mult, rd=['tmpa', 'S2', 'slf'], wr=['tmpb'])
                B.red(slf[:n, 2 * i + 1:2 * i + 2], tmpb[:n, :], ALU.add, rd=['tmpb', 'slf'], wr=['slf'])
            B.cp('dve', SL[:], slf[:], rd=['slf'], wr=['SL'])
            hb2 = [B.sb(st, f"hb2_{j}", [128, D], BF16) for j in range(2)]
            for i in range(NT):
                n = ts(i)
                j = i % 2
                r0 = 128 * i
                if n < 128:
                    B.memset('pool', hb2[j][:], 0.0, wr=[f"hb2_{j}"])
                B.dma('sp', hb2[j][:n, :], g['hbD'][r0:r0 + n, :], rd=[f"hbD{i}"], wr=[f"hb2_{j}"])
                for q in range(2):
                    S.op('pool', (lambda j=j, i=i, q=q: lambda e: e.indirect_dma_start(
                        out=XgD[:, :], out_offset=IOA(ap=SL[:, 2 * i + q:2 * i + q + 1], axis=0), in_=hb2[j][:, :], in_offset=None,
                        bounds_check=B.breg, oob_is_err=False))(), reads=[f"hb2_{j}", 'SL'] + [f"XgZ{zb}" for zb in range(NSLOT // 1024)], writes=[f"Xg{i}_{q}"], dma=True)
            B.barrier()

        with ExitStack() as st:
            w13 = [B.sb(st, f"bw13_{j}", [128, 8, 2 * EH], BF16) for j in range(2)]
            w2 = [B.sb(st, f"bw2_{j}", [128, 2, D], BF16) for j in range(2)]
            xb = [B.sb(st, f"xb{j}", [128, D], BF16) for j in range(2)]
            xT = B.sb(st, "xT", [128, 8, 128], BF16)
            sa = B.sb(st, "sa", [128, EH], F32)
            hid = B.sb(st, "hid", [128, EH], BF16)
            hidT = B.sb(st, "hidT", [128, 2, 128], BF16)
            yb = [B.sb(st, f"yb{j}", [128, D], F32) for j in range(2)]
            pX = B.ps(st, "pX", [128, 8, 128], BF16)
            pH = [B.ps(st, f"pH{j}", [128, 512], F32) for j in range(2)]
            pTh = B.ps(st, "pTh", [128, 2, 128], BF16)
            pO = [B.ps(st, f"pOm{j}", [128, 512], F32) for j in range(4)]
            for b in range(NBLK):
                j = b % 2
                S.op('pool', (lambda j=j, b=b: lambda e: e.indirect_dma_start(
                    out=w13[j][:].rearrange("p c n -> p (c n)"), out_offset=None, in_=W13D[:, :],
                    in_offset=IOA(ap=widx[:, b:b + 1], axis=0)))(), reads=['widx'], writes=[f"bw13_{j}"], dma=True)
                S.op('pool', (lambda j=j, b=b: lambda e: e.indirect_dma_start(
                    out=w2[j][:].rearrange("p c n -> p (c n)"), out_offset=None, in_=W2D[:, :],
                    in_offset=IOA(ap=widx[:, b:b + 1], axis=0)))(), reads=['widx'], writes=[f"bw2_{j}"], dma=True)
                B.dma('sp', xb[j][:], XgD[b * 128:(b + 1) * 128, :], wr=[f"xb{j}"])
                for c in range(8):
                    B.tr(pX[:, c, :], xb[j][:, c * 128:(c + 1) * 128], identb[:], rd=[f"xb{j}", 'identb'], wr=['pX'])
                B.cp('act', xT[:], pX[:], rd=['pX'], wr=['xT'])
                ph, kph = pH[j], f"pH{j}"
                for k in range(8):
                    B.mm(ph[:], xT[:, k, :], w13[j][:, k, :], start=(k == 0), stop=(k == 7), rd=['xT', f"bw13_{j}"], wr=[kph])
                B.act(sa[:], ph[:, 0:EH], AF.Silu, rd=[kph], wr=['sa'])
                B.tt('dve', hid[:], ph[:, EH:2 * EH], sa[:], ALU.mult, rd=[kph, 'sa'], wr=['hid'])
                for c in range(2):
                    B.tr(pTh[:, c, :], hid[:, c * 128:(c + 1) * 128], identb[:], rd=['hid', 'identb'], wr=['pTh'])
                B.cp('act', hidT[:], pTh[:], rd=['pTh'], wr=['hidT'])
                for half in range(2):
                    po, kpo = pO[2 * j + half], f"pOm{2 * j + half}"
                    for c in range(2):
                        B.mm(po[:], hidT[:, c, :], w2[j][:, c, half * 512:(half + 1) * 512], start=(c == 0), stop=(c == 1),
                             rd=['hidT', f"bw2_{j}"], wr=[kpo])
                    B.cp('dve' if half == 0 else 'act', yb[j][:, half * 512:(half + 1) * 512], po[:], rd=[kpo], wr=[f"yb{j}"])
                B.dma('sp', YgD[b * 128:(b + 1) * 128, :], yb[j][:], rd=[f"yb{j}"], wr=[f"YgD{b}"])
            B.barrier()

        with ExitStack() as st:
            gbc = B.sb(st, "g2bc", [128, D], F32)
            bbc = B.sb(st, "b2bc", [128, D], F32)
            B.dma('sp', gbc[:], FP['ln2_g'][li].partition_broadcast(128), wr=['gbc'])
            B.dma('sp', bbc[:], FP['ln2_b'][li].partition_broadcast(128), wr=['bbc'])
            hts = [B.sb(st, f"hG{j}", [128, D], F32) for j in range(2)]
            y1 = [B.sb(st, f"y1_{j}", [128, D], F32) for j in range(2)]
            y2 = [B.sb(st, f"y2_{j}", [128, D], F32) for j in range(2)]
            lnw = _ln_work(B, st)
            for i in range(NT):
                n = ts(i)
                j = i % 2
                r0 = 128 * i
                ht, kh = hts[j], f"hG{j}"
                B.dma('sp', ht[:n, :], hD[r0:r0 + n, :], wr=[kh])
                for q, yy in ((0, y1), (1, y2)):
                    S.op('pool', (lambda j=j, i=i, q=q, yy=yy: lambda e: e.indirect_dma_start(
                        out=yy[j][:, :], out_offset=None, in_=YgD[:, :], in_offset=IOA(ap=SL[:, 2 * i + q:2 * i + q + 1], axis=0),
                        bounds_check=B.breg, oob_is_err=False))(), reads=['SL'], writes=[f"y{q + 1}_{j}"], dma=True)
                B.tsc('dve', y1[j][:n, :], y1[j][:n, :], G[:n, i, 0:1], None, ALU.mult, rd=[f"y1_{j}", 'G'], wr=[f"y1_{j}"])
                B.stt(y1[j][:n, :], y2[j][:n, :], G[:n, i, 1:2], y1[j][:n, :], ALU.mult, ALU.add, rd=[f"y1_{j}", f"y2_{j}", 'G'], wr=[f"y1_{j}"])
                B.stt(ht[:n, :], ht[:n, :], float(DN_ALPHA), y1[j][:n, :], ALU.mult, ALU.add, rd=[kh, f"y1_{j}"], wr=[kh])
                _layer_norm(B, lnw, ht[:n, :], n, gbc, bbc, LN_EPS, kh)
                if not last:
                    B.dma('sp', hD[r0:r0 + n, :], ht[:n, :], rd=[kh], wr=[f"hD{i}"])
                else:
                    if i == 0:
                        o_ = B.dma('sp', out[0:128 - NMETA, :], ht[NMETA:128, :], rd=[kh], wr=[f"out{i}"])
                    else:
                        o_ = B.dma('sp', out[r0 - NMETA:r0 - NMETA + n, :], ht[:n, :], rd=[kh], wr=[f"out{i}"])
                    B.out_dmas.append(o_)
            B.barrier()
```
